# Optimizing a Trainium2 kernel written in Bass

```python
import jax, jax.numpy as jnp
from jax import lax
import numpy as np

D_MODEL = 1024
BATCH = 2
SEQ = 16384
DEPTH = 2

GRID_W = 64
CTX_LEN = 256
EPS = 1e-6
NEG_INF = -1e30

W_A = 256
W_B = 256
W_C = 256
W_D = 256
D_MIX = W_A + W_B + W_C + W_D
N_GROUPS = 4
GROUP_W = D_MIX // N_GROUPS
CONV_A = 3
CONV_B = 31
CHUNK = 128
H_C = 4
DH_C = W_C // H_C
H_D = 4
DH_D = W_D // H_D
WIN_R = 8
WIN_C = 16
OFF_A = 0
OFF_B = OFF_A + 3 * W_A
OFF_C = OFF_B + 2 * W_B
OFF_D = OFF_C + 2 * W_C
OFF_KV = OFF_D + W_D
IN_COLS = OFF_D + 3 * W_D
D_FF = 2816
N_EXPERTS = 8
TOP_K = 2
D_FF_EXPERT = 3584
MOE_BLOCK = 128
N_DENSE = (DEPTH + 1) // 2
N_MOE = DEPTH // 2

kernel_name = "hybrid_headgroup_dit_moe"


def rms_norm(x, g):
    xf = x.astype(jnp.float32)
    y = xf * lax.rsqrt(jnp.mean(xf * xf, axis=-1, keepdims=True) + EPS)
    return (y * g.astype(jnp.float32)).astype(x.dtype)


def layer_norm(x, g, b):
    xf = x.astype(jnp.float32)
    mu = jnp.mean(xf, axis=-1, keepdims=True)
    var = jnp.mean(jnp.square(xf - mu), axis=-1, keepdims=True)
    y = (xf - mu) * lax.rsqrt(var + EPS) * g.astype(jnp.float32) + b.astype(jnp.float32)
    return y.astype(x.dtype)


def modulate(h, shift, scale):
    return h * (1 + scale) + shift


def depthwise_conv(x, w):
    k, ch = w.shape
    return lax.conv_general_dilated(
        x, w.astype(x.dtype)[:, None, :], window_strides=(1,),
        padding=[((k - 1) // 2, k // 2)],
        dimension_numbers=("NWC", "WIO", "NWC"), feature_group_count=ch)


def short_conv(p, w):
    bg, cg, xa = p[..., :W_A], p[..., W_A:2 * W_A], p[..., 2 * W_A:]
    return bg * depthwise_conv(cg * xa, w)


def conformer_conv(p, w, bias, ln_g, ln_b):
    y = p[..., :W_B] * jax.nn.sigmoid(p[..., W_B:])
    y = depthwise_conv(y, w) + bias
    return jax.nn.silu(layer_norm(y, ln_g, ln_b))


def spatial_gating(p, ln_g, ln_b, w_s, b_s):
    b, s, _ = p.shape
    z = jax.nn.gelu(p)
    u, v = z[..., :W_C], z[..., W_C:]
    v = layer_norm(v, ln_g, ln_b).reshape(b, s // CHUNK, CHUNK, H_C, DH_C)
    v = jnp.einsum("hpq,bnqhd->bnphd", w_s, v) + b_s.T[:, :, None]
    return u * v.reshape(b, s, W_C)


def heads(t):
    return t.reshape(*t.shape[:-1], H_D, DH_D)


def neighbourhood_attention(q, k, v, k_ctx, v_ctx, rpb):
    b, n = q.shape[:2]
    rows = n // GRID_W
    kr = min(WIN_R, rows)
    kc = min(WIN_C, GRID_W)
    qg = (q * (DH_D ** -0.5)).reshape(b, rows, GRID_W, H_D, DH_D)
    kg = k.reshape(b, rows, GRID_W, H_D, DH_D)
    vg = v.reshape(b, rows, GRID_W, H_D, DH_D)
    r_idx = jnp.arange(rows)
    key_rows = jnp.clip(r_idx - kr // 2, 0, rows - kr)[:, None] + jnp.arange(kr)[None, :]
    k_blk = kg[:, key_rows]
    v_blk = vg[:, key_rows]
    c_idx = jnp.arange(GRID_W)
    c_start = jnp.clip(c_idx - kc // 2, 0, GRID_W - kc)
    col_ok = (c_idx[None, :] >= c_start[:, None]) & (c_idx[None, :] < c_start[:, None] + kc)
    dr = key_rows - r_idx[:, None] + (WIN_R - 1)
    dc = jnp.clip(c_idx[None, :] - c_idx[:, None], -(WIN_C - 1), WIN_C - 1) + (WIN_C - 1)
    bias = rpb[:, dr[:, None, :, None], dc[None, :, None, :]]
    s_loc = jnp.einsum("brqhd,brkchd->bhrqkc", qg, k_blk).astype(jnp.float32)
    s_loc = jnp.where(col_ok[:, None, :], s_loc + bias.astype(jnp.float32), NEG_INF)
    s_ctx = jnp.einsum("brqhd,blhd->bhrql", qg, k_ctx).astype(jnp.float32)
    n_loc = kr * GRID_W
    logits = jnp.concatenate([s_loc.reshape(b, H_D, rows, GRID_W, n_loc), s_ctx], axis=-1)
    prob = jax.nn.softmax(logits, axis=-1).astype(v.dtype)
    p_loc = prob[..., :n_loc].reshape(b, H_D, rows, GRID_W, kr, GRID_W)
    p_ctx = prob[..., n_loc:]
    out = (jnp.einsum("bhrqkc,brkchd->brqhd", p_loc, v_blk)
           + jnp.einsum("bhrql,blhd->brqhd", p_ctx, v_ctx))
    return out.reshape(b, n, W_D)


def context_attention(q, k, v):
    b, l = q.shape[:2]
    s = jnp.einsum("blhd,bmhd->bhlm", q * (DH_D ** -0.5), k).astype(jnp.float32)
    prob = jax.nn.softmax(s, axis=-1).astype(v.dtype)
    return jnp.einsum("bhlm,bmhd->blhd", prob, v).reshape(b, l, W_D)


def merge_groups(ys, group_g, w_out):
    y = jnp.concatenate(ys, axis=-1)
    y = rms_norm(y.reshape(*y.shape[:-1], N_GROUPS, GROUP_W), group_g.reshape(N_GROUPS, GROUP_W))
    return y.reshape(*y.shape[:-2], D_MIX) @ w_out


def hybrid_mixer(h, hc, w_in, conv_a_w, conv_b_w, conv_b_b, conv_ln_g, conv_ln_b,
                 sgu_ln_g, sgu_ln_b, sgu_w, sgu_b, rpb, group_g, w_out, with_ctx_out):
    p = h @ w_in
    q = heads(p[..., OFF_D:OFF_KV])
    k = heads(p[..., OFF_KV:OFF_KV + W_D])
    v = heads(p[..., OFF_KV + W_D:IN_COLS])
    kv_c = hc @ w_in[:, OFF_KV:IN_COLS]
    k_c, v_c = heads(kv_c[..., :W_D]), heads(kv_c[..., W_D:])
    y = merge_groups([
        short_conv(p[..., OFF_A:OFF_B], conv_a_w),
        conformer_conv(p[..., OFF_B:OFF_C], conv_b_w, conv_b_b, conv_ln_g, conv_ln_b),
        spatial_gating(p[..., OFF_C:OFF_D], sgu_ln_g, sgu_ln_b, sgu_w, sgu_b),
        neighbourhood_attention(q, k, v, k_c, v_c, rpb),
    ], group_g, w_out)
    if not with_ctx_out:
        return y, None
    pc = hc @ w_in[:, :OFF_KV]
    yc = merge_groups([
        short_conv(pc[..., OFF_A:OFF_B], conv_a_w),
        conformer_conv(pc[..., OFF_B:OFF_C], conv_b_w, conv_b_b, conv_ln_g, conv_ln_b),
        spatial_gating(pc[..., OFF_C:OFF_D], sgu_ln_g, sgu_ln_b, sgu_w, sgu_b),
        context_attention(heads(pc[..., OFF_D:OFF_KV]), k_c, v_c),
    ], group_g, w_out)
    return y, yc


def swiglu(h, w1, w3, w2):
    return (jax.nn.silu(h @ w1) * (h @ w3)) @ w2


def moe_swiglu(h, router_w, router_b, w1, w3, w2):
    b, s, d = h.shape
    xt = h.reshape(-1, d)
    t = xt.shape[0]
    logits = (xt @ router_w).astype(jnp.float32) + router_b.astype(jnp.float32)
    top_val, top_idx = lax.top_k(logits, TOP_K)
    gates = jax.nn.softmax(top_val, axis=-1)
    n_assign = t * TOP_K
    e_flat = top_idx.reshape(-1).astype(jnp.int32)
    tok_flat = jnp.repeat(jnp.arange(t, dtype=jnp.int32), TOP_K)
    g_flat = gates.reshape(-1)
    order = jnp.argsort(e_flat)
    e_s, tok_s, g_s = e_flat[order], tok_flat[order], g_flat[order]
    counts = jnp.bincount(e_flat, length=N_EXPERTS).astype(jnp.int32)
    padded = (counts + MOE_BLOCK - 1) // MOE_BLOCK * MOE_BLOCK
    pad_end = jnp.cumsum(padded)
    pad_start = pad_end - padded
    start = jnp.cumsum(counts) - counts
    pos = pad_start[e_s] + jnp.arange(n_assign, dtype=jnp.int32) - start[e_s]
    total = -(-n_assign // MOE_BLOCK) * MOE_BLOCK + N_EXPERTS * MOE_BLOCK
    n_blk = total // MOE_BLOCK
    buf_tok = jnp.full((total,), t, jnp.int32).at[pos].set(tok_s)
    buf_gate = jnp.zeros((total,), jnp.float32).at[pos].set(g_s)
    blk_exp = jnp.minimum(jnp.searchsorted(pad_end, jnp.arange(n_blk, dtype=jnp.int32) * MOE_BLOCK,
                                           side="right"), N_EXPERTS - 1)
    x_pad = jnp.concatenate([xt, jnp.zeros((1, d), xt.dtype)], axis=0)
    xb = x_pad[buf_tok].reshape(n_blk, MOE_BLOCK, d)

    def expert_block(args):
        xblk, e = args
        return swiglu(xblk, w1[e], w3[e], w2[e])

    yb = lax.map(expert_block, (xb, blk_exp)).reshape(total, d)
    yb = yb * buf_gate[:, None].astype(yb.dtype)
    out = jnp.zeros((t + 1, d), yb.dtype).at[buf_tok].add(yb)[:t]
    return out.reshape(b, s, d)


def setup_inputs(seed: int = 0) -> dict:
    key = jax.random.key(seed)
    ks = jax.random.split(key, 32)
    L = DEPTH

    def nrm(k, shape, scale):
        return jax.random.normal(k, shape, jnp.float32) * scale

    return {
        "x": nrm(ks[0], (BATCH, SEQ, D_MODEL), 1.0),
        "c": nrm(ks[1], (BATCH, D_MODEL), 1.0),
        "ctx": nrm(ks[2], (BATCH, CTX_LEN, D_MODEL), 1.0),
        "c_ctx": nrm(ks[3], (D_MODEL,), 1.0),
        "w_mod": nrm(ks[4], (L, D_MODEL, 6 * D_MODEL), 0.5 * D_MODEL ** -0.5),
        "b_mod": nrm(ks[5], (L, 6 * D_MODEL), 0.01),
        "norm1_g": 1.0 + nrm(ks[6], (L, D_MODEL), 0.1),
        "norm2_g": 1.0 + nrm(ks[7], (L, D_MODEL), 0.1),
        "w_in": nrm(ks[8], (L, D_MODEL, IN_COLS), D_MODEL ** -0.5),
        "conv_a_w": nrm(ks[9], (L, CONV_A, W_A), CONV_A ** -0.5),
        "conv_b_w": nrm(ks[10], (L, CONV_B, W_B), CONV_B ** -0.5),
        "conv_b_b": nrm(ks[11], (L, W_B), 0.02),
        "conv_ln_g": 1.0 + nrm(ks[12], (L, W_B), 0.1),
        "conv_ln_b": nrm(ks[13], (L, W_B), 0.1),
        "sgu_ln_g": 1.0 + nrm(ks[14], (L, W_C), 0.1),
        "sgu_ln_b": nrm(ks[15], (L, W_C), 0.1),
        "sgu_w": nrm(ks[16], (L, H_C, CHUNK, CHUNK), CHUNK ** -0.5),
        "sgu_b": 1.0 + nrm(ks[17], (L, H_C, CHUNK), 0.1),
        "rpb": nrm(ks[18], (L, H_D, 2 * WIN_R - 1, 2 * WIN_C - 1), 0.1),
        "group_g": 1.0 + nrm(ks[19], (L, D_MIX), 0.1),
        "w_out": nrm(ks[20], (L, D_MIX, D_MODEL), D_MIX ** -0.5),
        "ffn_w1": nrm(ks[21], (N_DENSE, D_MODEL, D_FF), D_MODEL ** -0.5),
        "ffn_w3": nrm(ks[22], (N_DENSE, D_MODEL, D_FF), D_MODEL ** -0.5),
        "ffn_w2": nrm(ks[23], (N_DENSE, D_FF, D_MODEL), D_FF ** -0.5),
        "router_w": nrm(ks[24], (N_MOE, D_MODEL, N_EXPERTS), D_MODEL ** -0.5),
        "router_b": nrm(ks[25], (N_MOE, N_EXPERTS), 0.01),
        "moe_w1": nrm(ks[26], (N_MOE, N_EXPERTS, D_MODEL, D_FF_EXPERT), D_MODEL ** -0.5),
        "moe_w3": nrm(ks[27], (N_MOE, N_EXPERTS, D_MODEL, D_FF_EXPERT), D_MODEL ** -0.5),
        "moe_w2": nrm(ks[28], (N_MOE, N_EXPERTS, D_FF_EXPERT, D_MODEL), D_FF_EXPERT ** -0.5),
        "final_g": 1.0 + nrm(ks[29], (D_MODEL,), 0.1),
    }


def reference(x, c, ctx, c_ctx, w_mod, b_mod, norm1_g, norm2_g, w_in, conv_a_w, conv_b_w,
              conv_b_b, conv_ln_g, conv_ln_b, sgu_ln_g, sgu_ln_b, sgu_w, sgu_b, rpb, group_g,
              w_out, ffn_w1, ffn_w3, ffn_w2, router_w, router_b, moe_w1, moe_w3, moe_w2, final_g):
    xl, xc = x, ctx
    n_ctx = ctx.shape[1]
    sc_lat = jax.nn.silu(c)
    sc_ctx = jax.nn.silu(c_ctx)
    for l in range(DEPTH):
        last = l == DEPTH - 1
        mod = (sc_lat @ w_mod[l] + b_mod[l])[:, None, :]
        mod_c = sc_ctx @ w_mod[l] + b_mod[l]
        sh1, s1, g1, sh2, s2, g2 = jnp.split(mod, 6, axis=-1)
        csh1, cs1, cg1, csh2, cs2, cg2 = jnp.split(mod_c, 6, axis=-1)
        h = modulate(rms_norm(xl, norm1_g[l]), sh1, s1)
        hc = modulate(rms_norm(xc, norm1_g[l]), csh1, cs1)
        y, yc = hybrid_mixer(h, hc, w_in[l], conv_a_w[l], conv_b_w[l], conv_b_b[l], conv_ln_g[l],
                             conv_ln_b[l], sgu_ln_g[l], sgu_ln_b[l], sgu_w[l], sgu_b[l], rpb[l],
                             group_g[l], w_out[l], not last)
        xl = xl + g1 * y
        h = modulate(rms_norm(xl, norm2_g[l]), sh2, s2)
        if not last:
            xc = xc + cg1 * yc
            hc = modulate(rms_norm(xc, norm2_g[l]), csh2, cs2)
            h = jnp.concatenate([hc, h], axis=1)
        if l % 2 == 0:
            f = swiglu(h, ffn_w1[l // 2], ffn_w3[l // 2], ffn_w2[l // 2])
        else:
            f = moe_swiglu(h, router_w[l // 2], router_b[l // 2], moe_w1[l // 2],
                           moe_w3[l // 2], moe_w2[l // 2])
        if not last:
            xc = xc + cg2 * f[:, :n_ctx]
            f = f[:, n_ctx:]
        xl = xl + g2 * f
    return rms_norm(xl, final_g)
```

```python
import numpy as np
from contextlib import ExitStack
import concourse.bass as bass
import concourse.mybir as mybir
from concourse.bass_utils import run_bass_kernel_spmd

F32 = mybir.dt.float32
BF16 = mybir.dt.bfloat16
I32 = mybir.dt.int32
AF = mybir.ActivationFunctionType
ALU = mybir.AluOpType

NCORES = 8
D = 1024
NT0 = 40
PADF, PADB = 1, 3
TT = PADF + NT0 + PADB
EXT_LO, EXT_HI = 2, 38
OWN_LO, OWN_HI = 4, 36
EPS = 1e-6
DFF = 2816
DFE = 3584
NEXP = 8
NEG = -1.0e4
DEBUG_OUT = False
DBG_P2 = 9
STOP_AFTER = None


class Track:
    __slots__ = ("w", "r")

    def __init__(self):
        self.w = None
        self.r = []


def tracks(n):
    return [Track() for _ in range(n)]


class Prog:
    def __init__(self, nc, es):
        self.nc = nc
        self.E = {"pe": nc.tensor, "act": nc.scalar, "dve": nc.vector, "pool": nc.gpsimd, "sp": nc.sync}
        self.sem = {}
        self.cnt = {}
        for e in ("pe", "act", "dve", "pool"):
            self.sem[e] = es.enter_context(nc.semaphore("s_" + e))
            self.cnt[e] = 0
        self.dq = {}
        for q, k in (("sp", 16), ("pool", 12), ("act", 6)):
            sems = [es.enter_context(nc.semaphore("d_%s%d" % (q, i))) for i in range(k)]
            self.dq[q] = {"sems": sems, "n": 0}
            for i, s in enumerate(sems):
                self.sem[("d", q, i)] = s
        self.waited = {e: {} for e in self.E}
        self.pending = {e: [] for e in self.E}
        self.ninst = 0

    def _deps(self, reads, writes):
        evs = []
        for t in reads:
            if t.w is not None:
                evs.append(t.w)
        for t in writes:
            if t.w is not None:
                evs.append(t.w)
            evs.extend(t.r)
        return evs

    def _wait(self, eng, evs):
        need = {}
        for (k, v) in evs:
            if k == eng and eng == "pe":
                continue
            if v > need.get(k, 0):
                need[k] = v
        w = self.waited[eng]
        for k, v in need.items():
            if w.get(k, 0) >= v:
                continue
            w[k] = v
            self.E[eng].wait_ge(self.sem[k], v)

    def op(self, eng, fn, reads=(), writes=(), signal=True):
        self._wait(eng, self._deps(reads, writes))
        inst = fn(self.E[eng])
        self.ninst += 1
        if signal:
            self.cnt[eng] += 1
            inst.then_inc(self.sem[eng], 1)
            ev = (eng, self.cnt[eng])
            for (rs, ws) in self.pending[eng] + [(reads, writes)]:
                for t in rs:
                    t.r.append(ev)
                for t in ws:
                    t.w = ev
                    t.r = []
            self.pending[eng] = []
        else:
            self.pending[eng].append((tuple(reads), tuple(writes)))
        return inst

    def dma(self, q, out, in_, reads=(), writes=()):
        pool = self.dq[q]
        i = pool["n"]
        k = len(pool["sems"])
        idx, gen = i % k, i // k
        key = ("d", q, idx)
        evs = self._deps(reads, writes)
        if gen > 0:
            evs.append((key, 16 * gen))
        self._wait(q, evs)
        inst = self.E[q].dma_start(out=out, in_=in_)
        inst.then_inc(pool["sems"][idx], 16)
        pool["n"] += 1
        self.ninst += 1
        ev = (key, 16 * (gen + 1))
        for t in reads:
            t.r.append(ev)
        for t in writes:
            t.w = ev
            t.r = []

    def idma(self, out, out_offset, in_, in_offset, reads=(), writes=()):
        q = "pool"
        pool = self.dq[q]
        i = pool["n"]
        k = len(pool["sems"])
        idx, gen = i % k, i // k
        key = ("d", q, idx)
        evs = self._deps(reads, writes)
        if gen > 0:
            evs.append((key, 16 * gen))
        self._wait(q, evs)
        inst = self.E[q].indirect_dma_start(out=out, out_offset=out_offset, in_=in_, in_offset=in_offset)
        inst.then_inc(pool["sems"][idx], 16)
        pool["n"] += 1
        self.ninst += 1
        ev = (key, 16 * (gen + 1))
        for t in reads:
            t.r.append(ev)
        for t in writes:
            t.w = ev
            t.r = []

    def barrier(self):
        for e in self.pending:
            assert not self.pending[e], "pending unsignaled instructions at barrier"
        evs = [(e, self.cnt[e]) for e in ("pe", "act", "dve", "pool") if self.cnt[e] > 0]
        for q, pool in self.dq.items():
            k = len(pool["sems"])
            for idx in range(k):
                n = (pool["n"] - idx + k - 1) // k
                if n > 0:
                    evs.append((("d", q, idx), 16 * n))
        for eng in self.E:
            self._wait(eng, [ev for ev in evs if ev[0] != eng])


def _pcol(v, nch):
    return np.ascontiguousarray(np.asarray(v, np.float32).reshape(nch, 128).T)


def _bias_tables(rpb_l, R0):
    out = np.full((5, 6, 128, 4, 128), NEG, np.float32)
    specs = [(100, 96)] + [(R0 + 0, R0 - 4), (R0 + 2, R0 - 2), (R0 + 60, R0 + 56), (R0 + 62, R0 + 56)]
    kk = np.arange(768)
    jr, kc = kk // 64, kk % 64
    qq = np.arange(128)
    a, qc = qq // 64, qq % 64
    c_start = np.clip(qc - 8, 0, 48)
    col_ok = (kc[:, None] >= c_start[None, :]) & (kc[:, None] < c_start[None, :] + 16)
    dc = np.clip(kc[:, None] - qc[None, :], -15, 15) + 15
    for s, (r, ws) in enumerate(specs):
        rk = ws + jr
        rq = r + a
        lo = np.clip(rq - 4, 0, 248)
        ok = (rk[:, None] >= lo[None, :]) & (rk[:, None] < lo[None, :] + 8) & (rk[:, None] >= 0) & (rk[:, None] < 256)
        ok = ok & col_ok
        dr = np.clip(rk[:, None] - rq[None, :] + 7, 0, 14)
        for h in range(4):
            b = rpb_l[h][dr, dc]
            t = np.where(ok, b, np.float32(NEG)).astype(np.float32)
            out[s, :, :, h, :] = t.reshape(6, 128, 128)
    return out


def _prep_inputs(inp):
    f = lambda k: np.asarray(inp[k], np.float32)
    x, c, ctx, c_ctx = f("x"), f("c"), f("ctx"), f("c_ctx")
    shared = {}
    shared["w_mod"] = f("w_mod")
    shared["b_mod"] = f("b_mod")
    shared["b_modT"] = np.stack([_pcol(f("b_mod")[l], 48) for l in range(2)])
    shared["n1gT"] = np.stack([_pcol(f("norm1_g")[l], 8) for l in range(2)])
    shared["n2gT"] = np.stack([_pcol(f("norm2_g")[l], 8) for l in range(2)])
    shared["w_in"] = f("w_in")
    caw = f("conv_a_w")
    shared["cawT"] = np.ascontiguousarray(caw.reshape(2, 3, 2, 128).transpose(0, 3, 2, 1))
    cbw = f("conv_b_w")
    shared["cbwT"] = np.ascontiguousarray(cbw.reshape(2, 31, 2, 128).transpose(0, 3, 2, 1))
    for nm, key in (("cbbT", "conv_b_b"), ("clgT", "conv_ln_g"), ("clbT", "conv_ln_b")):
        shared[nm] = np.stack([_pcol(f(key)[l], 2) for l in range(2)])
    shared["sgu_ln_g"] = f("sgu_ln_g")
    shared["sgu_ln_b"] = f("sgu_ln_b")
    shared["sgu_wT"] = np.ascontiguousarray(f("sgu_w").transpose(0, 3, 1, 2))
    shared["sgu_bT"] = np.ascontiguousarray(f("sgu_b").transpose(0, 2, 1))
    shared["ggT"] = np.stack([_pcol(f("group_g")[l], 8) for l in range(2)])
    shared["w_out"] = f("w_out")
    shared["ffn_w1"] = f("ffn_w1")[0]
    shared["ffn_w3"] = f("ffn_w3")[0]
    shared["ffn_w2"] = f("ffn_w2")[0]
    shared["router_wT"] = np.ascontiguousarray(f("router_w")[0].reshape(8, 128, 8).transpose(1, 0, 2))
    shared["router_b"] = f("router_b")[0]
    for nm in ("moe_w1", "moe_w3"):
        w = f(nm)[0].reshape(NEXP, 8, 128, 7, 512)
        shared[nm + "r"] = np.ascontiguousarray(w.transpose(0, 3, 2, 1, 4)).reshape(NEXP * 7 * 128, 4096)
    w = f("moe_w2")[0].reshape(NEXP, 7, 4, 128, D)
    shared["moe_w2r"] = np.ascontiguousarray(w.transpose(0, 1, 3, 2, 4)).reshape(NEXP * 7 * 128, 4096)
    shared["iota_p"] = np.arange(128, dtype=np.float32).reshape(128, 1)
    shared["eoff"] = np.tile((np.arange(NEXP, dtype=np.float32) * 4096.0)[None, :], (128, 1))
    shared["bidx"] = np.tile(np.arange(23, dtype=np.float32)[None, :], (128, 1))
    shared["utri"] = np.triu(np.ones((128, 128), np.float32), 1)
    shared["final_g"] = f("final_g")
    rpb = f("rpb")
    maps = []
    for core in range(NCORES):
        b, R0 = core // 4, 64 * (core % 4)
        m = dict(shared)
        xe = np.zeros((NT0 * 128, D), np.float32)
        lo, hi = R0 - 8, R0 + 72
        glo, ghi = max(lo, 0), min(hi, 256)
        xe[(glo - lo) * 64:(ghi - lo) * 64] = x[b, glo * 64:ghi * 64]
        m["x_ext"] = xe
        m["ctxb"] = np.ascontiguousarray(ctx[b])
        m["cT"] = np.ascontiguousarray(np.stack([_pcol(c[b], 8), _pcol(c_ctx, 8)], axis=-1))
        val = np.zeros((128, 10), np.float32)
        for s in range(10):
            r = lo + 8 * s
            val[:, s] = 1.0 if (0 <= r < 256) else 0.0
        m["valid"] = val
        m["btab"] = np.stack([_bias_tables(rpb[l], R0) for l in range(2)])
        maps.append(m)
    return maps


class Seg:
    def __init__(self, nc, name, ntiles, padf, padb):
        self.ntiles, self.padf, self.padb = ntiles, padf, padb
        self.tt = padf + ntiles + padb
        self.arr, self.trk = {}, {}
        for k in ("bg", "uA", "yB", "q", "k", "ynC"):
            self.arr[k] = nc.dram_tensor("%s_%s" % (name, k), [128, 2, self.tt * 128], BF16, kind="Internal").ap()
            self.trk[k] = tracks(self.tt)
        self.arr["v"] = nc.dram_tensor("%s_v" % name, [self.tt, 128, 264], BF16, kind="Internal").ap()
        self.trk["v"] = tracks(self.tt)

    def cols(self, t0, n):
        a = (t0 + self.padf) * 128
        return slice(a, a + n)

    def tr(self, k, t0, t1):
        return self.trk[k][t0 + self.padf:t1 + self.padf]


class KB:
    def __init__(self, nc):
        self.nc = nc

    def act(self, out, in_, func, reads, writes, bias=None, scale=None, accum_out=None):
        kw = {}
        if bias is not None:
            kw["bias"] = bias
        if scale is not None:
            kw["scale"] = scale
        if accum_out is not None:
            kw["accum_out"] = accum_out
        return self.P.op("act", lambda e: e.activation(out=out, in_=in_, func=func, **kw), reads, writes)

    def tt(self, eng, out, in0, in1, op, reads, writes):
        return self.P.op(eng, lambda e: e.tensor_tensor(out=out, in0=in0, in1=in1, op=op), reads, writes)

    def ts(self, eng, out, in0, s1, s2, op0, op1, reads, writes):
        if op1 is None:
            return self.P.op(eng, lambda e: e.tensor_scalar(out=out, in0=in0, scalar1=s1, scalar2=None, op0=op0), reads, writes)
        return self.P.op(eng, lambda e: e.tensor_scalar(out=out, in0=in0, scalar1=s1, scalar2=s2, op0=op0, op1=op1), reads, writes)

    def stt(self, out, in0, scalar, in1, op0, op1, reads, writes):
        return self.P.op("dve", lambda e: e.scalar_tensor_tensor(out=out, in0=in0, scalar=scalar, in1=in1, op0=op0, op1=op1), reads, writes)

    def copy(self, eng, out, in_, reads, writes):
        if eng == "act":
            return self.P.op("act", lambda e: e.activation(out=out, in_=in_, func=AF.Copy), reads, writes)
        return self.P.op(eng, lambda e: e.tensor_copy(out=out, in_=in_), reads, writes)

    def mm(self, out, lhsT, rhs, start, stop, reads, writes, signal=None, sgc=False):
        if signal is None:
            signal = stop
        return self.P.op("pe", lambda e: e.matmul(out, lhsT=lhsT, rhs=rhs, start=start, stop=stop, skip_group_check=sgc),
                         reads, writes, signal=signal)

    def tr(self, out, in_, reads, writes, signal=True):
        ident = self.ident[:]
        return self.P.op("pe", lambda e: e.transpose(out, in_, ident), list(reads) + [self.t_const], writes, signal=signal)

    def sb(self, es, name, shape, dt):
        self._uid = getattr(self, "_uid", 0) + 1
        return es.enter_context(self.nc.sbuf_tensor("%s_u%d" % (name, self._uid), shape, dt))

    def rstd_from(self, dst, src, scale, reads_t, t_dst):
        self.act(dst, src, AF.Ln, reads_t + [self.t_const], [t_dst], bias=self.eps[:, 0:1], scale=scale)
        self.act(dst, dst, AF.Exp, [t_dst], [t_dst], scale=-0.5)

    def setup(self, es):
        nc = self.nc
        self.P = Prog(nc, es)
        P = self.P
        di = lambda name, shape: nc.dram_tensor(name, list(shape), F32, kind="ExternalInput").ap()
        I = {}
        for name, shape in (
            ("x_ext", [NT0 * 128, D]), ("ctxb", [256, D]), ("cT", [128, 8, 2]), ("valid", [128, 10]),
            ("btab", [2, 5, 6, 128, 4, 128]), ("w_mod", [2, D, 6 * D]), ("b_mod", [2, 6 * D]), ("b_modT", [2, 128, 48]),
            ("n1gT", [2, 128, 8]), ("n2gT", [2, 128, 8]), ("w_in", [2, D, 2560]), ("cawT", [2, 128, 2, 3]),
            ("cbwT", [2, 128, 2, 31]), ("cbbT", [2, 128, 2]), ("clgT", [2, 128, 2]), ("clbT", [2, 128, 2]),
            ("sgu_ln_g", [2, 256]), ("sgu_ln_b", [2, 256]), ("sgu_wT", [2, 128, 4, 128]), ("sgu_bT", [2, 128, 4]),
            ("ggT", [2, 128, 8]), ("w_out", [2, D, D]), ("ffn_w1", [D, DFF]), ("ffn_w3", [D, DFF]), ("ffn_w2", [DFF, D]),
            ("router_wT", [128, 8, 8]), ("router_b", [8]), ("moe_w1r", [NEXP * 7 * 128, 4096]), ("moe_w3r", [NEXP * 7 * 128, 4096]),
            ("moe_w2r", [NEXP * 7 * 128, 4096]), ("final_g", [D]), ("ident", [128, 128]), ("iota_p", [128, 1]), ("utri", [128, 128]), ("eoff", [128, NEXP]), ("bidx", [128, 23]),
        ):
            if STOP_AFTER is not None and name.startswith("moe_w"):
                shape = [128, 128]
            I[name] = di(name, shape)
        self.I = I
        self.out = nc.dram_tensor("out", [32 * 128, D], F32, kind="ExternalOutput").ap()
        kind = "ExternalOutput" if DEBUG_OUT else "Internal"
        self.xmid = nc.dram_tensor("xmid", [NT0 * 128, D], F32, kind=kind).ap()
        self.xl1 = nc.dram_tensor("xl1", [NT0 * 128, D], F32, kind=kind).ap()
        self.xcmid = nc.dram_tensor("xcmid", [256, D], F32, kind=kind).ap()
        self.xc1 = nc.dram_tensor("xc1", [256, D], F32, kind=kind).ap()
        self.t_xmid, self.t_xl1 = tracks(NT0), tracks(NT0)
        self.t_xcmid, self.t_xc1 = tracks(2), tracks(2)
        self.segL = Seg(nc, "L", NT0, PADF, PADB)
        self.segC = Seg(nc, "C", 2, 1, 1)
        self.psall = es.enter_context(nc.psum_tensor("psall", [128, 4096], F32))
        self.ps = [self.psall[:, i * 512:(i + 1) * 512] for i in range(8)]
        self.t_ps = tracks(8)
        self.t_const = Track()
        self.ident = self.sb(es, "ident_sb", [128, 128], BF16)
        self.onesM = self.sb(es, "onesM", [128, 128], BF16)
        self.eps = self.sb(es, "eps", [128, 1], F32)
        self.one1 = self.sb(es, "one1", [128, 1], F32)
        self.zeros = self.sb(es, "zeros", [128, 1024], BF16)
        self.valid = self.sb(es, "valid_sb", [128, 10], F32)
        self.scT = self.sb(es, "scT", [128, 8, 2], BF16)
        self.screp = self.sb(es, "screp", [128, 2, 8, 128], BF16)
        cTf = self.sb(es, "cTf", [128, 8, 2], F32)
        tc = self.t_const
        P.dma("pool", self.ident[:], I["ident"], writes=[tc])
        P.dma("sp", self.valid[:], I["valid"], writes=[tc])
        P.dma("sp", cTf[:], I["cT"], writes=[tc])
        P.op("dve", lambda e: e.memset(self.onesM[:], 1.0 / 256.0), [], [tc])
        P.op("dve", lambda e: e.memset(self.eps[:], EPS), [], [tc])
        P.op("dve", lambda e: e.memset(self.one1[:], 1.0), [], [tc])
        P.op("dve", lambda e: e.memset(self.zeros[:], 0.0), [], [tc])
        self.act(self.scT[:], cTf[:], AF.Silu, [tc], [tc])
        for j in range(2):
            for k in range(8):
                self.copy("dve", self.screp[:, j, k, :], self.scT[:, k, j:j + 1].to_broadcast([128, 128]), [tc], [tc])
        NB = self.NBLK
        self.moe_Xs = nc.dram_tensor("moe_xs", [NEXP * 4096, D], BF16, kind="Internal").ap()
        self.moe_Ys = nc.dram_tensor("moe_ys", [NB * 512, D], F32, kind="Internal").ap()
        self.t_moe_Xs = Track()
        for seg in (self.segL, self.segC):
            for k in ("uA", "yB", "k"):
                a = seg.arr[k]
                for c in range(2):
                    P.dma("sp", a[:, c, 0:seg.padf * 128], self.zeros[:, 0:seg.padf * 128], reads=[tc], writes=seg.trk[k][0:seg.padf])
                    b0 = (seg.padf + seg.ntiles) * 128
                    P.dma("sp", a[:, c, b0:b0 + seg.padb * 128], self.zeros[:, 0:seg.padb * 128], reads=[tc], writes=seg.trk[k][seg.padf + seg.ntiles:])
            va = seg.arr["v"]
            for t in list(range(seg.padf)) + list(range(seg.padf + seg.ntiles, seg.tt)):
                P.dma("sp", va[t], self.zeros[:, 0:264], reads=[tc], writes=[seg.trk["v"][t]])

    def mod_phase(self, l, es_layer):
        P, I = self.P, self.I
        M = {}
        for nm in ("modL", "modC"):
            M[nm] = self.sb(es_layer, "%s_%d" % (nm, l), [128, 48], F32)
        for nm in ("gs1L", "gs2L", "gs1C", "gs2C"):
            M[nm] = self.sb(es_layer, "%s_%d" % (nm, l), [128, 8], F32)
        for nm in ("g1L", "g2L", "g1C", "g2C"):
            M[nm] = self.sb(es_layer, "%s_%d" % (nm, l), [128, D], F32)
        t_mod = Track()
        M["t"] = t_mod
        tc = self.t_const
        with ExitStack() as es:
            wm = [self.sb(es, "wm%d" % i, [128, 8, 512], BF16) for i in range(2)]
            t_wm = tracks(2)
            bmb = self.sb(es, "bmb", [128, 2, D], F32)
            bmT = self.sb(es, "bmT", [128, 48], F32)
            ngT = self.sb(es, "ngT", [128, 2, 8], F32)
            t_misc = Track()
            P.dma("sp", bmb[:, 0, :], I["b_mod"][l, 2048:3072].partition_broadcast(128), writes=[t_misc])
            P.dma("sp", bmb[:, 1, :], I["b_mod"][l, 5120:6144].partition_broadcast(128), writes=[t_misc])
            P.dma("sp", bmT[:], I["b_modT"][l], writes=[t_misc])
            P.dma("sp", ngT[:, 0, :], I["n1gT"][l], writes=[t_misc])
            P.dma("sp", ngT[:, 1, :], I["n2gT"][l], writes=[t_misc])
            wsrc = I["w_mod"][l].rearrange("(kc p) n -> p kc n", p=128)
            psm, t_psm = self.ps[0], self.t_ps[0]
            psm_v = psm[:, 0:96].rearrange("p (n j) -> p n j", j=2)
            bi = 0
            for nb in range(12):
                w, tw = wm[nb % 2], t_wm[nb % 2]
                P.dma("pool", w[:], wsrc[:, :, nb * 512:(nb + 1) * 512], writes=[tw])
                for j in range(4):
                    n = nb * 4 + j
                    for k in range(8):
                        self.mm(psm_v[:, n, :], w[:, k, j * 128:(j + 1) * 128], self.scT[:, k, :], k == 0, k == 7,
                                [tw, tc], [t_psm], signal=(k == 7 and j == 3))
                if nb in (4, 5, 10, 11):
                    gi, half = (0 if nb < 6 else 1), nb % 2
                    for ci, nm in enumerate(("L", "C")):
                        pb, tpb = self.ps[1 + bi % 2], self.t_ps[1 + bi % 2]
                        bi += 1
                        for k in range(8):
                            self.mm(pb[:, :], self.screp[:, ci, k, :], w[:, k, :], k == 0, k == 7, [tw, tc], [tpb])
                        dst = M["g%d%s" % (gi + 1, nm)]
                        self.tt("dve", dst[:, half * 512:(half + 1) * 512], pb[:, :], bmb[:, gi, half * 512:(half + 1) * 512],
                                ALU.add, [tpb, t_misc], [t_mod])
            self.tt("dve", M["modL"][:], psm_v[:, :, 0], bmT[:], ALU.add, [t_psm, t_misc], [t_mod])
            self.tt("dve", M["modC"][:], psm_v[:, :, 1], bmT[:], ALU.add, [t_psm, t_misc], [t_mod])
            for nm in ("L", "C"):
                for i, (gs, off) in enumerate((("gs1", 8), ("gs2", 32))):
                    dst = M[gs + nm]
                    self.ts("dve", dst[:], M["mod" + nm][:, off:off + 8], 1.0, None, ALU.add, None, [t_mod], [t_mod])
                    self.tt("dve", dst[:], dst[:], ngT[:, i, :], ALU.mult, [t_mod, t_misc], [t_mod])
            P.barrier()
        return M

    def p1_alloc(self, es, l):
        P, I = self.P, self.I
        B = {}
        B["w_in"] = self.sb(es, "w_in_sb", [128, 8, 2560], BF16)
        B["t_w"] = Track()
        wsrc = I["w_in"][l].rearrange("(kc p) n -> p kc n", p=128)
        for cb in range(5):
            P.dma("pool", B["w_in"][:, :, cb * 512:(cb + 1) * 512], wsrc[:, :, cb * 512:(cb + 1) * 512], writes=[B["t_w"]])
        B["xt"] = [self.sb(es, "p1xt%d" % i, [128, D], F32) for i in range(2)]
        B["t_xt"] = tracks(2)
        B["junk"] = self.sb(es, "p1junk", [128, D], BF16)
        B["xn"] = [self.sb(es, "p1xn%d" % i, [128, D], BF16) for i in range(2)]
        B["t_xn"] = tracks(2)
        B["st"] = self.sb(es, "p1st", [128, 16], F32)
        B["t_st"] = Track()
        B["hT"] = [self.sb(es, "p1hT%d" % i, [128, 8, 512], BF16) for i in range(3)]
        B["t_hT"] = tracks(3)
        for nm in ("bg", "uA", "yB", "q", "k", "ynC"):
            B[nm] = [self.sb(es, "p1%s%d" % (nm, i), [128, 2, 512], BF16) for i in range(2)]
            B["t_" + nm] = tracks(2)
        B["v"] = [self.sb(es, "p1v%d" % i, [128, 4, 4, 66], BF16) for i in range(2)]
        B["t_v"] = tracks(2)
        B["cg"] = [self.sb(es, "p1cg%d" % i, [128, 512], F32) for i in range(2)]
        B["t_cg"] = tracks(2)
        B["sig"] = [self.sb(es, "p1sig%d" % i, [128, 512], BF16) for i in range(2)]
        B["t_sig"] = tracks(2)
        B["zc"] = [self.sb(es, "p1zc%d" % i, [128, 512], F32) for i in range(4)]
        B["t_zc"] = tracks(4)
        B["tv"] = self.sb(es, "p1tv", [128, 256], F32)
        B["vnb"] = self.sb(es, "p1vnb", [128, 256], BF16)
        B["yC"] = self.sb(es, "p1yC", [128, 256], F32)
        B["ynCt"] = self.sb(es, "p1ynCt", [128, 256], BF16)
        B["t_sgu"] = Track()
        B["bst"] = self.sb(es, "p1bst", [128, 16], F32)
        B["swT"] = self.sb(es, "p1swT", [128, 4, 128], BF16)
        B["sbT"] = self.sb(es, "p1sbT", [128, 4], F32)
        B["slg"] = self.sb(es, "p1slg", [128, 256], F32)
        B["slb"] = self.sb(es, "p1slb", [128, 256], F32)
        B["ggT"] = self.sb(es, "p1ggT", [128, 8], F32)
        B["t_par"] = Track()
        tp = B["t_par"]
        P.dma("pool", B["swT"][:], I["sgu_wT"][l], writes=[tp])
        P.dma("sp", B["sbT"][:], I["sgu_bT"][l], writes=[tp])
        P.dma("sp", B["slg"][:], I["sgu_ln_g"][l].partition_broadcast(128), writes=[tp])
        P.dma("sp", B["slb"][:], I["sgu_ln_b"][l].partition_broadcast(128), writes=[tp])
        P.dma("sp", B["ggT"][:], I["ggT"][l], writes=[tp])
        for i in range(2):
            P.op("dve", lambda e, i=i: e.memset(B["v"][i][:], 1.0), [], [B["t_v"][i]])
        B["n"] = 0
        B["nx"] = 0
        return B

    def norm_to_hT(self, B, xt, t_xt, gsT, shT, t_mod, hT_dst, t_hT, ps_i):
        P = self.P
        i = B["nx"] % 2
        B["nx"] += 1
        xn, t_xn = B["xn"][i], B["t_xn"][i]
        st, t_st = B["st"], B["t_st"]
        col = st[:, i:i + 1]
        self.act(B["junk"][:], xt[:], AF.Square, [t_xt], [t_st], accum_out=col)
        self.rstd_from(col, col, 1.0 / D, [t_st], t_st)
        self.ts("dve", xn[:], xt[:], col, None, ALU.mult, None, [t_xt, t_st], [t_xn])
        psT, t_psT = self.ps[ps_i], self.t_ps[ps_i]
        pv = psT[:, :].bitcast(BF16).rearrange("p (k t) -> p k t", k=8)
        for k in range(8):
            self.tr(pv[:, k, :], xn[:, k * 128:(k + 1) * 128], [t_xn], [t_psT], signal=(k == 7))
        for k in range(8):
            self.ts("dve", hT_dst[:, k, :], pv[:, k, :], gsT[:, k:k + 1], shT[:, k:k + 1], ALU.mult, ALU.add, [t_psT, t_mod], [t_hT])

    def phase1(self, B, seg, tiles, xsrc, t_xsrc, gsT, shT, t_mod, valid_ap, kv_only=False):
        for g in self.phase1_parts(B, seg, tiles, xsrc, t_xsrc, gsT, shT, t_mod, valid_ap, kv_only):
            for _ in g:
                pass

    def phase1_parts(self, B, seg, tiles, xsrc, t_xsrc, gsT, shT, t_mod, valid_ap, kv_only=False):
        it = B["n"]
        B["n"] += 1
        sel = it % 2
        hsel = it % 3
        S = {nm: (B[nm][sel], B["t_" + nm][sel]) for nm in ("bg", "uA", "yB", "q", "k", "ynC", "v")}
        return (self.phase1_N(B, tiles, xsrc, t_xsrc, gsT, shT, t_mod, hsel),
                self.phase1_F(B, seg, tiles, valid_ap, kv_only, hsel, S),
                self.phase1_B(B, seg, tiles, kv_only, hsel, S))

    def phase1_N(self, B, tiles, xsrc, t_xsrc, gsT, shT, t_mod, hsel):
        P = self.P
        hT, t_hT = B["hT"][hsel], B["t_hT"][hsel]
        for j, t in enumerate(tiles):
            i = B["nx"] % 2
            xt, t_xt = B["xt"][i], B["t_xt"][i]
            P.dma("sp", xt[:], xsrc[t * 128:(t + 1) * 128, :], reads=([t_xsrc[t]] if t_xsrc is not None else []), writes=[t_xt])
            self.norm_to_hT(B, xt, t_xt, gsT, shT, t_mod, hT[:, :, j * 128:(j + 1) * 128], t_hT, 3)
            yield

    def phase1_F(self, B, seg, tiles, valid_ap, kv_only, sel, S):
        P = self.P
        nt = len(tiles)
        N = nt * 128
        hT, t_hT = B["hT"][sel], B["t_hT"][sel]
        w_in, t_w = B["w_in"], B["t_w"]
        tc = self.t_const
        order = [16, 17] if kv_only else [0, 1, 2, 4, 3, 5, 8, 6, 9, 7, 14, 15, 16, 17]
        for oi, cc in enumerate(order):
            pb, tpb = self.ps[oi % 3], self.t_ps[oi % 3]
            for k in range(8):
                self.mm(pb[:, 0:N], w_in[:, k, cc * 128:(cc + 1) * 128], hT[:, k, 0:N], k == 0, k == 7, [t_w, t_hT], [tpb])
            c = cc % 2
            if cc in (0, 1):
                self.copy("act", S["bg"][0][:, c, 0:N], pb[:, 0:N], [tpb], [S["bg"][1]])
            elif cc in (2, 3):
                self.copy("act", B["cg"][c][:, 0:N], pb[:, 0:N], [tpb], [B["t_cg"][c]])
            elif cc in (4, 5):
                self.stt(S["uA"][0][:, c, 0:N], pb[:, 0:N], valid_ap, B["cg"][c][:, 0:N], ALU.mult, ALU.mult,
                         [tpb, B["t_cg"][c], tc], [S["uA"][1]])
            elif cc in (8, 9):
                self.act(B["sig"][c][:, 0:N], pb[:, 0:N], AF.Sigmoid, [tpb], [B["t_sig"][c]])
            elif cc in (6, 7):
                self.stt(S["yB"][0][:, c, 0:N], pb[:, 0:N], valid_ap, B["sig"][c][:, 0:N], ALU.mult, ALU.mult,
                         [tpb, B["t_sig"][c], tc], [S["yB"][1]])
            elif cc in (14, 15):
                self.copy("act" if c else "dve", S["q"][0][:, c, 0:N], pb[:, 0:N], [tpb], [S["q"][1]])
            else:
                self.copy("act" if c else "dve", S["k"][0][:, c, 0:N], pb[:, 0:N], [tpb], [S["k"][1]])
            yield
        t0 = tiles[0]
        cs = seg.cols(t0, N)
        names = ("k",) if kv_only else ("bg", "uA", "yB", "q", "k")
        for nm in names:
            for c in range(2):
                P.dma("pool", seg.arr[nm][:, c, cs], S[nm][0][:, c, 0:N], reads=[S[nm][1]], writes=seg.tr(nm, t0, t0 + nt))
        yield

    def phase1_B(self, B, seg, tiles, kv_only, sel, S):
        P = self.P
        nt = len(tiles)
        N = nt * 128
        hT, t_hT = B["hT"][sel], B["t_hT"][sel]
        w_in, t_w = B["w_in"], B["t_w"]
        for j, t in enumerate(tiles):
            pv, tpv = self.ps[6], self.t_ps[6]
            for k in range(8):
                self.mm(pv[:, 0:256], hT[:, k, j * 128:(j + 1) * 128], w_in[:, k, 2304:2560], k == 0, k == 7, [t_w, t_hT], [tpv])
            self.copy("dve", S["v"][0][:, j, :, 0:64], pv[:, 0:256].rearrange("p (h d) -> p h d", h=4), [tpv], [S["v"][1]])
            yield
            if kv_only:
                continue
            pc, tpc = self.ps[4 + j % 2], self.t_ps[4 + j % 2]
            for k in range(8):
                self.mm(pc[:, :], hT[:, k, j * 128:(j + 1) * 128], w_in[:, k, 1280:1792], k == 0, k == 7, [t_w, t_hT], [tpc])
            self.act(B["zc"][j][:], pc[:, :], AF.Gelu_apprx_tanh, [tpc], [B["t_zc"][j]])
            yield
        if not kv_only:
            for j, t in enumerate(tiles):
                for _ in self.sgu_tile(B, S["ynC"][0][:, :, j * 128:(j + 1) * 128], S["ynC"][1], j):
                    yield
        t0 = tiles[0]
        cs = seg.cols(t0, N)
        if not kv_only:
            for c in range(2):
                P.dma("pool", seg.arr["ynC"][:, c, cs], S["ynC"][0][:, c, 0:N], reads=[S["ynC"][1]], writes=seg.tr("ynC", t0, t0 + nt))
        for j, t in enumerate(tiles):
            P.dma("pool", seg.arr["v"][t + seg.padf], S["v"][0][:, j, :, :].rearrange("p h d -> p (h d)"),
                  reads=[S["v"][1]], writes=seg.tr("v", t, t + 1))
        yield

    def sgu_tile(self, B, ynC_dst, t_ynC, j):
        P = self.P
        zc, t_zc = B["zc"][j], B["t_zc"][j]
        tp, ts_ = B["t_par"], B["t_sgu"]
        bst = B["bst"]
        stats = B["tv"]
        P.op("dve", lambda e: e.bn_stats(out=bst[:, 0:6], in_=zc[:, 256:512]), [t_zc], [ts_])
        mv = bst[:, 8:10]
        P.op("dve", lambda e: e.bn_aggr(out=mv[:, 0:2], in_=bst[:, 0:6]), [ts_], [ts_])
        self.rstd_from(mv[:, 1:2], mv[:, 1:2], 1.0, [ts_], ts_)
        self.ts("dve", stats[:], zc[:, 256:512], mv[:, 0:1], mv[:, 1:2], ALU.subtract, ALU.mult, [t_zc, ts_], [ts_])
        self.tt("dve", stats[:], stats[:], B["slg"][:], ALU.mult, [ts_, tp], [ts_])
        self.tt("dve", B["vnb"][:], stats[:], B["slb"][:], ALU.add, [ts_, tp], [ts_])
        yield
        pm, tpm = self.ps[6], self.t_ps[6]
        for h in range(4):
            self.mm(pm[:, 256 + h * 64:256 + (h + 1) * 64], B["swT"][:, h, :], B["vnb"][:, h * 64:(h + 1) * 64], True, True,
                    [ts_, tp], [tpm], signal=(h == 3))
        yC = B["yC"]
        for h in range(4):
            self.stt(yC[:, h * 64:(h + 1) * 64], pm[:, 256 + h * 64:256 + (h + 1) * 64], B["sbT"][:, h:h + 1],
                     zc[:, h * 64:(h + 1) * 64], ALU.add, ALU.mult, [tpm, tp, t_zc, ts_], [ts_])
        yield
        self.group_norm_T(B, yC, ts_, B["ynCt"], 4, ynC_dst, t_ynC)
        yield

    def group_norm_T(self, B, y, t_y, ynb, gidx, dstT, t_dst):
        P = self.P
        ts_ = t_y
        col = B["bst"][:, 10:11]
        self.act(B["junk"][:, 0:256], y[:], AF.Square, [t_y], [ts_], accum_out=col)
        self.rstd_from(col, col, 1.0 / 256.0, [ts_], ts_)
        self.ts("dve", ynb[:], y[:], col, None, ALU.mult, None, [ts_], [ts_])
        pT, tpT = self.ps[7], self.t_ps[7]
        pv = pT[:, :].bitcast(BF16)
        for c in range(2):
            self.tr(pv[:, c * 128:(c + 1) * 128], ynb[:, c * 128:(c + 1) * 128], [ts_], [tpT], signal=(c == 1))
        for c in range(2):
            self.act(dstT[:, c, :], pv[:, c * 128:(c + 1) * 128], AF.Copy, [tpT, B["t_par"]], [t_dst],
                     scale=B["ggT"][:, gidx + c:gidx + c + 1])

    def p2_alloc(self, es, l):
        P, I = self.P, self.I
        B = {}
        B["w_out"] = self.sb(es, "w_out_sb", [128, 8, D], BF16)
        B["t_w"] = Track()
        wsrc = I["w_out"][l].rearrange("(kc p) n -> p kc n", p=128)
        for cb in range(2):
            P.dma("pool", B["w_out"][:, :, cb * 512:(cb + 1) * 512], wsrc[:, :, cb * 512:(cb + 1) * 512], writes=[B["t_w"]])
        B["t_par"] = Track()
        tp = B["t_par"]
        for nm, key, shape in (("caw", "cawT", [128, 2, 3]), ("cbw", "cbwT", [128, 2, 31]), ("cbb", "cbbT", [128, 2]),
                               ("clg", "clgT", [128, 2]), ("clb", "clbT", [128, 2]), ("ggT", "ggT", [128, 8])):
            B[nm] = self.sb(es, "p2" + nm, shape, F32)
            P.dma("sp", B[nm][:], I[key][l], writes=[tp])
        for nm, shape in (("uAw", [128, 2, 514]), ("bgw", [128, 2, 512]), ("yBw", [128, 2, 542]), ("qw", [128, 2, 512]),
                          ("kw", [128, 2, 1280]), ("vw", [128, 10, 4, 66])):
            B[nm] = [self.sb(es, "p2%s%d" % (nm, i), shape, BF16) for i in range(2)]
            B["t_" + nm] = tracks(2)
        B["ynT"] = [self.sb(es, "p2ynT%d" % i, [128, 8, 512], BF16) for i in range(2)]
        B["t_ynT"] = tracks(2)
        B["dg"] = self.sb(es, "p2dg", [128, 2, 31, 128], BF16)
        for c in range(2):
            for jt in range(31):
                self.ts("dve", B["dg"][:, c, jt, :], self.ident[:], B["cbw"][:, c, jt:jt + 1], None, ALU.mult, None,
                        [self.t_const, tp], [tp])
        B["accA"] = self.sb(es, "p2accA", [128, 2, 512], F32)
        B["zf"] = self.sb(es, "p2zf", [128, 2, 512], F32)
        B["zb"] = self.sb(es, "p2zb", [128, 2, 512], BF16)
        B["zsq"] = self.sb(es, "p2zsq", [128, 2, 512], BF16)
        B["mean"] = self.sb(es, "p2mean", [128, 512], F32)
        B["var"] = self.sb(es, "p2var", [128, 512], F32)
        B["tmpn"] = self.sb(es, "p2tmpn", [128, 512], F32)
        B["t_cv"] = Track()
        B["PT"] = [self.sb(es, "p2PT%d" % i, [128, 512], BF16) for i in range(2)]
        B["t_PT"] = tracks(2)
        B["rec"] = self.sb(es, "p2rec", [128, 4], F32)
        B["yD"] = self.sb(es, "p2yD", [128, 256], F32)
        B["ynD"] = self.sb(es, "p2ynD", [128, 256], BF16)
        B["bst"] = self.sb(es, "p2bst", [128, 16], F32)
        B["junk"] = self.sb(es, "p2junk", [128, 256], BF16)
        B["t_at"] = Track()
        B["TG"] = self.sb(es, "p2TG", [128, 6, 512], BF16)
        B["TS"] = [self.sb(es, "p2TS%d" % i, [128, 6, 512], BF16) for i in range(2)]
        B["t_TG"], B["t_TS"] = Track(), tracks(2)
        B["tst"] = [self.sb(es, "p2tst%d" % i, [128, 512], F32) for i in range(2)]
        B["t_tst"] = tracks(2)
        B["cK"] = self.sb(es, "p2cK", [128, 2, 256], BF16)
        B["cV"] = self.sb(es, "p2cV", [128, 2, 4, 66], BF16)
        B["t_ckv"] = Track()
        B["xres"] = [self.sb(es, "p2xres%d" % i, [128, D], F32) for i in range(2)]
        B["t_xres"] = tracks(2)
        B["tmpo"] = [self.sb(es, "p2tmpo%d" % i, [128, D], F32) for i in range(2)]
        B["t_tmpo"] = tracks(2)
        B["n"] = 0
        B["nt"] = 0
        B["ntab"] = 0
        B["l"] = l
        return B

    def table_prep(self, B, slot, dst, t_dst):
        P, I = self.P, self.I
        for ch in range(6):
            i = B["ntab"] % 2
            B["ntab"] += 1
            st, t_st = B["tst"][i], B["t_tst"][i]
            P.dma("sp", st[:], I["btab"][B["l"], slot, ch].rearrange("p h q -> p (h q)"), writes=[t_st])
            self.act(dst[:, ch, :], st[:], AF.Exp, [t_st], [t_dst])

    def load_ctx_kv(self, B):
        P, seg = self.P, self.segC
        cs = seg.cols(0, 256)
        for c in range(2):
            P.dma("sp", B["cK"][:, c, :], seg.arr["k"][:, c, cs], reads=seg.tr("k", 0, 2), writes=[B["t_ckv"]])
        for t in range(2):
            P.dma("sp", B["cV"][:, t, :, :].rearrange("p h d -> p (h d)"), seg.arr["v"][t + seg.padf],
                  reads=seg.tr("v", t, t + 1), writes=[B["t_ckv"]])

    def attn_super(self, B, qw, t_qw, specs, t_ynT):
        O, tO = self.ps[6], self.t_ps[6]
        Ov = O[:, 0:260].rearrange("p (h d) -> p h d", h=4)
        flat = [(j, ci, len(chunks), ch, dst) for (j, chunks, dst) in specs for ci, ch in enumerate(chunks)]

        def s_mm(idx):
            j, ci, nch, (kap, vap, tab, rtr), dst = flat[idx]
            ba = 4 if idx % 2 == 0 else 2
            for h in range(4):
                c, hp = h // 2, (h % 2) * 64
                bk = ba + (h % 2)
                self.mm(self.ps[bk][:, c * 128:(c + 1) * 128], kap[hp:hp + 64, c, :], qw[hp:hp + 64, c, j * 128:(j + 1) * 128], True, True,
                        list(rtr) + [t_qw], [self.t_ps[bk]], signal=(h >= 2))

        for idx in range(min(2, len(flat))):
            s_mm(idx)
        for idx, (j, ci, nch, (kap, vap, tab, rtr), dst) in enumerate(flat):
            ba = 4 if idx % 2 == 0 else 2
            PT, tPT = B["PT"][idx % 2], B["t_PT"][idx % 2]
            src = self.psall[:, ba * 512:(ba + 2) * 512].rearrange("p (two r) -> p two r", two=2)[:, :, 0:256].rearrange("p two (c q) -> p two c q", c=2)
            dstv = PT[:].rearrange("p (c two q) -> p two c q", c=2, two=2)
            self.act(dstv, src, AF.Exp, [self.t_ps[ba], self.t_ps[ba + 1]], [tPT], scale=0.125)
            if idx + 2 < len(flat):
                s_mm(idx + 2)
            if tab is not None:
                self.tt("dve", PT[:], PT[:], tab[0], ALU.mult, [tPT, tab[1]], [tPT])
            for h in range(4):
                self.mm(Ov[:, h, :], PT[:, h * 128:(h + 1) * 128], vap[:, h, 0:65], (ci == 0 and h == 0), (ci == nch - 1),
                        list(rtr) + [tPT], [tO], signal=(h == 3), sgc=True)
            yield
            if ci == nch - 1:
                ta = B["t_at"]
                self.P.op("dve", lambda e: e.reciprocal(out=B["rec"][:], in_=Ov[:, :, 64]), [tO], [ta])
                for h in range(4):
                    self.ts("dve", B["yD"][:, h * 64:(h + 1) * 64], Ov[:, h, 0:64], B["rec"][:, h:h + 1], None, ALU.mult, None, [tO, ta], [ta])
                self.group_norm_T(B, B["yD"], ta, B["ynD"], 6, dst, t_ynT)
                yield

    def xpart_rms(self, B, y, gidx, ynT, t_ynT, N):
        tcv, tp = B["t_cv"], B["t_par"]
        sq = B["zsq"]
        for c in range(2):
            self.act(sq[:, c, 0:N], y[:, c, 0:N], AF.Square, [tcv], [tcv])
        pm, tpm = self.ps[1], self.t_ps[1]
        for c in range(2):
            self.mm(pm[:, 0:N], self.onesM[:], sq[:, c, 0:N], c == 0, c == 1, [tcv, self.t_const], [tpm])
        r = B["var"]
        self.rstd_from(r[:, 0:N], pm[:, 0:N], 1.0, [tpm, tcv], tcv)
        for c in range(2):
            self.stt(ynT[:, gidx + c, 0:N], y[:, c, 0:N], B["ggT"][:, gidx + c:gidx + c + 1], r[:, 0:N], ALU.mult, ALU.mult,
                     [tcv, tp], [t_ynT])

    def phase2(self, B, seg, tiles, is_ctx, xsrc, t_xsrc, xdst, t_xdst, gate):
        ga, gb = self.phase2_parts(B, seg, tiles, is_ctx, xsrc, t_xsrc, xdst, t_xdst, gate)
        for _ in ga:
            pass
        for _ in gb:
            pass

    def phase2_parts(self, B, seg, tiles, is_ctx, xsrc, t_xsrc, xdst, t_xdst, gate):
        sel = B["n"] % 2
        B["n"] += 1
        W = {nm: (B[nm][sel], B["t_" + nm][sel]) for nm in ("uAw", "bgw", "yBw", "qw", "kw", "vw", "ynT")}
        return (self.phase2_A(B, seg, tiles, is_ctx, W), self.phase2_B(B, seg, tiles, is_ctx, xsrc, t_xsrc, xdst, t_xdst, gate, W))

    def phase2_A(self, B, seg, tiles, is_ctx, W):
        P = self.P
        nt = len(tiles)
        N = nt * 128
        t0 = tiles[0]
        ynT, t_ynT = W["ynT"]
        a0 = (t0 + seg.padf) * 128
        wl = 1 if is_ctx else 3
        nwin = nt + 2 if is_ctx else nt + 6
        tcv, tp = B["t_cv"], B["t_par"]
        for c in range(2):
            P.dma("sp", W["uAw"][0][:, c, 0:N + 2], seg.arr["uA"][:, c, a0 - 1:a0 + N + 1], reads=seg.tr("uA", t0 - 1, t0 + nt + 1), writes=[W["uAw"][1]])
            P.dma("sp", W["yBw"][0][:, c, 0:N + 30], seg.arr["yB"][:, c, a0 - 15:a0 + N + 15], reads=seg.tr("yB", t0 - 1, t0 + nt + 1), writes=[W["yBw"][1]])
            P.dma("sp", W["bgw"][0][:, c, 0:N], seg.arr["bg"][:, c, a0:a0 + N], reads=seg.tr("bg", t0, t0 + nt), writes=[W["bgw"][1]])
            P.dma("sp", W["qw"][0][:, c, 0:N], seg.arr["q"][:, c, a0:a0 + N], reads=seg.tr("q", t0, t0 + nt), writes=[W["qw"][1]])
            P.dma("sp", ynT[:, 4 + c, 0:N], seg.arr["ynC"][:, c, a0:a0 + N], reads=seg.tr("ynC", t0, t0 + nt), writes=[t_ynT])
            if not is_ctx:
                P.dma("sp", W["kw"][0][:, c, 0:nwin * 128], seg.arr["k"][:, c, a0 - wl * 128:a0 + (nwin - wl) * 128],
                      reads=seg.tr("k", t0 - wl, t0 - wl + nwin), writes=[W["kw"][1]])
        if not is_ctx:
            for wi in range(nwin):
                tt_ = t0 - wl + wi
                P.dma("sp", W["vw"][0][:, wi, :, :].rearrange("p h d -> p (h d)"), seg.arr["v"][tt_ + seg.padf],
                      reads=seg.tr("v", tt_, tt_ + 1), writes=[W["vw"][1]])
        yield
        uAw, t_uAw = W["uAw"]
        acc = B["accA"]
        for c in range(2):
            self.ts("dve", acc[:, c, 0:N], uAw[:, c, 0:N], B["caw"][:, c, 0:1], None, ALU.mult, None, [t_uAw, tp], [tcv])
            for jt in (1, 2):
                self.stt(acc[:, c, 0:N], uAw[:, c, jt:jt + N], B["caw"][:, c, jt:jt + 1], acc[:, c, 0:N], ALU.mult, ALU.add, [t_uAw, tp, tcv], [tcv])
            yield
            self.tt("dve", acc[:, c, 0:N], acc[:, c, 0:N], W["bgw"][0][:, c, 0:N], ALU.mult, [tcv, W["bgw"][1]], [tcv])
            yield
        self.xpart_rms(B, acc, 0, ynT, t_ynT, N)
        yield
        yBw, t_yBw = W["yBw"]
        zf = B["zf"]
        for c in range(2):
            pcv, tpcv = self.ps[c], self.t_ps[c]
            for jt in range(31):
                self.mm(pcv[:, 0:N], B["dg"][:, c, jt, :], yBw[:, c, jt:jt + N], jt == 0, jt == 30, [t_yBw, tp], [tpcv])
                if jt % 4 == 3:
                    yield
            self.act(zf[:, c, 0:N], pcv[:, 0:N], AF.Identity, [tpcv, tp], [tcv], bias=B["cbb"][:, c:c + 1])
            yield
            self.copy("act", B["zb"][:, c, 0:N], zf[:, c, 0:N], [tcv], [tcv])
            self.act(B["zsq"][:, c, 0:N], zf[:, c, 0:N], AF.Square, [tcv], [tcv])
            yield
        pmean, tpmean = self.ps[0], self.t_ps[0]
        pmsq, tpmsq = self.ps[1], self.t_ps[1]
        for c in range(2):
            self.mm(pmean[:, 0:N], self.onesM[:], B["zb"][:, c, 0:N], c == 0, c == 1, [tcv, self.t_const], [tpmean])
        for c in range(2):
            self.mm(pmsq[:, 0:N], self.onesM[:], B["zsq"][:, c, 0:N], c == 0, c == 1, [tcv, self.t_const], [tpmsq])
        mean, var, tmpn = B["mean"], B["var"], B["tmpn"]
        yield
        self.copy("act", mean[:, 0:N], pmean[:, 0:N], [tpmean], [tcv])
        self.act(tmpn[:, 0:N], pmean[:, 0:N], AF.Square, [tpmean], [tcv])
        yield
        self.tt("dve", var[:, 0:N], pmsq[:, 0:N], tmpn[:, 0:N], ALU.subtract, [tpmsq, tcv], [tcv])
        self.ts("dve", var[:, 0:N], var[:, 0:N], 0.0, None, ALU.max, None, [tcv], [tcv])
        self.rstd_from(var[:, 0:N], var[:, 0:N], 1.0, [tcv], tcv)
        yield
        for c in range(2):
            self.tt("dve", tmpn[:, 0:N], zf[:, c, 0:N], mean[:, 0:N], ALU.subtract, [tcv], [tcv])
            self.tt("dve", tmpn[:, 0:N], tmpn[:, 0:N], var[:, 0:N], ALU.mult, [tcv], [tcv])
            self.act(zf[:, c, 0:N], tmpn[:, 0:N], AF.Silu, [tcv, tp], [tcv], bias=B["clb"][:, c:c + 1], scale=B["clg"][:, c:c + 1])
            yield
        self.xpart_rms(B, zf, 2, ynT, t_ynT, N)
        yield

    def phase2_B(self, B, seg, tiles, is_ctx, xsrc, t_xsrc, xdst, t_xdst, gate, W):
        P = self.P
        nt = len(tiles)
        N = nt * 128
        t0 = tiles[0]
        ynT, t_ynT = W["ynT"]
        wl = 1 if is_ctx else 3
        qw, t_qw = W["qw"]
        kw, t_kw = W["kw"]
        vw, t_vw = W["vw"]
        ctx_chunks = [(B["cK"][:, :, ci * 128:(ci + 1) * 128], B["cV"][:, ci, :, :], None, [B["t_ckv"]]) for ci in range(2)]
        specs = []
        tabs = {}
        for j, t in enumerate(tiles):
            chunks = []
            if not is_ctx:
                own = t - OWN_LO
                ws, slot = t - 2, 0
                if own in (0, 1, 30, 31):
                    slot = {0: 1, 1: 2, 30: 3, 31: 4}[own]
                    if own == 31:
                        ws = t - 3
                    tab, t_tab = B["TS"][j % 2], B["t_TS"][j % 2]
                    self.table_prep(B, slot, tab, t_tab)
                else:
                    tab, t_tab = B["TG"], B["t_TG"]
                for ch in range(6):
                    wi = ws + ch - (t0 - wl)
                    chunks.append((kw[:, :, wi * 128:(wi + 1) * 128], vw[:, wi, :, :], (tab[:, ch, :], t_tab), [t_kw, t_vw]))
            specs.append((j, chunks + ctx_chunks, ynT[:, 6:8, j * 128:(j + 1) * 128]))
        for _ in self.attn_super(B, qw, t_qw, specs, t_ynT):
            yield
        for j, t in enumerate(tiles):
            i = B["nt"] % 2
            B["nt"] += 1
            xr, t_xr = B["xres"][i], B["t_xres"][i]
            tm, t_tm = B["tmpo"][i], B["t_tmpo"][i]
            P.dma("sp", xr[:], xsrc[t * 128:(t + 1) * 128, :], reads=([t_xsrc[t]] if t_xsrc is not None else []), writes=[t_xr])
            for cb in range(2):
                po, tpo = self.ps[2 + cb], self.t_ps[2 + cb]
                for k in range(8):
                    self.mm(po[:, :], ynT[:, k, j * 128:(j + 1) * 128], B["w_out"][:, k, cb * 512:(cb + 1) * 512], k == 0, k == 7,
                            [t_ynT, B["t_w"]], [tpo])
                self.tt("dve", tm[:, cb * 512:(cb + 1) * 512], po[:, :], gate[0][:, cb * 512:(cb + 1) * 512], ALU.mult, [tpo, gate[1]], [t_tm])
            self.tt("pool", tm[:], tm[:], xr[:], ALU.add, [t_tm, t_xr], [t_tm])
            P.dma("pool", xdst[t * 128:(t + 1) * 128, :], tm[:], reads=[t_tm], writes=[t_xdst[t]])
            yield

    def ffn_phase(self, l, M, blocks, experts, moe):
        P, I = self.P, self.I
        assert not moe
        w1, w3, w2, nf = experts[0]
        with ExitStack() as es:
            B = {}
            B["xt"] = [self.sb(es, "p3xt%d" % i, [128, D], F32) for i in range(2)]
            B["t_xt"] = tracks(2)
            B["junk"] = self.sb(es, "p3junk", [128, D], BF16)
            B["xn"] = [self.sb(es, "p3xn%d" % i, [128, D], BF16) for i in range(2)]
            B["t_xn"] = tracks(2)
            B["st"] = self.sb(es, "p3st", [128, 16], F32)
            B["t_st"] = Track()
            B["nx"] = 0
            h2T = [self.sb(es, "p3h2T%d" % i, [128, 8, 1024], BF16) for i in range(2)]
            t_h2T = tracks(2)
            yacc = self.sb(es, "p3yacc", [128, 8, D], F32)
            t_yacc = tracks(8)
            NWB = 3
            w1b = [self.sb(es, "p3w1b%d" % i, [128, 8, 512], BF16) for i in range(NWB)]
            w3b = [self.sb(es, "p3w3b%d" % i, [128, 8, 512], BF16) for i in range(NWB)]
            w2b = [self.sb(es, "p3w2b%d" % i, [128, 4, D], BF16) for i in range(NWB)]
            t_w = tracks(NWB)
            gT = [self.sb(es, "p3gT%d" % i, [128, 4, 512], BF16) for i in range(2)]
            t_gT = tracks(2)
            sl = [self.sb(es, "p3sl%d" % i, [128, 512], BF16) for i in range(2)]
            t_sl = tracks(2)
            ro = [self.sb(es, "p3ro%d" % i, [128, D], F32) for i in range(2)]
            t_ro = tracks(2)
            units = []
            f0 = 0
            while f0 < nf:
                nfc = min(4, nf - f0)
                units.append((f0, nfc))
                f0 += nfc
            allu = [(bi, u) for bi in range(len(blocks)) for u in units]
            nw = [0]

            def load_unit(u):
                f0, nfc = u
                sel = nw[0] % NWB
                nw[0] += 1
                s1 = w1.rearrange("(kc p) f -> p kc f", p=128)[:, :, f0 * 128:(f0 + nfc) * 128]
                s3 = w3.rearrange("(kc p) f -> p kc f", p=128)[:, :, f0 * 128:(f0 + nfc) * 128]
                s2 = w2.rearrange("(fc p) d -> p fc d", p=128)[:, f0:f0 + nfc, :]
                P.dma("pool", w1b[sel][:, :, 0:nfc * 128], s1, writes=[t_w[sel]])
                P.dma("pool", w3b[sel][:, :, 0:nfc * 128], s3, writes=[t_w[sel]])
                P.dma("pool", w2b[sel][:, 0:nfc, :], s2, writes=[t_w[sel]])

            def gen_norm(bi):
                hb, t_hb = h2T[bi % 2], t_h2T[bi % 2]
                for j, td in enumerate(blocks[bi]):
                    i = B["nx"] % 2
                    xt, t_xt = B["xt"][i], B["t_xt"][i]
                    P.dma("sp", xt[:], td["src"], reads=[td["t_src"]], writes=[t_xt])
                    self.norm_to_hT(B, xt, t_xt, M["gs2" + td["mod"]], M["mod" + td["mod"]][:, 24:32], M["t"],
                                    hb[:, :, j * 128:(j + 1) * 128], t_hb, 7)
                    yield

            for u0 in range(NWB - 1):
                load_unit(allu[u0][1])
            for _ in gen_norm(0):
                pass
            ng = 0
            gu = 0
            for bi, blk in enumerate(blocks):
                nt = len(blk)
                hb, t_hb = h2T[bi % 2], t_h2T[bi % 2]
                nxt = gen_norm(bi + 1) if bi + 1 < len(blocks) else None
                for ui, u in enumerate(units):
                    cur = gu % NWB
                    if gu + NWB - 1 < len(allu):
                        load_unit(allu[gu + NWB - 1][1])
                    gu += 1
                    f0, nfc = u
                    first = (ui == 0)
                    for sb0 in range(0, nt, 4):
                        sbt = min(4, nt - sb0)
                        N = sbt * 128
                        g, t_g = gT[ng % 2], t_gT[ng % 2]
                        ng += 1
                        for fc in range(nfc):
                            p1, tp1 = self.ps[(fc % 2) * 2], self.t_ps[(fc % 2) * 2]
                            p3, tp3 = self.ps[(fc % 2) * 2 + 1], self.t_ps[(fc % 2) * 2 + 1]
                            for k in range(8):
                                self.mm(p1[:, 0:N], w1b[cur][:, k, fc * 128:(fc + 1) * 128], hb[:, k, sb0 * 128:sb0 * 128 + N], k == 0, k == 7,
                                        [t_w[cur], t_hb], [tp1])
                            for k in range(8):
                                self.mm(p3[:, 0:N], w3b[cur][:, k, fc * 128:(fc + 1) * 128], hb[:, k, sb0 * 128:sb0 * 128 + N], k == 0, k == 7,
                                        [t_w[cur], t_hb], [tp3])
                            s_, t_s = sl[fc % 2], t_sl[fc % 2]
                            self.act(s_[:, 0:N], p1[:, 0:N], AF.Silu, [tp1], [t_s])
                            self.tt("dve", g[:, fc, 0:N], p3[:, 0:N], s_[:, 0:N], ALU.mult, [tp3, t_s], [t_g])
                        for jj in range(sbt):
                            j = sb0 + jj
                            for cb in range(2):
                                py, tpy = self.ps[4 + (jj % 2) * 2 + cb], self.t_ps[4 + (jj % 2) * 2 + cb]
                                for fc in range(nfc):
                                    self.mm(py[:, :], g[:, fc, jj * 128:(jj + 1) * 128], w2b[cur][:, fc, cb * 512:(cb + 1) * 512], fc == 0, fc == nfc - 1,
                                            [t_g, t_w[cur]], [tpy])
                                ya = yacc[:, j, cb * 512:(cb + 1) * 512]
                                if first:
                                    self.copy("dve", ya, py[:, :], [tpy], [t_yacc[j]])
                                else:
                                    self.tt("dve", ya, py[:, :], ya, ALU.add, [tpy, t_yacc[j]], [t_yacc[j]])
                        if nxt is not None and ui >= 1:
                            for _ in range(1):
                                try:
                                    next(nxt)
                                except StopIteration:
                                    nxt = None
                                    break
                if nxt is not None:
                    for _ in nxt:
                        pass
                for j, td in enumerate(blk):
                    i = B["nx"] % 2
                    B["nx"] += 1
                    xt, t_xt = B["xt"][i], B["t_xt"][i]
                    P.dma("sp", xt[:], td["src"], reads=[td["t_src"]], writes=[t_xt])
                    g2 = M["g2" + td["mod"]]
                    r_, t_r = ro[j % 2], t_ro[j % 2]
                    self.tt("dve", r_[:], yacc[:, j, :], g2[:], ALU.mult, [t_yacc[j], M["t"]], [t_r])
                    self.tt("dve", r_[:], r_[:], xt[:], ALU.add, [t_r, t_xt], [t_r])
                    P.dma("pool", td["dst"], r_[:], reads=[t_r], writes=[td["t_dst"]])
            P.barrier()

    def pipeline(self, parts):
        if not parts:
            return
        ns = len(parts[0])
        n = len(parts)
        for step in range(n + ns - 1):
            live = []
            for k in range(ns):
                s_ = step - k
                if 0 <= s_ < n:
                    live.append(parts[s_][k])
            live.reverse()
            while live:
                for g in list(live):
                    try:
                        next(g)
                    except StopIteration:
                        live.remove(g)

    NBLK = 23

    def moe_sparse(self, M, tiles):
        P, I, nc = self.P, self.I, self.nc
        NB = self.NBLK
        IO = bass.IndirectOffsetOnAxis
        Xs, Ys, t_Xs = self.moe_Xs, self.moe_Ys, self.t_moe_Xs
        t_Ys = tracks(NB * 4)
        ntile = len(tiles)
        with ExitStack() as es0:
            slots_i = self.sb(es0, "ms_slots", [128, ntile, 2], I32)
            ghl = self.sb(es0, "ms_ghl", [128, ntile, 2], F32)
            idx_i = self.sb(es0, "ms_idx", [128, NB, 7], I32)
            idxX_i = self.sb(es0, "ms_idxX", [128, NB, 4], I32)
            fing = self.sb(es0, "ms_fing", [128, D], F32)
            t_rt = Track()
            t_par = Track()
            t_Xw = tracks(2 * ntile)
            P.dma("sp", fing[:], I["final_g"].partition_broadcast(128), writes=[t_par])
            NWB = 3
            w1b = [self.sb(es0, "mew1b%d" % i, [128, 8, 512], BF16) for i in range(NWB)]
            w3b = [self.sb(es0, "mew3b%d" % i, [128, 8, 512], BF16) for i in range(NWB)]
            w2b = [self.sb(es0, "mew2b%d" % i, [128, 4, D], BF16) for i in range(NWB)]
            t_w = tracks(NWB)
            units = [(b, fb) for b in range(NB) for fb in range(7)]
            nw = [0]

            def load_unit(u):
                b, fb = u
                sel = nw[0] % NWB
                nw[0] += 1
                off = IO(ap=idx_i[:, b, fb:fb + 1], axis=0)
                P.idma(w1b[sel][:].rearrange("p k f -> p (k f)"), None, I["moe_w1r"], off, reads=[t_rt], writes=[t_w[sel]])
                P.idma(w3b[sel][:].rearrange("p k f -> p (k f)"), None, I["moe_w3r"], off, reads=[t_rt], writes=[t_w[sel]])
                P.idma(w2b[sel][:].rearrange("p k f -> p (k f)"), None, I["moe_w2r"], off, reads=[t_rt], writes=[t_w[sel]])
                return sel
            with ExitStack() as es:
                B = {}
                B["xt"] = [self.sb(es, "msxt%d" % i, [128, D], F32) for i in range(2)]
                B["t_xt"] = tracks(2)
                B["junk"] = self.sb(es, "msjunk", [128, D], BF16)
                B["xn"] = [self.sb(es, "msxn%d" % i, [128, D], BF16) for i in range(2)]
                B["t_xn"] = tracks(2)
                B["st"] = self.sb(es, "msst", [128, 16], F32)
                B["t_st"] = Track()
                B["nx"] = 0
                h2tok = self.sb(es, "msh2tok", [128, ntile, D], BF16)
                t_h2tok = tracks(ntile)
                hTt = [self.sb(es, "mshTt%d" % i, [128, 8, 128], BF16) for i in range(2)]
                t_hTt = tracks(2)
                zer = self.sb(es, "mszer", [128, D], BF16)
                rw = self.sb(es, "msrw", [128, 8, 8], BF16)
                rbb = self.sb(es, "msrbb", [128, 8], F32)
                utri = self.sb(es, "msutri", [128, 128], BF16)
                onesb = self.sb(es, "msones", [128, 128], BF16)
                iota = self.sb(es, "msiota", [128, 1], F32)
                rs = self.sb(es, "msrs", [128, 8, 8], F32)
                t_rs = Track()
                maskb = self.sb(es, "msmaskb", [128, 8], BF16)
                mask_all = self.sb(es, "msmask", [128, ntile, 8], F32)
                gates_all = self.sb(es, "msgates", [128, ntile, 8], F32)
                rank_all = self.sb(es, "msrank", [128, ntile, 8], F32)
                carry = self.sb(es, "mscarry", [128, 8], F32)
                sc = self.sb(es, "mssc", [128, 16, 8], F32)
                sci = self.sb(es, "mssci", [128, 8], I32)
                be = self.sb(es, "msbe", [128, NB], F32)
                idx_f = self.sb(es, "msidxf", [128, NB, 7], F32)
                slots_f = self.sb(es, "msslotsf", [128, ntile, 2], F32)
                slotsR_i = self.sb(es, "msslotsRi", [128, ntile, 2], I32)
                eoff = self.sb(es, "mseoff", [128, NEXP], F32)
                bidx = self.sb(es, "msbidx", [128, NB], F32)
                sbk = self.sb(es, "mssbk", [128, NB], F32)
                idxX_f = self.sb(es, "msidxXf", [128, NB, 4], F32)
                sr = self.sb(es, "mssr", [128, 4, 8], F32)
                t_sr = Track()
                P.dma("pool", rw[:], I["router_wT"], writes=[t_par])
                P.dma("pool", utri[:], I["utri"], writes=[t_par])
                P.dma("sp", rbb[:], I["router_b"].partition_broadcast(128), writes=[t_par])
                P.dma("sp", iota[:], I["iota_p"], writes=[t_par])
                P.dma("sp", eoff[:], I["eoff"], writes=[t_par])
                P.dma("sp", bidx[:], I["bidx"], writes=[t_par])
                P.op("dve", lambda e: e.memset(onesb[:], 1.0), [], [t_par])
                P.op("dve", lambda e: e.memset(zer[:], 0.0), [], [t_par])
                P.op("dve", lambda e: e.memset(carry[:], 0.0), [], [t_rt])
                def stage_a(j, td):
                    hT, t_hT = hTt[j % 2], t_hTt[j % 2]
                    i = B["nx"] % 2
                    xt, t_xt = B["xt"][i], B["t_xt"][i]
                    P.dma("sp", xt[:], td["src"], reads=[td["t_src"]], writes=[t_xt])
                    self.norm_to_hT(B, xt, t_xt, M["gs2L"], M["modL"][:, 24:32], M["t"], hT[:, :, :], t_hT, 7)
                    yield

                def stage_b(j, td):
                    hT, t_hT = hTt[j % 2], t_hTt[j % 2]
                    pl, tpl = self.ps[6], self.t_ps[6]
                    for k in range(8):
                        self.mm(pl[:, 0:8], hT[:, k, :], rw[:, k, :], k == 0, k == 7, [t_hT, t_par], [tpl])
                    lg, top, ex, msk = rs[:, 0, :], rs[:, 1, :], rs[:, 2, :], mask_all[:, j, :]
                    self.tt("dve", lg, pl[:, 0:8], rbb[:], ALU.add, [tpl, t_par], [t_rs])
                    P.op("dve", lambda e_, top=top, lg=lg: e_.max(out=top, in_=lg), [t_rs], [t_rs])
                    self.ts("dve", rs[:, 3, 0:1], top[:, 0:1], -1.0, None, ALU.mult, None, [t_rs], [t_rs])
                    self.act(ex, lg, AF.Exp, [t_rs], [t_rs], bias=rs[:, 3, 0:1])
                    self.ts("dve", msk, lg, top[:, 1:2], None, ALU.is_ge, None, [t_rs], [t_rt])
                    self.tt("dve", ex, ex, msk, ALU.mult, [t_rs, t_rt], [t_rs])
                    P.op("dve", lambda e_, ex=ex: e_.reduce_sum(out=rs[:, 3, 1:2], in_=ex, axis=mybir.AxisListType.X), [t_rs], [t_rs])
                    P.op("dve", lambda e_: e_.reciprocal(out=rs[:, 3, 1:2], in_=rs[:, 3, 1:2]), [t_rs], [t_rs])
                    self.ts("dve", gates_all[:, j, :], ex, rs[:, 3, 1:2], None, ALU.mult, None, [t_rs], [t_rt])
                    yield
                    self.copy("dve", maskb[:], msk, [t_rt], [t_rs])
                    pr, tpr = self.ps[5], self.t_ps[5]
                    self.mm(pr[:, 0:8], utri[:], maskb[:], True, True, [t_rs, t_par], [tpr], signal=False)
                    self.mm(pr[:, 8:16], onesb[:], maskb[:], True, True, [t_rs, t_par], [tpr])
                    self.tt("dve", rank_all[:, j, :], pr[:, 0:8], carry[:], ALU.add, [tpr, t_rt], [t_rt])
                    self.tt("dve", carry[:], pr[:, 8:16], carry[:], ALU.add, [tpr, t_rt], [t_rt])
                    kr, m8r = sr[:, 0, :], sr[:, 1, :]
                    self.tt("dve", kr, rank_all[:, j, :], eoff[:], ALU.add, [t_rt, t_par], [t_sr])
                    self.stt(kr, kr, 1.0, msk, ALU.add, ALU.mult, [t_sr, t_rt], [t_sr])
                    P.op("dve", lambda e_, m8r=m8r, kr=kr: e_.max(out=m8r, in_=kr), [t_sr], [t_sr])
                    self.ts("dve", sr[:, 2, 0:2], m8r[:, 0:2], -1.0, None, ALU.add, None, [t_sr], [t_sr])
                    t_sx = Track()
                    self.copy("dve", slotsR_i[:, j, :], sr[:, 2, 0:2], [t_sr], [t_sx])
                    t_scat.append(t_sx)
                    yield
                    pT, tpT = self.ps[4], self.t_ps[4]
                    pv = pT[:, :].bitcast(BF16)
                    for k in range(8):
                        self.tr(pv[:, k * 128:(k + 1) * 128], hT[:, k, :], [t_hT], [tpT], signal=(k == 7))
                    self.copy("act", h2tok[:, j, :], pv[:, :], [tpT], [t_h2tok[j]])
                    for kk in range(2):
                        P.idma(Xs, IO(ap=slotsR_i[:, j, kk:kk + 1], axis=0), h2tok[:, j, :], None,
                               reads=[t_scat[j], t_h2tok[j]], writes=[t_Xw[j * 2 + kk]])
                    yield

                t_scat = []
                self.pipeline([(stage_a(j, td), stage_b(j, td)) for j, td in enumerate(tiles)])
                nbk, pend, pst = sc[:, 0, :], sc[:, 1, :], sc[:, 2, :]
                vq = sc[:, 7, :]
                self.ts("dve", vq, carry[:], 511.0, 1.0 / 512.0, ALU.add, ALU.mult, [t_rt], [t_rt])
                self.copy("dve", sci[:], vq, [t_rt], [t_rt])
                self.copy("dve", nbk, sci[:], [t_rt], [t_rt])
                self.tt("dve", sc[:, 8, :], nbk, vq, ALU.is_gt, [t_rt], [t_rt])
                self.tt("dve", nbk, nbk, sc[:, 8, :], ALU.subtract, [t_rt], [t_rt])
                self.copy("dve", pend[:, 0:1], nbk[:, 0:1], [t_rt], [t_rt])
                for e in range(1, NEXP):
                    self.tt("dve", pend[:, e:e + 1], pend[:, e - 1:e], nbk[:, e:e + 1], ALU.add, [t_rt], [t_rt])
                self.tt("dve", pst, pend, nbk, ALU.subtract, [t_rt], [t_rt])
                self.ts("dve", pst, pst, 512.0, None, ALU.mult, None, [t_rt], [t_rt])
                for b in range(NB):
                    self.ts("dve", sc[:, 3, :], pend, float(b), None, ALU.is_le, None, [t_rt], [t_rt])
                    P.op("dve", lambda e_, b=b: e_.reduce_sum(out=be[:, b:b + 1], in_=sc[:, 3, :], axis=mybir.AxisListType.X), [t_rt], [t_rt])
                    self.tt("dve", sc[:, 9, :], sc[:, 3, :], nbk, ALU.mult, [t_rt], [t_rt])
                    P.op("dve", lambda e_, b=b: e_.reduce_sum(out=sbk[:, b:b + 1], in_=sc[:, 9, :], axis=mybir.AxisListType.X), [t_rt], [t_rt])
                self.ts("dve", be[:], be[:], 7.0, None, ALU.min, None, [t_rt], [t_rt])
                self.tt("dve", sbk[:], bidx[:], sbk[:], ALU.subtract, [t_rt, t_par], [t_rt])
                self.ts("dve", sbk[:], sbk[:], 7.0, 0.0, ALU.min, ALU.max, [t_rt], [t_rt])
                self.ts("dve", sbk[:], sbk[:], 512.0, iota[:, 0:1], ALU.mult, ALU.add, [t_rt, t_par], [t_rt])
                self.stt(sbk[:], be[:], 4096.0, sbk[:], ALU.mult, ALU.add, [t_rt], [t_rt])
                for jj in range(4):
                    self.ts("dve", idxX_f[:, :, jj], sbk[:], float(jj * 128), None, ALU.add, None, [t_rt], [t_rt])
                self.copy("dve", idxX_i[:], idxX_f[:], [t_rt], [t_rt])
                self.ts("dve", be[:], be[:], 896.0, None, ALU.mult, None, [t_rt], [t_rt])
                self.ts("dve", be[:], be[:], iota[:, 0:1], None, ALU.add, None, [t_rt, t_par], [t_rt])
                for fb in range(7):
                    self.ts("dve", idx_f[:, :, fb], be[:], float(fb * 128), None, ALU.add, None, [t_rt], [t_rt])
                self.copy("dve", idx_i[:], idx_f[:], [t_rt], [t_rt])
                for u0 in range(NWB - 1):
                    load_unit(units[u0])
                t_slots = []
                for j in range(ntile):
                    key, m8, oh = sc[:, 4, :], sc[:, 5, :], sc[:, 6, :]
                    self.tt("dve", key, rank_all[:, j, :], pst, ALU.add, [t_rt], [t_rt])
                    self.stt(key, key, 1.0, mask_all[:, j, :], ALU.add, ALU.mult, [t_rt], [t_rt])
                    P.op("dve", lambda e_, m8=m8, key=key: e_.max(out=m8, in_=key), [t_rt], [t_rt])
                    self.ts("dve", slots_f[:, j, :], m8[:, 0:2], -1.0, None, ALU.add, None, [t_rt], [t_rt])
                    self.ts("dve", oh, key, m8[:, 0:1], None, ALU.is_equal, None, [t_rt], [t_rt])
                    self.tt("dve", oh, oh, gates_all[:, j, :], ALU.mult, [t_rt], [t_rt])
                    P.op("dve", lambda e_, j=j, oh=oh: e_.reduce_sum(out=ghl[:, j, 0:1], in_=oh, axis=mybir.AxisListType.X), [t_rt], [t_rt])
                    self.ts("dve", ghl[:, j, 1:2], ghl[:, j, 0:1], -1.0, 1.0, ALU.mult, ALU.add, [t_rt], [t_rt])
                    t_sl = Track()
                    self.copy("dve", slots_i[:, j, :], slots_f[:, j, :], [t_rt], [t_sl])
                    t_slots.append(t_sl)
                P.barrier()
            with ExitStack() as es:
                xtok = [self.sb(es, "mextok%d" % i, [128, D], BF16) for i in range(2)]
                t_xtok = tracks(2)
                XT = [self.sb(es, "meXT%d" % i, [128, 8, 512], BF16) for i in range(2)]
                t_XT = tracks(2)
                yblk = [self.sb(es, "meyb%d" % i, [128, 4, D], F32) for i in range(2)]
                t_yblk = [tracks(4) for _ in range(2)]
                gT = [self.sb(es, "megT%d" % i, [128, 4, 512], BF16) for i in range(2)]
                t_gT = tracks(2)
                sl = [self.sb(es, "mesl%d" % i, [128, 512], BF16) for i in range(2)]
                t_sl = tracks(2)
                nxt = [0]

                def load_x(b):
                    xs_, t_xs = XT[b % 2], t_XT[b % 2]
                    for jj in range(4):
                        i = nxt[0] % 2
                        nxt[0] += 1
                        r = (b * 4 + jj) * 128
                        P.idma(xtok[i][:], None, Xs, IO(ap=idxX_i[:, b, jj:jj + 1], axis=0), reads=[t_rt] + t_Xw, writes=[t_xtok[i]])
                        pT, tpT = self.ps[3], self.t_ps[3]
                        pv = pT[:, :].bitcast(BF16)
                        for k in range(8):
                            self.tr(pv[:, k * 128:(k + 1) * 128], xtok[i][:, k * 128:(k + 1) * 128], [t_xtok[i]], [tpT], signal=(k == 7))
                        self.copy("act", xs_[:, :, jj * 128:(jj + 1) * 128], pv[:, :].rearrange("p (k t) -> p k t", k=8), [tpT], [t_xs])

                load_x(0)
                ng = 0
                for ui, (b, fb) in enumerate(units):
                    cur = ui % NWB
                    if ui + NWB - 1 < len(units):
                        load_unit(units[ui + NWB - 1])
                    if fb == 3 and b + 1 < NB:
                        load_x(b + 1)
                    xs_, t_xs = XT[b % 2], t_XT[b % 2]
                    yb, t_yb = yblk[b % 2], t_yblk[b % 2]
                    g, t_g = gT[ng % 2], t_gT[ng % 2]
                    ng += 1
                    for fc in range(4):
                        p1, tp1 = self.ps[(fc % 2) * 2], self.t_ps[(fc % 2) * 2]
                        p3, tp3 = self.ps[(fc % 2) * 2 + 1], self.t_ps[(fc % 2) * 2 + 1]
                        for k in range(8):
                            self.mm(p1[:, :], w1b[cur][:, k, fc * 128:(fc + 1) * 128], xs_[:, k, :], k == 0, k == 7, [t_w[cur], t_xs], [tp1])
                        for k in range(8):
                            self.mm(p3[:, :], w3b[cur][:, k, fc * 128:(fc + 1) * 128], xs_[:, k, :], k == 0, k == 7, [t_w[cur], t_xs], [tp3])
                        s_, t_s = sl[fc % 2], t_sl[fc % 2]
                        self.act(s_[:, :], p1[:, :], AF.Silu, [tp1], [t_s])
                        self.tt("dve", g[:, fc, :], p3[:, :], s_[:, :], ALU.mult, [tp3, t_s], [t_g])
                    for jj in range(4):
                        for cb in range(2):
                            py, tpy = self.ps[4 + (jj % 2) * 2 + cb], self.t_ps[4 + (jj % 2) * 2 + cb]
                            for fc in range(4):
                                self.mm(py[:, :], g[:, fc, jj * 128:(jj + 1) * 128], w2b[cur][:, fc, cb * 512:(cb + 1) * 512], fc == 0, fc == 3,
                                        [t_g, t_w[cur]], [tpy])
                            ya = yb[:, jj, cb * 512:(cb + 1) * 512]
                            if fb == 0:
                                self.copy("dve", ya, py[:, :], [tpy], [t_yb[jj]])
                            else:
                                self.tt("dve", ya, py[:, :], ya, ALU.add, [tpy, t_yb[jj]], [t_yb[jj]])
                        if fb == 6:
                            r = (b * 4 + jj) * 128
                            P.dma("sp", Ys[r:r + 128, :], yb[:, jj, :], reads=[t_yb[jj]], writes=[t_Ys[b * 4 + jj]])
                P.barrier()
            with ExitStack() as es:
                yh = [self.sb(es, "mcyh%d" % i, [128, D], F32) for i in range(2)]
                yl = [self.sb(es, "mcyl%d" % i, [128, D], F32) for i in range(2)]
                xr = [self.sb(es, "mcxr%d" % i, [128, D], F32) for i in range(2)]
                t_yh, t_yl, t_xr = tracks(2), tracks(2), tracks(2)
                junk = self.sb(es, "mcjunk", [128, D], BF16)
                st = self.sb(es, "mcst", [128, 4], F32)
                t_st = Track()
                g2 = M["g2L"]
                for j, td in enumerate(tiles):
                    i = j % 2
                    P.idma(yh[i][:], None, Ys, IO(ap=slots_i[:, j, 0:1], axis=0), reads=[t_rt, t_slots[j]] + t_Ys, writes=[t_yh[i]])
                    P.idma(yl[i][:], None, Ys, IO(ap=slots_i[:, j, 1:2], axis=0), reads=[t_rt, t_slots[j]] + t_Ys, writes=[t_yl[i]])
                    P.dma("sp", xr[i][:], td["src"], reads=[td["t_src"]], writes=[t_xr[i]])
                    self.ts("dve", yh[i][:], yh[i][:], ghl[:, j, 0:1], None, ALU.mult, None, [t_yh[i], t_rt], [t_yh[i]])
                    self.stt(yh[i][:], yl[i][:], ghl[:, j, 1:2], yh[i][:], ALU.mult, ALU.add, [t_yl[i], t_yh[i], t_rt], [t_yh[i]])
                    self.tt("dve", yh[i][:], yh[i][:], g2[:], ALU.mult, [t_yh[i], M["t"]], [t_yh[i]])
                    self.tt("dve", yh[i][:], yh[i][:], xr[i][:], ALU.add, [t_yh[i], t_xr[i]], [t_yh[i]])
                    col = st[:, i:i + 1]
                    self.act(junk[:], yh[i][:], AF.Square, [t_yh[i]], [t_st], accum_out=col)
                    self.rstd_from(col, col, 1.0 / D, [t_st], t_st)
                    self.stt(yh[i][:], yh[i][:], col, fing[:], ALU.mult, ALU.mult, [t_yh[i], t_st, t_par], [t_yh[i]])
                    P.dma("sp", td["dst"], yh[i][:], reads=[t_yh[i]], writes=[td["t_dst"]])
                P.barrier()

    def build(self):
        nc = self.nc
        with ExitStack() as es:
            self.setup(es)
            P, I = self.P, self.I
            segL, segC = self.segL, self.segC
            groups = [[t for t in range(4 * s, 4 * s + 4)] for s in range(10)]
            for l in range(2):
                with ExitStack() as esl:
                    M = self.mod_phase(l, esl)
                    if STOP_AFTER == "mod":
                        break
                    last = (l == 1)
                    xsrc, t_xsrc = (I["x_ext"], None) if l == 0 else (self.xl1, self.t_xl1)
                    csrc, t_csrc = (I["ctxb"], None) if l == 0 else (self.xc1, self.t_xc1)
                    lo, hi = (0, NT0) if l == 0 else (EXT_LO, EXT_HI)
                    with ExitStack() as es1:
                        B = self.p1_alloc(es1, l)
                        self.phase1(B, segC, [0, 1], csrc, t_csrc, M["gs1C"], M["modC"][:, 0:8], M["t"], self.one1[:, 0:1], kv_only=last)
                        parts = []
                        for s, g in enumerate(groups):
                            tl = [t for t in g if lo <= t < hi]
                            if tl:
                                parts.append(self.phase1_parts(B, segL, tl, xsrc, t_xsrc, M["gs1L"], M["modL"][:, 0:8], M["t"], self.valid[:, s:s + 1]))
                        self.pipeline(parts)
                        P.barrier()
                    if STOP_AFTER == "l%dp1" % l:
                        break
                    lo2, hi2 = (EXT_LO, EXT_HI) if l == 0 else (OWN_LO, OWN_HI)
                    with ExitStack() as es2:
                        B = self.p2_alloc(es2, l)
                        self.table_prep(B, 0, B["TG"], B["t_TG"])
                        self.load_ctx_kv(B)
                        if not last:
                            self.phase2(B, segC, [0, 1], True, csrc, t_csrc, self.xcmid, self.t_xcmid, (M["g1C"], M["t"]))
                        parts = []
                        for g in groups:
                            tl = [t for t in g if lo2 <= t < hi2]
                            if tl:
                                parts.append(self.phase2_parts(B, segL, tl, False, xsrc, t_xsrc, self.xmid, self.t_xmid, (M["g1L"], M["t"])))
                        self.pipeline(parts)
                        P.barrier()
                    if STOP_AFTER == "l%dp2" % l:
                        break
                    tiles = []
                    for t in range(lo2, hi2):
                        if last:
                            dst, t_dst = self.out[(t - OWN_LO) * 128:(t - OWN_LO + 1) * 128, :], self.t_out[t - OWN_LO]
                        else:
                            dst, t_dst = self.xl1[t * 128:(t + 1) * 128, :], self.t_xl1[t]
                        tiles.append({"src": self.xmid[t * 128:(t + 1) * 128, :], "t_src": self.t_xmid[t], "dst": dst, "t_dst": t_dst,
                                      "mod": "L", "final": last})
                    if not last:
                        for t in range(2):
                            tiles.append({"src": self.xcmid[t * 128:(t + 1) * 128, :], "t_src": self.t_xcmid[t],
                                          "dst": self.xc1[t * 128:(t + 1) * 128, :], "t_dst": self.t_xc1[t], "mod": "C", "final": False})
                    blocks = [tiles[i:i + 8] for i in range(0, len(tiles), 8)]
                    if not last:
                        experts = [(I["ffn_w1"], I["ffn_w3"], I["ffn_w2"], DFF // 128)]
                        self.ffn_phase(l, M, blocks, experts, False)
                    else:
                        self.moe_sparse(M, tiles)
                    if STOP_AFTER == "l%d" % l:
                        break
            P.barrier()
        return nc


def build_program():
    nc = bass.Bass("TRN2", target_bir_lowering=False)
    kb = KB(nc)
    kb.t_out = tracks(32)
    kb.build()
    return nc, kb


def kernel(**inputs):
    maps = _prep_inputs(inputs)
    ident = np.eye(128, dtype=np.float32)
    for m in maps:
        m["ident"] = ident
        if STOP_AFTER is not None:
            for k in ("moe_w1r", "moe_w3r", "moe_w2r"):
                m[k] = np.zeros((128, 128), np.float32)
    nc, kb = build_program()
    res = run_bass_kernel_spmd(nc, maps, core_ids=list(range(NCORES)))
    out = np.zeros((2, 16384, D), np.float32)
    for core in range(NCORES):
        b, R0 = core // 4, 64 * (core % 4)
        out[b, R0 * 64:(R0 + 64) * 64] = res.results[core]["out"]
    kernel.last_results = res.results
    return out
```

```python
import numpy as np
from contextlib import ExitStack
import concourse.bass as bass
import concourse.mybir as mybir
from concourse.bass_utils import run_bass_kernel_spmd

F32 = mybir.dt.float32
BF16 = mybir.dt.bfloat16
I32 = mybir.dt.int32
AF = mybir.ActivationFunctionType
ALU = mybir.AluOpType

NCORES = 8
D = 1024
NT0 = 40
PADF, PADB = 1, 3
TT = PADF + NT0 + PADB
EXT_LO, EXT_HI = 2, 38
OWN_LO, OWN_HI = 4, 36
EPS = 1e-6
DFF = 2816
DFE = 3584
NEXP = 8
NEG = -1.0e4
DEBUG_OUT = False
DBG_P2 = 9
STOP_AFTER = None


class Track:
    __slots__ = ("w", "r")

    def __init__(self):
        self.w = None
        self.r = []


def tracks(n):
    return [Track() for _ in range(n)]


class Prog:
    def __init__(self, nc, es):
        self.nc = nc
        self.E = {"pe": nc.tensor, "act": nc.scalar, "dve": nc.vector, "pool": nc.gpsimd, "sp": nc.sync}
        self.sem = {}
        self.cnt = {}
        for e in ("pe", "act", "dve", "pool"):
            self.sem[e] = es.enter_context(nc.semaphore("s_" + e))
            self.cnt[e] = 0
        self.dq = {}
        for q, k in (("sp", 16), ("pool", 12), ("act", 6)):
            sems = [es.enter_context(nc.semaphore("d_%s%d" % (q, i))) for i in range(k)]
            self.dq[q] = {"sems": sems, "n": 0}
            for i, s in enumerate(sems):
                self.sem[("d", q, i)] = s
        self.waited = {e: {} for e in self.E}
        self.pending = {e: [] for e in self.E}
        self.ninst = 0

    def _deps(self, reads, writes):
        evs = []
        for t in reads:
            if t.w is not None:
                evs.append(t.w)
        for t in writes:
            if t.w is not None:
                evs.append(t.w)
            evs.extend(t.r)
        return evs

    def _wait(self, eng, evs):
        need = {}
        for (k, v) in evs:
            if k == eng and eng == "pe":
                continue
            if v > need.get(k, 0):
                need[k] = v
        w = self.waited[eng]
        for k, v in need.items():
            if w.get(k, 0) >= v:
                continue
            w[k] = v
            self.E[eng].wait_ge(self.sem[k], v)

    def op(self, eng, fn, reads=(), writes=(), signal=True):
        self._wait(eng, self._deps(reads, writes))
        inst = fn(self.E[eng])
        self.ninst += 1
        if signal:
            self.cnt[eng] += 1
            inst.then_inc(self.sem[eng], 1)
            ev = (eng, self.cnt[eng])
            for (rs, ws) in self.pending[eng] + [(reads, writes)]:
                for t in rs:
                    t.r.append(ev)
                for t in ws:
                    t.w = ev
                    t.r = []
            self.pending[eng] = []
        else:
            self.pending[eng].append((tuple(reads), tuple(writes)))
        return inst

    def dma(self, q, out, in_, reads=(), writes=()):
        pool = self.dq[q]
        i = pool["n"]
        k = len(pool["sems"])
        idx, gen = i % k, i // k
        key = ("d", q, idx)
        evs = self._deps(reads, writes)
        if gen > 0:
            evs.append((key, 16 * gen))
        self._wait(q, evs)
        inst = self.E[q].dma_start(out=out, in_=in_)
        inst.then_inc(pool["sems"][idx], 16)
        pool["n"] += 1
        self.ninst += 1
        ev = (key, 16 * (gen + 1))
        for t in reads:
            t.r.append(ev)
        for t in writes:
            t.w = ev
            t.r = []

    def idma(self, out, out_offset, in_, in_offset, reads=(), writes=()):
        q = "pool"
        pool = self.dq[q]
        i = pool["n"]
        k = len(pool["sems"])
        idx, gen = i % k, i // k
        key = ("d", q, idx)
        evs = self._deps(reads, writes)
        if gen > 0:
            evs.append((key, 16 * gen))
        self._wait(q, evs)
        inst = self.E[q].indirect_dma_start(out=out, out_offset=out_offset, in_=in_, in_offset=in_offset)
        inst.then_inc(pool["sems"][idx], 16)
        pool["n"] += 1
        self.ninst += 1
        ev = (key, 16 * (gen + 1))
        for t in reads:
            t.r.append(ev)
        for t in writes:
            t.w = ev
            t.r = []

    def barrier(self):
        for e in self.pending:
            assert not self.pending[e], "pending unsignaled instructions at barrier"
        evs = [(e, self.cnt[e]) for e in ("pe", "act", "dve", "pool") if self.cnt[e] > 0]
        for q, pool in self.dq.items():
            k = len(pool["sems"])
            for idx in range(k):
                n = (pool["n"] - idx + k - 1) // k
                if n > 0:
                    evs.append((("d", q, idx), 16 * n))
        for eng in self.E:
            self._wait(eng, [ev for ev in evs if ev[0] != eng])


def _pcol(v, nch):
    return np.ascontiguousarray(np.asarray(v, np.float32).reshape(nch, 128).T)


def _bias_tables(rpb_l, R0):
    out = np.full((5, 6, 128, 4, 128), NEG, np.float32)
    specs = [(100, 96)] + [(R0 + 0, R0 - 4), (R0 + 2, R0 - 2), (R0 + 60, R0 + 56), (R0 + 62, R0 + 56)]
    kk = np.arange(768)
    jr, kc = kk // 64, kk % 64
    qq = np.arange(128)
    a, qc = qq // 64, qq % 64
    c_start = np.clip(qc - 8, 0, 48)
    col_ok = (kc[:, None] >= c_start[None, :]) & (kc[:, None] < c_start[None, :] + 16)
    dc = np.clip(kc[:, None] - qc[None, :], -15, 15) + 15
    for s, (r, ws) in enumerate(specs):
        rk = ws + jr
        rq = r + a
        lo = np.clip(rq - 4, 0, 248)
        ok = (rk[:, None] >= lo[None, :]) & (rk[:, None] < lo[None, :] + 8) & (rk[:, None] >= 0) & (rk[:, None] < 256)
        ok = ok & col_ok
        dr = np.clip(rk[:, None] - rq[None, :] + 7, 0, 14)
        for h in range(4):
            b = rpb_l[h][dr, dc]
            t = np.where(ok, b, np.float32(NEG)).astype(np.float32)
            out[s, :, :, h, :] = t.reshape(6, 128, 128)
    return out


def _prep_inputs(inp):
    f = lambda k: np.asarray(inp[k], np.float32)
    x, c, ctx, c_ctx = f("x"), f("c"), f("ctx"), f("c_ctx")
    shared = {}
    shared["w_mod"] = f("w_mod")
    shared["b_mod"] = f("b_mod")
    shared["b_modT"] = np.stack([_pcol(f("b_mod")[l], 48) for l in range(2)])
    shared["n1gT"] = np.stack([_pcol(f("norm1_g")[l], 8) for l in range(2)])
    shared["n2gT"] = np.stack([_pcol(f("norm2_g")[l], 8) for l in range(2)])
    shared["w_in"] = f("w_in")
    caw = f("conv_a_w")
    shared["cawT"] = np.ascontiguousarray(caw.reshape(2, 3, 2, 128).transpose(0, 3, 2, 1))
    cbw = f("conv_b_w")
    shared["cbwT"] = np.ascontiguousarray(cbw.reshape(2, 31, 2, 128).transpose(0, 3, 2, 1))
    for nm, key in (("cbbT", "conv_b_b"), ("clgT", "conv_ln_g"), ("clbT", "conv_ln_b")):
        shared[nm] = np.stack([_pcol(f(key)[l], 2) for l in range(2)])
    shared["sgu_ln_g"] = f("sgu_ln_g")
    shared["sgu_ln_b"] = f("sgu_ln_b")
    shared["sgu_wT"] = np.ascontiguousarray(f("sgu_w").transpose(0, 3, 1, 2))
    shared["sgu_bT"] = np.ascontiguousarray(f("sgu_b").transpose(0, 2, 1))
    shared["ggT"] = np.stack([_pcol(f("group_g")[l], 8) for l in range(2)])
    shared["w_out"] = f("w_out")
    shared["ffn_w1"] = f("ffn_w1")[0]
    shared["ffn_w3"] = f("ffn_w3")[0]
    shared["ffn_w2"] = f("ffn_w2")[0]
    shared["router_wT"] = np.ascontiguousarray(f("router_w")[0].reshape(8, 128, 8).transpose(1, 0, 2))
    shared["router_b"] = f("router_b")[0]
    wr = np.empty((NEXP * 7 * 128, 3 * 4096), np.float32)
    for i, nm in enumerate(("moe_w1", "moe_w3")):
        w = f(nm)[0].reshape(NEXP, 8, 128, 7, 512)
        wr[:, i * 4096:(i + 1) * 4096] = w.transpose(0, 3, 2, 1, 4).reshape(NEXP * 7 * 128, 4096)
    w = f("moe_w2")[0].reshape(NEXP, 7, 4, 128, D)
    wr[:, 8192:12288] = w.transpose(0, 1, 3, 2, 4).reshape(NEXP * 7 * 128, 4096)
    shared["moe_wr"] = wr
    shared["iota_p"] = np.arange(128, dtype=np.float32).reshape(128, 1)
    shared["eoff"] = np.tile((np.arange(NEXP, dtype=np.float32) * 4096.0)[None, :], (128, 1))
    shared["bidx"] = np.tile(np.arange(23, dtype=np.float32)[None, :], (128, 1))
    shared["utri"] = np.triu(np.ones((128, 128), np.float32), 1)
    shared["final_g"] = f("final_g")
    rpb = f("rpb")
    maps = []
    for core in range(NCORES):
        b, R0 = core // 4, 64 * (core % 4)
        m = dict(shared)
        xe = np.zeros((NT0 * 128, D), np.float32)
        lo, hi = R0 - 8, R0 + 72
        glo, ghi = max(lo, 0), min(hi, 256)
        xe[(glo - lo) * 64:(ghi - lo) * 64] = x[b, glo * 64:ghi * 64]
        m["x_ext"] = xe
        m["ctxb"] = np.ascontiguousarray(ctx[b])
        m["cT"] = np.ascontiguousarray(np.stack([_pcol(c[b], 8), _pcol(c_ctx, 8)], axis=-1))
        val = np.zeros((128, 10), np.float32)
        for s in range(10):
            r = lo + 8 * s
            val[:, s] = 1.0 if (0 <= r < 256) else 0.0
        m["valid"] = val
        m["btab"] = np.stack([_bias_tables(rpb[l], R0) for l in range(2)])
        maps.append(m)
    return maps


class Seg:
    def __init__(self, nc, name, ntiles, padf, padb):
        self.ntiles, self.padf, self.padb = ntiles, padf, padb
        self.tt = padf + ntiles + padb
        self.arr, self.trk = {}, {}
        for k in ("bg", "uA", "yB", "q", "k", "ynC"):
            self.arr[k] = nc.dram_tensor("%s_%s" % (name, k), [128, 2, self.tt * 128], BF16, kind="Internal").ap()
            self.trk[k] = tracks(self.tt)
        self.arr["v"] = nc.dram_tensor("%s_v" % name, [self.tt, 128, 264], BF16, kind="Internal").ap()
        self.trk["v"] = tracks(self.tt)

    def cols(self, t0, n):
        a = (t0 + self.padf) * 128
        return slice(a, a + n)

    def tr(self, k, t0, t1):
        return self.trk[k][t0 + self.padf:t1 + self.padf]


class KB:
    def __init__(self, nc):
        self.nc = nc

    def act(self, out, in_, func, reads, writes, bias=None, scale=None, accum_out=None):
        kw = {}
        if bias is not None:
            kw["bias"] = bias
        if scale is not None:
            kw["scale"] = scale
        if accum_out is not None:
            kw["accum_out"] = accum_out
        return self.P.op("act", lambda e: e.activation(out=out, in_=in_, func=func, **kw), reads, writes)

    def tt(self, eng, out, in0, in1, op, reads, writes):
        return self.P.op(eng, lambda e: e.tensor_tensor(out=out, in0=in0, in1=in1, op=op), reads, writes)

    def ts(self, eng, out, in0, s1, s2, op0, op1, reads, writes):
        if op1 is None:
            return self.P.op(eng, lambda e: e.tensor_scalar(out=out, in0=in0, scalar1=s1, scalar2=None, op0=op0), reads, writes)
        return self.P.op(eng, lambda e: e.tensor_scalar(out=out, in0=in0, scalar1=s1, scalar2=s2, op0=op0, op1=op1), reads, writes)

    def stt(self, out, in0, scalar, in1, op0, op1, reads, writes):
        return self.P.op("dve", lambda e: e.scalar_tensor_tensor(out=out, in0=in0, scalar=scalar, in1=in1, op0=op0, op1=op1), reads, writes)

    def copy(self, eng, out, in_, reads, writes):
        if eng == "act":
            return self.P.op("act", lambda e: e.activation(out=out, in_=in_, func=AF.Copy), reads, writes)
        return self.P.op(eng, lambda e: e.tensor_copy(out=out, in_=in_), reads, writes)

    def mm(self, out, lhsT, rhs, start, stop, reads, writes, signal=None, sgc=False):
        if signal is None:
            signal = stop
        return self.P.op("pe", lambda e: e.matmul(out, lhsT=lhsT, rhs=rhs, start=start, stop=stop, skip_group_check=sgc),
                         reads, writes, signal=signal)

    def tr(self, out, in_, reads, writes, signal=True):
        ident = self.ident[:]
        return self.P.op("pe", lambda e: e.transpose(out, in_, ident), list(reads) + [self.t_const], writes, signal=signal)

    def sb(self, es, name, shape, dt):
        self._uid = getattr(self, "_uid", 0) + 1
        return es.enter_context(self.nc.sbuf_tensor("%s_u%d" % (name, self._uid), shape, dt))

    def rstd_from(self, dst, src, scale, reads_t, t_dst):
        self.act(dst, src, AF.Ln, reads_t + [self.t_const], [t_dst], bias=self.eps[:, 0:1], scale=scale)
        self.act(dst, dst, AF.Exp, [t_dst], [t_dst], scale=-0.5)

    def setup(self, es):
        nc = self.nc
        self.P = Prog(nc, es)
        P = self.P
        di = lambda name, shape: nc.dram_tensor(name, list(shape), F32, kind="ExternalInput").ap()
        I = {}
        for name, shape in (
            ("x_ext", [NT0 * 128, D]), ("ctxb", [256, D]), ("cT", [128, 8, 2]), ("valid", [128, 10]),
            ("btab", [2, 5, 6, 128, 4, 128]), ("w_mod", [2, D, 6 * D]), ("b_mod", [2, 6 * D]), ("b_modT", [2, 128, 48]),
            ("n1gT", [2, 128, 8]), ("n2gT", [2, 128, 8]), ("w_in", [2, D, 2560]), ("cawT", [2, 128, 2, 3]),
            ("cbwT", [2, 128, 2, 31]), ("cbbT", [2, 128, 2]), ("clgT", [2, 128, 2]), ("clbT", [2, 128, 2]),
            ("sgu_ln_g", [2, 256]), ("sgu_ln_b", [2, 256]), ("sgu_wT", [2, 128, 4, 128]), ("sgu_bT", [2, 128, 4]),
            ("ggT", [2, 128, 8]), ("w_out", [2, D, D]), ("ffn_w1", [D, DFF]), ("ffn_w3", [D, DFF]), ("ffn_w2", [DFF, D]),
            ("router_wT", [128, 8, 8]), ("router_b", [8]), ("moe_wr", [NEXP * 7 * 128, 3 * 4096]), ("final_g", [D]), ("ident", [128, 128]), ("iota_p", [128, 1]), ("utri", [128, 128]), ("eoff", [128, NEXP]), ("bidx", [128, 23]),
        ):
            if STOP_AFTER is not None and name.startswith("moe_w"):
                shape = [128, 128]
            I[name] = di(name, shape)
        self.I = I
        self.out = nc.dram_tensor("out", [32 * 128, D], F32, kind="ExternalOutput").ap()
        kind = "ExternalOutput" if DEBUG_OUT else "Internal"
        self.xmid = nc.dram_tensor("xmid", [NT0 * 128, D], F32, kind=kind).ap()
        self.xl1 = nc.dram_tensor("xl1", [NT0 * 128, D], F32, kind=kind).ap()
        self.xcmid = nc.dram_tensor("xcmid", [256, D], F32, kind=kind).ap()
        self.xc1 = nc.dram_tensor("xc1", [256, D], F32, kind=kind).ap()
        self.t_xmid, self.t_xl1 = tracks(NT0), tracks(NT0)
        self.t_xcmid, self.t_xc1 = tracks(2), tracks(2)
        self.segL = Seg(nc, "L", NT0, PADF, PADB)
        self.segC = Seg(nc, "C", 2, 1, 1)
        self.psall = es.enter_context(nc.psum_tensor("psall", [128, 4096], F32))
        self.ps = [self.psall[:, i * 512:(i + 1) * 512] for i in range(8)]
        self.t_ps = tracks(8)
        self.t_const = Track()
        self.ident = self.sb(es, "ident_sb", [128, 128], BF16)
        self.onesM = self.sb(es, "onesM", [128, 128], BF16)
        self.eps = self.sb(es, "eps", [128, 1], F32)
        self.one1 = self.sb(es, "one1", [128, 1], F32)
        self.zeros = self.sb(es, "zeros", [128, 1024], BF16)
        self.valid = self.sb(es, "valid_sb", [128, 10], F32)
        self.scT = self.sb(es, "scT", [128, 8, 2], BF16)
        self.screp = self.sb(es, "screp", [128, 2, 8, 128], BF16)
        cTf = self.sb(es, "cTf", [128, 8, 2], F32)
        tc = self.t_const
        P.dma("pool", self.ident[:], I["ident"], writes=[tc])
        P.dma("sp", self.valid[:], I["valid"], writes=[tc])
        P.dma("sp", cTf[:], I["cT"], writes=[tc])
        P.op("dve", lambda e: e.memset(self.onesM[:], 1.0 / 256.0), [], [tc])
        P.op("dve", lambda e: e.memset(self.eps[:], EPS), [], [tc])
        P.op("dve", lambda e: e.memset(self.one1[:], 1.0), [], [tc])
        P.op("dve", lambda e: e.memset(self.zeros[:], 0.0), [], [tc])
        self.act(self.scT[:], cTf[:], AF.Silu, [tc], [tc])
        for j in range(2):
            for k in range(8):
                self.copy("dve", self.screp[:, j, k, :], self.scT[:, k, j:j + 1].to_broadcast([128, 128]), [tc], [tc])
        NB = self.NBLK
        self.moe_Xs = nc.dram_tensor("moe_xs", [NEXP * 4096, D], BF16, kind="Internal").ap()
        self.moe_Ys = nc.dram_tensor("moe_ys", [NB * 512, D], F32, kind="Internal").ap()
        self.t_moe_Xs = Track()
        for seg in (self.segL, self.segC):
            for k in ("uA", "yB", "k"):
                a = seg.arr[k]
                for c in range(2):
                    P.dma("sp", a[:, c, 0:seg.padf * 128], self.zeros[:, 0:seg.padf * 128], reads=[tc], writes=seg.trk[k][0:seg.padf])
                    b0 = (seg.padf + seg.ntiles) * 128
                    P.dma("sp", a[:, c, b0:b0 + seg.padb * 128], self.zeros[:, 0:seg.padb * 128], reads=[tc], writes=seg.trk[k][seg.padf + seg.ntiles:])
            va = seg.arr["v"]
            for t in list(range(seg.padf)) + list(range(seg.padf + seg.ntiles, seg.tt)):
                P.dma("sp", va[t], self.zeros[:, 0:264], reads=[tc], writes=[seg.trk["v"][t]])

    def mod_phase(self, l, es_layer):
        P, I = self.P, self.I
        M = {}
        for nm in ("modL", "modC"):
            M[nm] = self.sb(es_layer, "%s_%d" % (nm, l), [128, 48], F32)
        for nm in ("gs1L", "gs2L", "gs1C", "gs2C"):
            M[nm] = self.sb(es_layer, "%s_%d" % (nm, l), [128, 8], F32)
        for nm in ("g1L", "g2L", "g1C", "g2C"):
            M[nm] = self.sb(es_layer, "%s_%d" % (nm, l), [128, D], F32)
        t_mod = Track()
        M["t"] = t_mod
        tc = self.t_const
        with ExitStack() as es:
            wm = [self.sb(es, "wm%d" % i, [128, 8, 512], BF16) for i in range(2)]
            t_wm = tracks(2)
            bmb = self.sb(es, "bmb", [128, 2, D], F32)
            bmT = self.sb(es, "bmT", [128, 48], F32)
            ngT = self.sb(es, "ngT", [128, 2, 8], F32)
            t_misc = Track()
            P.dma("sp", bmb[:, 0, :], I["b_mod"][l, 2048:3072].partition_broadcast(128), writes=[t_misc])
            P.dma("sp", bmb[:, 1, :], I["b_mod"][l, 5120:6144].partition_broadcast(128), writes=[t_misc])
            P.dma("sp", bmT[:], I["b_modT"][l], writes=[t_misc])
            P.dma("sp", ngT[:, 0, :], I["n1gT"][l], writes=[t_misc])
            P.dma("sp", ngT[:, 1, :], I["n2gT"][l], writes=[t_misc])
            wsrc = I["w_mod"][l].rearrange("(kc p) n -> p kc n", p=128)
            psm, t_psm = self.ps[0], self.t_ps[0]
            psm_v = psm[:, 0:96].rearrange("p (n j) -> p n j", j=2)
            bi = 0
            for nb in range(12):
                w, tw = wm[nb % 2], t_wm[nb % 2]
                P.dma("pool", w[:], wsrc[:, :, nb * 512:(nb + 1) * 512], writes=[tw])
                for j in range(4):
                    n = nb * 4 + j
                    for k in range(8):
                        self.mm(psm_v[:, n, :], w[:, k, j * 128:(j + 1) * 128], self.scT[:, k, :], k == 0, k == 7,
                                [tw, tc], [t_psm], signal=(k == 7 and j == 3))
                if nb in (4, 5, 10, 11):
                    gi, half = (0 if nb < 6 else 1), nb % 2
                    for ci, nm in enumerate(("L", "C")):
                        pb, tpb = self.ps[1 + bi % 2], self.t_ps[1 + bi % 2]
                        bi += 1
                        for k in range(8):
                            self.mm(pb[:, :], self.screp[:, ci, k, :], w[:, k, :], k == 0, k == 7, [tw, tc], [tpb])
                        dst = M["g%d%s" % (gi + 1, nm)]
                        self.tt("dve", dst[:, half * 512:(half + 1) * 512], pb[:, :], bmb[:, gi, half * 512:(half + 1) * 512],
                                ALU.add, [tpb, t_misc], [t_mod])
            self.tt("dve", M["modL"][:], psm_v[:, :, 0], bmT[:], ALU.add, [t_psm, t_misc], [t_mod])
            self.tt("dve", M["modC"][:], psm_v[:, :, 1], bmT[:], ALU.add, [t_psm, t_misc], [t_mod])
            for nm in ("L", "C"):
                for i, (gs, off) in enumerate((("gs1", 8), ("gs2", 32))):
                    dst = M[gs + nm]
                    self.ts("dve", dst[:], M["mod" + nm][:, off:off + 8], 1.0, None, ALU.add, None, [t_mod], [t_mod])
                    self.tt("dve", dst[:], dst[:], ngT[:, i, :], ALU.mult, [t_mod, t_misc], [t_mod])
            P.barrier()
        return M

    def p1_alloc(self, es, l):
        P, I = self.P, self.I
        B = {}
        B["w_in"] = self.sb(es, "w_in_sb", [128, 8, 2560], BF16)
        B["t_w"] = Track()
        wsrc = I["w_in"][l].rearrange("(kc p) n -> p kc n", p=128)
        for cb in range(5):
            P.dma("pool", B["w_in"][:, :, cb * 512:(cb + 1) * 512], wsrc[:, :, cb * 512:(cb + 1) * 512], writes=[B["t_w"]])
        B["xt"] = [self.sb(es, "p1xt%d" % i, [128, D], F32) for i in range(2)]
        B["t_xt"] = tracks(2)
        B["junk"] = self.sb(es, "p1junk", [128, D], BF16)
        B["xn"] = [self.sb(es, "p1xn%d" % i, [128, D], BF16) for i in range(2)]
        B["t_xn"] = tracks(2)
        B["st"] = self.sb(es, "p1st", [128, 16], F32)
        B["t_st"] = Track()
        B["hT"] = [self.sb(es, "p1hT%d" % i, [128, 8, 512], BF16) for i in range(3)]
        B["t_hT"] = tracks(3)
        for nm in ("bg", "uA", "yB", "q", "k", "ynC"):
            B[nm] = [self.sb(es, "p1%s%d" % (nm, i), [128, 2, 512], BF16) for i in range(2)]
            B["t_" + nm] = tracks(2)
        B["v"] = [self.sb(es, "p1v%d" % i, [128, 4, 4, 66], BF16) for i in range(2)]
        B["t_v"] = tracks(2)
        B["cg"] = [self.sb(es, "p1cg%d" % i, [128, 512], F32) for i in range(2)]
        B["t_cg"] = tracks(2)
        B["sig"] = [self.sb(es, "p1sig%d" % i, [128, 512], BF16) for i in range(2)]
        B["t_sig"] = tracks(2)
        B["zc"] = [self.sb(es, "p1zc%d" % i, [128, 512], F32) for i in range(4)]
        B["t_zc"] = tracks(4)
        B["tv"] = self.sb(es, "p1tv", [128, 256], F32)
        B["vnb"] = self.sb(es, "p1vnb", [128, 256], BF16)
        B["yC"] = self.sb(es, "p1yC", [128, 256], F32)
        B["ynCt"] = self.sb(es, "p1ynCt", [128, 256], BF16)
        B["t_sgu"] = Track()
        B["bst"] = self.sb(es, "p1bst", [128, 16], F32)
        B["swT"] = self.sb(es, "p1swT", [128, 4, 128], BF16)
        B["sbT"] = self.sb(es, "p1sbT", [128, 4], F32)
        B["slg"] = self.sb(es, "p1slg", [128, 256], F32)
        B["slb"] = self.sb(es, "p1slb", [128, 256], F32)
        B["ggT"] = self.sb(es, "p1ggT", [128, 8], F32)
        B["t_par"] = Track()
        tp = B["t_par"]
        P.dma("pool", B["swT"][:], I["sgu_wT"][l], writes=[tp])
        P.dma("sp", B["sbT"][:], I["sgu_bT"][l], writes=[tp])
        P.dma("sp", B["slg"][:], I["sgu_ln_g"][l].partition_broadcast(128), writes=[tp])
        P.dma("sp", B["slb"][:], I["sgu_ln_b"][l].partition_broadcast(128), writes=[tp])
        P.dma("sp", B["ggT"][:], I["ggT"][l], writes=[tp])
        for i in range(2):
            P.op("dve", lambda e, i=i: e.memset(B["v"][i][:], 1.0), [], [B["t_v"][i]])
        B["n"] = 0
        B["nx"] = 0
        return B

    def norm_to_hT(self, B, xt, t_xt, gsT, shT, t_mod, hT_dst, t_hT, ps_i):
        P = self.P
        i = B["nx"] % 2
        B["nx"] += 1
        xn, t_xn = B["xn"][i], B["t_xn"][i]
        st, t_st = B["st"], B["t_st"]
        col = st[:, i:i + 1]
        self.act(B["junk"][:], xt[:], AF.Square, [t_xt], [t_st], accum_out=col)
        self.rstd_from(col, col, 1.0 / D, [t_st], t_st)
        self.ts("dve", xn[:], xt[:], col, None, ALU.mult, None, [t_xt, t_st], [t_xn])
        psT, t_psT = self.ps[ps_i], self.t_ps[ps_i]
        pv = psT[:, :].bitcast(BF16).rearrange("p (k t) -> p k t", k=8)
        for k in range(8):
            self.tr(pv[:, k, :], xn[:, k * 128:(k + 1) * 128], [t_xn], [t_psT], signal=(k == 7))
        for k in range(8):
            self.ts("dve", hT_dst[:, k, :], pv[:, k, :], gsT[:, k:k + 1], shT[:, k:k + 1], ALU.mult, ALU.add, [t_psT, t_mod], [t_hT])

    def phase1(self, B, seg, tiles, xsrc, t_xsrc, gsT, shT, t_mod, valid_ap, kv_only=False):
        for g in self.phase1_parts(B, seg, tiles, xsrc, t_xsrc, gsT, shT, t_mod, valid_ap, kv_only):
            for _ in g:
                pass

    def phase1_parts(self, B, seg, tiles, xsrc, t_xsrc, gsT, shT, t_mod, valid_ap, kv_only=False):
        it = B["n"]
        B["n"] += 1
        sel = it % 2
        hsel = it % 3
        S = {nm: (B[nm][sel], B["t_" + nm][sel]) for nm in ("bg", "uA", "yB", "q", "k", "ynC", "v")}
        return (self.phase1_N(B, tiles, xsrc, t_xsrc, gsT, shT, t_mod, hsel),
                self.phase1_F(B, seg, tiles, valid_ap, kv_only, hsel, S),
                self.phase1_B(B, seg, tiles, kv_only, hsel, S))

    def phase1_N(self, B, tiles, xsrc, t_xsrc, gsT, shT, t_mod, hsel):
        P = self.P
        hT, t_hT = B["hT"][hsel], B["t_hT"][hsel]
        for j, t in enumerate(tiles):
            i = B["nx"] % 2
            xt, t_xt = B["xt"][i], B["t_xt"][i]
            P.dma("sp", xt[:], xsrc[t * 128:(t + 1) * 128, :], reads=([t_xsrc[t]] if t_xsrc is not None else []), writes=[t_xt])
            self.norm_to_hT(B, xt, t_xt, gsT, shT, t_mod, hT[:, :, j * 128:(j + 1) * 128], t_hT, 3)
            yield

    def phase1_F(self, B, seg, tiles, valid_ap, kv_only, sel, S):
        P = self.P
        nt = len(tiles)
        N = nt * 128
        hT, t_hT = B["hT"][sel], B["t_hT"][sel]
        w_in, t_w = B["w_in"], B["t_w"]
        tc = self.t_const
        order = [16, 17] if kv_only else [0, 1, 2, 4, 3, 5, 8, 6, 9, 7, 14, 15, 16, 17]
        for oi, cc in enumerate(order):
            pb, tpb = self.ps[oi % 3], self.t_ps[oi % 3]
            for k in range(8):
                self.mm(pb[:, 0:N], w_in[:, k, cc * 128:(cc + 1) * 128], hT[:, k, 0:N], k == 0, k == 7, [t_w, t_hT], [tpb])
            c = cc % 2
            if cc in (0, 1):
                self.copy("act", S["bg"][0][:, c, 0:N], pb[:, 0:N], [tpb], [S["bg"][1]])
            elif cc in (2, 3):
                self.copy("act", B["cg"][c][:, 0:N], pb[:, 0:N], [tpb], [B["t_cg"][c]])
            elif cc in (4, 5):
                self.stt(S["uA"][0][:, c, 0:N], pb[:, 0:N], valid_ap, B["cg"][c][:, 0:N], ALU.mult, ALU.mult,
                         [tpb, B["t_cg"][c], tc], [S["uA"][1]])
            elif cc in (8, 9):
                self.act(B["sig"][c][:, 0:N], pb[:, 0:N], AF.Sigmoid, [tpb], [B["t_sig"][c]])
            elif cc in (6, 7):
                self.stt(S["yB"][0][:, c, 0:N], pb[:, 0:N], valid_ap, B["sig"][c][:, 0:N], ALU.mult, ALU.mult,
                         [tpb, B["t_sig"][c], tc], [S["yB"][1]])
            elif cc in (14, 15):
                self.copy("act" if c else "dve", S["q"][0][:, c, 0:N], pb[:, 0:N], [tpb], [S["q"][1]])
            else:
                self.copy("act" if c else "dve", S["k"][0][:, c, 0:N], pb[:, 0:N], [tpb], [S["k"][1]])
            yield
        t0 = tiles[0]
        cs = seg.cols(t0, N)
        names = ("k",) if kv_only else ("bg", "uA", "yB", "q", "k")
        for nm in names:
            for c in range(2):
                P.dma("pool", seg.arr[nm][:, c, cs], S[nm][0][:, c, 0:N], reads=[S[nm][1]], writes=seg.tr(nm, t0, t0 + nt))
        yield

    def phase1_B(self, B, seg, tiles, kv_only, sel, S):
        P = self.P
        nt = len(tiles)
        N = nt * 128
        hT, t_hT = B["hT"][sel], B["t_hT"][sel]
        w_in, t_w = B["w_in"], B["t_w"]
        for j, t in enumerate(tiles):
            pv, tpv = self.ps[6], self.t_ps[6]
            for k in range(8):
                self.mm(pv[:, 0:256], hT[:, k, j * 128:(j + 1) * 128], w_in[:, k, 2304:2560], k == 0, k == 7, [t_w, t_hT], [tpv])
            self.copy("dve", S["v"][0][:, j, :, 0:64], pv[:, 0:256].rearrange("p (h d) -> p h d", h=4), [tpv], [S["v"][1]])
            yield
            if kv_only:
                continue
            pc, tpc = self.ps[4 + j % 2], self.t_ps[4 + j % 2]
            for k in range(8):
                self.mm(pc[:, :], hT[:, k, j * 128:(j + 1) * 128], w_in[:, k, 1280:1792], k == 0, k == 7, [t_w, t_hT], [tpc])
            self.act(B["zc"][j][:], pc[:, :], AF.Gelu_apprx_tanh, [tpc], [B["t_zc"][j]])
            yield
        if not kv_only:
            for j, t in enumerate(tiles):
                for _ in self.sgu_tile(B, S["ynC"][0][:, :, j * 128:(j + 1) * 128], S["ynC"][1], j):
                    yield
        t0 = tiles[0]
        cs = seg.cols(t0, N)
        if not kv_only:
            for c in range(2):
                P.dma("pool", seg.arr["ynC"][:, c, cs], S["ynC"][0][:, c, 0:N], reads=[S["ynC"][1]], writes=seg.tr("ynC", t0, t0 + nt))
        for j, t in enumerate(tiles):
            P.dma("pool", seg.arr["v"][t + seg.padf], S["v"][0][:, j, :, :].rearrange("p h d -> p (h d)"),
                  reads=[S["v"][1]], writes=seg.tr("v", t, t + 1))
        yield

    def sgu_tile(self, B, ynC_dst, t_ynC, j):
        P = self.P
        zc, t_zc = B["zc"][j], B["t_zc"][j]
        tp, ts_ = B["t_par"], B["t_sgu"]
        bst = B["bst"]
        stats = B["tv"]
        P.op("dve", lambda e: e.bn_stats(out=bst[:, 0:6], in_=zc[:, 256:512]), [t_zc], [ts_])
        mv = bst[:, 8:10]
        P.op("dve", lambda e: e.bn_aggr(out=mv[:, 0:2], in_=bst[:, 0:6]), [ts_], [ts_])
        self.rstd_from(mv[:, 1:2], mv[:, 1:2], 1.0, [ts_], ts_)
        self.ts("dve", stats[:], zc[:, 256:512], mv[:, 0:1], mv[:, 1:2], ALU.subtract, ALU.mult, [t_zc, ts_], [ts_])
        self.tt("dve", stats[:], stats[:], B["slg"][:], ALU.mult, [ts_, tp], [ts_])
        self.tt("dve", B["vnb"][:], stats[:], B["slb"][:], ALU.add, [ts_, tp], [ts_])
        yield
        pm, tpm = self.ps[6], self.t_ps[6]
        for h in range(4):
            self.mm(pm[:, 256 + h * 64:256 + (h + 1) * 64], B["swT"][:, h, :], B["vnb"][:, h * 64:(h + 1) * 64], True, True,
                    [ts_, tp], [tpm], signal=(h == 3))
        yC = B["yC"]
        for h in range(4):
            self.stt(yC[:, h * 64:(h + 1) * 64], pm[:, 256 + h * 64:256 + (h + 1) * 64], B["sbT"][:, h:h + 1],
                     zc[:, h * 64:(h + 1) * 64], ALU.add, ALU.mult, [tpm, tp, t_zc, ts_], [ts_])
        yield
        self.group_norm_T(B, yC, ts_, B["ynCt"], 4, ynC_dst, t_ynC)
        yield

    def group_norm_T(self, B, y, t_y, ynb, gidx, dstT, t_dst):
        P = self.P
        ts_ = t_y
        col = B["bst"][:, 10:11]
        self.act(B["junk"][:, 0:256], y[:], AF.Square, [t_y], [ts_], accum_out=col)
        self.rstd_from(col, col, 1.0 / 256.0, [ts_], ts_)
        self.ts("dve", ynb[:], y[:], col, None, ALU.mult, None, [ts_], [ts_])
        pT, tpT = self.ps[7], self.t_ps[7]
        pv = pT[:, :].bitcast(BF16)
        for c in range(2):
            self.tr(pv[:, c * 128:(c + 1) * 128], ynb[:, c * 128:(c + 1) * 128], [ts_], [tpT], signal=(c == 1))
        for c in range(2):
            self.act(dstT[:, c, :], pv[:, c * 128:(c + 1) * 128], AF.Copy, [tpT, B["t_par"]], [t_dst],
                     scale=B["ggT"][:, gidx + c:gidx + c + 1])

    def p2_alloc(self, es, l):
        P, I = self.P, self.I
        B = {}
        B["w_out"] = self.sb(es, "w_out_sb", [128, 8, D], BF16)
        B["t_w"] = Track()
        wsrc = I["w_out"][l].rearrange("(kc p) n -> p kc n", p=128)
        for cb in range(2):
            P.dma("pool", B["w_out"][:, :, cb * 512:(cb + 1) * 512], wsrc[:, :, cb * 512:(cb + 1) * 512], writes=[B["t_w"]])
        B["t_par"] = Track()
        tp = B["t_par"]
        for nm, key, shape in (("caw", "cawT", [128, 2, 3]), ("cbw", "cbwT", [128, 2, 31]), ("cbb", "cbbT", [128, 2]),
                               ("clg", "clgT", [128, 2]), ("clb", "clbT", [128, 2]), ("ggT", "ggT", [128, 8])):
            B[nm] = self.sb(es, "p2" + nm, shape, F32)
            P.dma("sp", B[nm][:], I[key][l], writes=[tp])
        for nm, shape in (("uAw", [128, 2, 514]), ("bgw", [128, 2, 512]), ("yBw", [128, 2, 542]), ("qw", [128, 2, 512]),
                          ("kw", [128, 2, 1280]), ("vw", [128, 10, 4, 66])):
            B[nm] = [self.sb(es, "p2%s%d" % (nm, i), shape, BF16) for i in range(2)]
            B["t_" + nm] = tracks(2)
        B["ynT"] = [self.sb(es, "p2ynT%d" % i, [128, 8, 512], BF16) for i in range(2)]
        B["t_ynT"] = tracks(2)
        B["dg"] = self.sb(es, "p2dg", [128, 2, 31, 128], BF16)
        for c in range(2):
            for jt in range(31):
                self.ts("dve", B["dg"][:, c, jt, :], self.ident[:], B["cbw"][:, c, jt:jt + 1], None, ALU.mult, None,
                        [self.t_const, tp], [tp])
        B["accA"] = self.sb(es, "p2accA", [128, 2, 512], F32)
        B["zf"] = self.sb(es, "p2zf", [128, 2, 512], F32)
        B["zb"] = self.sb(es, "p2zb", [128, 2, 512], BF16)
        B["zsq"] = self.sb(es, "p2zsq", [128, 2, 512], BF16)
        B["mean"] = self.sb(es, "p2mean", [128, 512], F32)
        B["var"] = self.sb(es, "p2var", [128, 512], F32)
        B["tmpn"] = self.sb(es, "p2tmpn", [128, 512], F32)
        B["t_cv"] = Track()
        B["PT"] = [self.sb(es, "p2PT%d" % i, [128, 512], BF16) for i in range(2)]
        B["t_PT"] = tracks(2)
        B["rec"] = self.sb(es, "p2rec", [128, 4], F32)
        B["yD"] = self.sb(es, "p2yD", [128, 256], F32)
        B["ynD"] = self.sb(es, "p2ynD", [128, 256], BF16)
        B["bst"] = self.sb(es, "p2bst", [128, 16], F32)
        B["junk"] = self.sb(es, "p2junk", [128, 256], BF16)
        B["t_at"] = Track()
        B["TG"] = self.sb(es, "p2TG", [128, 6, 512], BF16)
        B["TS"] = [self.sb(es, "p2TS%d" % i, [128, 6, 512], BF16) for i in range(2)]
        B["t_TG"], B["t_TS"] = Track(), tracks(2)
        B["tst"] = [self.sb(es, "p2tst%d" % i, [128, 512], F32) for i in range(2)]
        B["t_tst"] = tracks(2)
        B["cK"] = self.sb(es, "p2cK", [128, 2, 256], BF16)
        B["cV"] = self.sb(es, "p2cV", [128, 2, 4, 66], BF16)
        B["t_ckv"] = Track()
        B["xres"] = [self.sb(es, "p2xres%d" % i, [128, D], F32) for i in range(2)]
        B["t_xres"] = tracks(2)
        B["tmpo"] = [self.sb(es, "p2tmpo%d" % i, [128, D], F32) for i in range(2)]
        B["t_tmpo"] = tracks(2)
        B["n"] = 0
        B["nt"] = 0
        B["ntab"] = 0
        B["l"] = l
        return B

    def table_prep(self, B, slot, dst, t_dst):
        P, I = self.P, self.I
        for ch in range(6):
            i = B["ntab"] % 2
            B["ntab"] += 1
            st, t_st = B["tst"][i], B["t_tst"][i]
            P.dma("sp", st[:], I["btab"][B["l"], slot, ch].rearrange("p h q -> p (h q)"), writes=[t_st])
            self.act(dst[:, ch, :], st[:], AF.Exp, [t_st], [t_dst])

    def load_ctx_kv(self, B):
        P, seg = self.P, self.segC
        cs = seg.cols(0, 256)
        for c in range(2):
            P.dma("sp", B["cK"][:, c, :], seg.arr["k"][:, c, cs], reads=seg.tr("k", 0, 2), writes=[B["t_ckv"]])
        for t in range(2):
            P.dma("sp", B["cV"][:, t, :, :].rearrange("p h d -> p (h d)"), seg.arr["v"][t + seg.padf],
                  reads=seg.tr("v", t, t + 1), writes=[B["t_ckv"]])

    def attn_super(self, B, qw, t_qw, specs, t_ynT):
        O, tO = self.ps[6], self.t_ps[6]
        Ov = O[:, 0:260].rearrange("p (h d) -> p h d", h=4)
        flat = [(j, ci, len(chunks), ch, dst) for (j, chunks, dst) in specs for ci, ch in enumerate(chunks)]

        def s_mm(idx):
            j, ci, nch, (kap, vap, tab, rtr), dst = flat[idx]
            ba = 4 if idx % 2 == 0 else 2
            for h in range(4):
                c, hp = h // 2, (h % 2) * 64
                bk = ba + (h % 2)
                self.mm(self.ps[bk][:, c * 128:(c + 1) * 128], kap[hp:hp + 64, c, :], qw[hp:hp + 64, c, j * 128:(j + 1) * 128], True, True,
                        list(rtr) + [t_qw], [self.t_ps[bk]], signal=(h >= 2))

        for idx in range(min(2, len(flat))):
            s_mm(idx)
        for idx, (j, ci, nch, (kap, vap, tab, rtr), dst) in enumerate(flat):
            ba = 4 if idx % 2 == 0 else 2
            PT, tPT = B["PT"][idx % 2], B["t_PT"][idx % 2]
            src = self.psall[:, ba * 512:(ba + 2) * 512].rearrange("p (two r) -> p two r", two=2)[:, :, 0:256].rearrange("p two (c q) -> p two c q", c=2)
            dstv = PT[:].rearrange("p (c two q) -> p two c q", c=2, two=2)
            self.act(dstv, src, AF.Exp, [self.t_ps[ba], self.t_ps[ba + 1]], [tPT], scale=0.125)
            if idx + 2 < len(flat):
                s_mm(idx + 2)
            if tab is not None:
                self.tt("dve", PT[:], PT[:], tab[0], ALU.mult, [tPT, tab[1]], [tPT])
            for h in range(4):
                self.mm(Ov[:, h, :], PT[:, h * 128:(h + 1) * 128], vap[:, h, 0:65], (ci == 0 and h == 0), (ci == nch - 1),
                        list(rtr) + [tPT], [tO], signal=(h == 3), sgc=True)
            yield
            if ci == nch - 1:
                ta = B["t_at"]
                self.P.op("dve", lambda e: e.reciprocal(out=B["rec"][:], in_=Ov[:, :, 64]), [tO], [ta])
                for h in range(4):
                    self.ts("dve", B["yD"][:, h * 64:(h + 1) * 64], Ov[:, h, 0:64], B["rec"][:, h:h + 1], None, ALU.mult, None, [tO, ta], [ta])
                self.group_norm_T(B, B["yD"], ta, B["ynD"], 6, dst, t_ynT)
                yield

    def xpart_rms(self, B, y, gidx, ynT, t_ynT, N):
        tcv, tp = B["t_cv"], B["t_par"]
        sq = B["zsq"]
        for c in range(2):
            self.act(sq[:, c, 0:N], y[:, c, 0:N], AF.Square, [tcv], [tcv])
        pm, tpm = self.ps[1], self.t_ps[1]
        for c in range(2):
            self.mm(pm[:, 0:N], self.onesM[:], sq[:, c, 0:N], c == 0, c == 1, [tcv, self.t_const], [tpm])
        r = B["var"]
        self.rstd_from(r[:, 0:N], pm[:, 0:N], 1.0, [tpm, tcv], tcv)
        for c in range(2):
            self.stt(ynT[:, gidx + c, 0:N], y[:, c, 0:N], B["ggT"][:, gidx + c:gidx + c + 1], r[:, 0:N], ALU.mult, ALU.mult,
                     [tcv, tp], [t_ynT])

    def phase2(self, B, seg, tiles, is_ctx, xsrc, t_xsrc, xdst, t_xdst, gate):
        ga, gb = self.phase2_parts(B, seg, tiles, is_ctx, xsrc, t_xsrc, xdst, t_xdst, gate)
        for _ in ga:
            pass
        for _ in gb:
            pass

    def phase2_parts(self, B, seg, tiles, is_ctx, xsrc, t_xsrc, xdst, t_xdst, gate):
        sel = B["n"] % 2
        B["n"] += 1
        W = {nm: (B[nm][sel], B["t_" + nm][sel]) for nm in ("uAw", "bgw", "yBw", "qw", "kw", "vw", "ynT")}
        return (self.phase2_A(B, seg, tiles, is_ctx, W), self.phase2_B(B, seg, tiles, is_ctx, xsrc, t_xsrc, xdst, t_xdst, gate, W))

    def phase2_A(self, B, seg, tiles, is_ctx, W):
        P = self.P
        nt = len(tiles)
        N = nt * 128
        t0 = tiles[0]
        ynT, t_ynT = W["ynT"]
        a0 = (t0 + seg.padf) * 128
        wl = 1 if is_ctx else 3
        nwin = nt + 2 if is_ctx else nt + 6
        tcv, tp = B["t_cv"], B["t_par"]
        for c in range(2):
            P.dma("sp", W["uAw"][0][:, c, 0:N + 2], seg.arr["uA"][:, c, a0 - 1:a0 + N + 1], reads=seg.tr("uA", t0 - 1, t0 + nt + 1), writes=[W["uAw"][1]])
            P.dma("sp", W["yBw"][0][:, c, 0:N + 30], seg.arr["yB"][:, c, a0 - 15:a0 + N + 15], reads=seg.tr("yB", t0 - 1, t0 + nt + 1), writes=[W["yBw"][1]])
            P.dma("sp", W["bgw"][0][:, c, 0:N], seg.arr["bg"][:, c, a0:a0 + N], reads=seg.tr("bg", t0, t0 + nt), writes=[W["bgw"][1]])
            P.dma("sp", W["qw"][0][:, c, 0:N], seg.arr["q"][:, c, a0:a0 + N], reads=seg.tr("q", t0, t0 + nt), writes=[W["qw"][1]])
            P.dma("sp", ynT[:, 4 + c, 0:N], seg.arr["ynC"][:, c, a0:a0 + N], reads=seg.tr("ynC", t0, t0 + nt), writes=[t_ynT])
            if not is_ctx:
                P.dma("sp", W["kw"][0][:, c, 0:nwin * 128], seg.arr["k"][:, c, a0 - wl * 128:a0 + (nwin - wl) * 128],
                      reads=seg.tr("k", t0 - wl, t0 - wl + nwin), writes=[W["kw"][1]])
        if not is_ctx:
            for wi in range(nwin):
                tt_ = t0 - wl + wi
                P.dma("sp", W["vw"][0][:, wi, :, :].rearrange("p h d -> p (h d)"), seg.arr["v"][tt_ + seg.padf],
                      reads=seg.tr("v", tt_, tt_ + 1), writes=[W["vw"][1]])
        yield
        uAw, t_uAw = W["uAw"]
        acc = B["accA"]
        for c in range(2):
            self.ts("dve", acc[:, c, 0:N], uAw[:, c, 0:N], B["caw"][:, c, 0:1], None, ALU.mult, None, [t_uAw, tp], [tcv])
            for jt in (1, 2):
                self.stt(acc[:, c, 0:N], uAw[:, c, jt:jt + N], B["caw"][:, c, jt:jt + 1], acc[:, c, 0:N], ALU.mult, ALU.add, [t_uAw, tp, tcv], [tcv])
            yield
            self.tt("dve", acc[:, c, 0:N], acc[:, c, 0:N], W["bgw"][0][:, c, 0:N], ALU.mult, [tcv, W["bgw"][1]], [tcv])
            yield
        self.xpart_rms(B, acc, 0, ynT, t_ynT, N)
        yield
        yBw, t_yBw = W["yBw"]
        zf = B["zf"]
        for c in range(2):
            pcv, tpcv = self.ps[c], self.t_ps[c]
            for jt in range(31):
                self.mm(pcv[:, 0:N], B["dg"][:, c, jt, :], yBw[:, c, jt:jt + N], jt == 0, jt == 30, [t_yBw, tp], [tpcv])
                if jt % 4 == 3:
                    yield
            self.act(zf[:, c, 0:N], pcv[:, 0:N], AF.Identity, [tpcv, tp], [tcv], bias=B["cbb"][:, c:c + 1])
            yield
            self.copy("act", B["zb"][:, c, 0:N], zf[:, c, 0:N], [tcv], [tcv])
            self.act(B["zsq"][:, c, 0:N], zf[:, c, 0:N], AF.Square, [tcv], [tcv])
            yield
        pmean, tpmean = self.ps[0], self.t_ps[0]
        pmsq, tpmsq = self.ps[1], self.t_ps[1]
        for c in range(2):
            self.mm(pmean[:, 0:N], self.onesM[:], B["zb"][:, c, 0:N], c == 0, c == 1, [tcv, self.t_const], [tpmean])
        for c in range(2):
            self.mm(pmsq[:, 0:N], self.onesM[:], B["zsq"][:, c, 0:N], c == 0, c == 1, [tcv, self.t_const], [tpmsq])
        mean, var, tmpn = B["mean"], B["var"], B["tmpn"]
        yield
        self.copy("act", mean[:, 0:N], pmean[:, 0:N], [tpmean], [tcv])
        self.act(tmpn[:, 0:N], pmean[:, 0:N], AF.Square, [tpmean], [tcv])
        yield
        self.tt("dve", var[:, 0:N], pmsq[:, 0:N], tmpn[:, 0:N], ALU.subtract, [tpmsq, tcv], [tcv])
        self.ts("dve", var[:, 0:N], var[:, 0:N], 0.0, None, ALU.max, None, [tcv], [tcv])
        self.rstd_from(var[:, 0:N], var[:, 0:N], 1.0, [tcv], tcv)
        yield
        for c in range(2):
            self.tt("dve", tmpn[:, 0:N], zf[:, c, 0:N], mean[:, 0:N], ALU.subtract, [tcv], [tcv])
            self.tt("dve", tmpn[:, 0:N], tmpn[:, 0:N], var[:, 0:N], ALU.mult, [tcv], [tcv])
            self.act(zf[:, c, 0:N], tmpn[:, 0:N], AF.Silu, [tcv, tp], [tcv], bias=B["clb"][:, c:c + 1], scale=B["clg"][:, c:c + 1])
            yield
        self.xpart_rms(B, zf, 2, ynT, t_ynT, N)
        yield

    def phase2_B(self, B, seg, tiles, is_ctx, xsrc, t_xsrc, xdst, t_xdst, gate, W):
        P = self.P
        nt = len(tiles)
        N = nt * 128
        t0 = tiles[0]
        ynT, t_ynT = W["ynT"]
        wl = 1 if is_ctx else 3
        qw, t_qw = W["qw"]
        kw, t_kw = W["kw"]
        vw, t_vw = W["vw"]
        ctx_chunks = [(B["cK"][:, :, ci * 128:(ci + 1) * 128], B["cV"][:, ci, :, :], None, [B["t_ckv"]]) for ci in range(2)]
        specs = []
        tabs = {}
        for j, t in enumerate(tiles):
            chunks = []
            if not is_ctx:
                own = t - OWN_LO
                ws, slot = t - 2, 0
                if own in (0, 1, 30, 31):
                    slot = {0: 1, 1: 2, 30: 3, 31: 4}[own]
                    if own == 31:
                        ws = t - 3
                    tab, t_tab = B["TS"][j % 2], B["t_TS"][j % 2]
                    self.table_prep(B, slot, tab, t_tab)
                else:
                    tab, t_tab = B["TG"], B["t_TG"]
                for ch in range(6):
                    wi = ws + ch - (t0 - wl)
                    chunks.append((kw[:, :, wi * 128:(wi + 1) * 128], vw[:, wi, :, :], (tab[:, ch, :], t_tab), [t_kw, t_vw]))
            specs.append((j, chunks + ctx_chunks, ynT[:, 6:8, j * 128:(j + 1) * 128]))
        for _ in self.attn_super(B, qw, t_qw, specs, t_ynT):
            yield
        for j, t in enumerate(tiles):
            i = B["nt"] % 2
            B["nt"] += 1
            xr, t_xr = B["xres"][i], B["t_xres"][i]
            tm, t_tm = B["tmpo"][i], B["t_tmpo"][i]
            P.dma("sp", xr[:], xsrc[t * 128:(t + 1) * 128, :], reads=([t_xsrc[t]] if t_xsrc is not None else []), writes=[t_xr])
            for cb in range(2):
                po, tpo = self.ps[2 + cb], self.t_ps[2 + cb]
                for k in range(8):
                    self.mm(po[:, :], ynT[:, k, j * 128:(j + 1) * 128], B["w_out"][:, k, cb * 512:(cb + 1) * 512], k == 0, k == 7,
                            [t_ynT, B["t_w"]], [tpo])
                self.tt("dve", tm[:, cb * 512:(cb + 1) * 512], po[:, :], gate[0][:, cb * 512:(cb + 1) * 512], ALU.mult, [tpo, gate[1]], [t_tm])
            self.tt("pool", tm[:], tm[:], xr[:], ALU.add, [t_tm, t_xr], [t_tm])
            P.dma("pool", xdst[t * 128:(t + 1) * 128, :], tm[:], reads=[t_tm], writes=[t_xdst[t]])
            yield

    def ffn_phase(self, l, M, blocks, experts, moe):
        P, I = self.P, self.I
        assert not moe
        w1, w3, w2, nf = experts[0]
        with ExitStack() as es:
            B = {}
            B["xt"] = [self.sb(es, "p3xt%d" % i, [128, D], F32) for i in range(2)]
            B["t_xt"] = tracks(2)
            B["junk"] = self.sb(es, "p3junk", [128, D], BF16)
            B["xn"] = [self.sb(es, "p3xn%d" % i, [128, D], BF16) for i in range(2)]
            B["t_xn"] = tracks(2)
            B["st"] = self.sb(es, "p3st", [128, 16], F32)
            B["t_st"] = Track()
            B["nx"] = 0
            h2T = [self.sb(es, "p3h2T%d" % i, [128, 8, 1024], BF16) for i in range(2)]
            t_h2T = tracks(2)
            yacc = self.sb(es, "p3yacc", [128, 8, D], F32)
            t_yacc = tracks(8)
            NWB = 3
            w1b = [self.sb(es, "p3w1b%d" % i, [128, 8, 512], BF16) for i in range(NWB)]
            w3b = [self.sb(es, "p3w3b%d" % i, [128, 8, 512], BF16) for i in range(NWB)]
            w2b = [self.sb(es, "p3w2b%d" % i, [128, 4, D], BF16) for i in range(NWB)]
            t_w = tracks(NWB)
            gT = [self.sb(es, "p3gT%d" % i, [128, 4, 512], BF16) for i in range(2)]
            t_gT = tracks(2)
            sl = [self.sb(es, "p3sl%d" % i, [128, 512], BF16) for i in range(2)]
            t_sl = tracks(2)
            ro = [self.sb(es, "p3ro%d" % i, [128, D], F32) for i in range(2)]
            t_ro = tracks(2)
            units = []
            f0 = 0
            while f0 < nf:
                nfc = min(4, nf - f0)
                units.append((f0, nfc))
                f0 += nfc
            allu = [(bi, u) for bi in range(len(blocks)) for u in units]
            nw = [0]

            def load_unit(u):
                f0, nfc = u
                sel = nw[0] % NWB
                nw[0] += 1
                s1 = w1.rearrange("(kc p) f -> p kc f", p=128)[:, :, f0 * 128:(f0 + nfc) * 128]
                s3 = w3.rearrange("(kc p) f -> p kc f", p=128)[:, :, f0 * 128:(f0 + nfc) * 128]
                s2 = w2.rearrange("(fc p) d -> p fc d", p=128)[:, f0:f0 + nfc, :]
                P.dma("pool", w1b[sel][:, :, 0:nfc * 128], s1, writes=[t_w[sel]])
                P.dma("pool", w3b[sel][:, :, 0:nfc * 128], s3, writes=[t_w[sel]])
                P.dma("pool", w2b[sel][:, 0:nfc, :], s2, writes=[t_w[sel]])

            def gen_norm(bi):
                hb, t_hb = h2T[bi % 2], t_h2T[bi % 2]
                for j, td in enumerate(blocks[bi]):
                    i = B["nx"] % 2
                    xt, t_xt = B["xt"][i], B["t_xt"][i]
                    P.dma("sp", xt[:], td["src"], reads=[td["t_src"]], writes=[t_xt])
                    self.norm_to_hT(B, xt, t_xt, M["gs2" + td["mod"]], M["mod" + td["mod"]][:, 24:32], M["t"],
                                    hb[:, :, j * 128:(j + 1) * 128], t_hb, 7)
                    yield

            for u0 in range(NWB - 1):
                load_unit(allu[u0][1])
            for _ in gen_norm(0):
                pass
            ng = 0
            gu = 0
            for bi, blk in enumerate(blocks):
                nt = len(blk)
                hb, t_hb = h2T[bi % 2], t_h2T[bi % 2]
                nxt = gen_norm(bi + 1) if bi + 1 < len(blocks) else None
                for ui, u in enumerate(units):
                    cur = gu % NWB
                    if gu + NWB - 1 < len(allu):
                        load_unit(allu[gu + NWB - 1][1])
                    gu += 1
                    f0, nfc = u
                    first = (ui == 0)
                    for sb0 in range(0, nt, 4):
                        sbt = min(4, nt - sb0)
                        N = sbt * 128
                        g, t_g = gT[ng % 2], t_gT[ng % 2]
                        ng += 1
                        for fc in range(nfc):
                            p1, tp1 = self.ps[(fc % 2) * 2], self.t_ps[(fc % 2) * 2]
                            p3, tp3 = self.ps[(fc % 2) * 2 + 1], self.t_ps[(fc % 2) * 2 + 1]
                            for k in range(8):
                                self.mm(p1[:, 0:N], w1b[cur][:, k, fc * 128:(fc + 1) * 128], hb[:, k, sb0 * 128:sb0 * 128 + N], k == 0, k == 7,
                                        [t_w[cur], t_hb], [tp1])
                            for k in range(8):
                                self.mm(p3[:, 0:N], w3b[cur][:, k, fc * 128:(fc + 1) * 128], hb[:, k, sb0 * 128:sb0 * 128 + N], k == 0, k == 7,
                                        [t_w[cur], t_hb], [tp3])
                            s_, t_s = sl[fc % 2], t_sl[fc % 2]
                            self.act(s_[:, 0:N], p1[:, 0:N], AF.Silu, [tp1], [t_s])
                            self.tt("dve", g[:, fc, 0:N], p3[:, 0:N], s_[:, 0:N], ALU.mult, [tp3, t_s], [t_g])
                        for jj in range(sbt):
                            j = sb0 + jj
                            for cb in range(2):
                                py, tpy = self.ps[4 + (jj % 2) * 2 + cb], self.t_ps[4 + (jj % 2) * 2 + cb]
                                for fc in range(nfc):
                                    self.mm(py[:, :], g[:, fc, jj * 128:(jj + 1) * 128], w2b[cur][:, fc, cb * 512:(cb + 1) * 512], fc == 0, fc == nfc - 1,
                                            [t_g, t_w[cur]], [tpy])
                                ya = yacc[:, j, cb * 512:(cb + 1) * 512]
                                if first:
                                    self.copy("dve", ya, py[:, :], [tpy], [t_yacc[j]])
                                else:
                                    self.tt("dve", ya, py[:, :], ya, ALU.add, [tpy, t_yacc[j]], [t_yacc[j]])
                        if nxt is not None and ui >= 1:
                            for _ in range(1):
                                try:
                                    next(nxt)
                                except StopIteration:
                                    nxt = None
                                    break
                if nxt is not None:
                    for _ in nxt:
                        pass
                for j, td in enumerate(blk):
                    i = B["nx"] % 2
                    B["nx"] += 1
                    xt, t_xt = B["xt"][i], B["t_xt"][i]
                    P.dma("sp", xt[:], td["src"], reads=[td["t_src"]], writes=[t_xt])
                    g2 = M["g2" + td["mod"]]
                    r_, t_r = ro[j % 2], t_ro[j % 2]
                    self.tt("dve", r_[:], yacc[:, j, :], g2[:], ALU.mult, [t_yacc[j], M["t"]], [t_r])
                    self.tt("dve", r_[:], r_[:], xt[:], ALU.add, [t_r, t_xt], [t_r])
                    P.dma("pool", td["dst"], r_[:], reads=[t_r], writes=[td["t_dst"]])
            P.barrier()

    def pipeline(self, parts):
        if not parts:
            return
        ns = len(parts[0])
        n = len(parts)
        for step in range(n + ns - 1):
            live = []
            for k in range(ns):
                s_ = step - k
                if 0 <= s_ < n:
                    live.append(parts[s_][k])
            live.reverse()
            while live:
                for g in list(live):
                    try:
                        next(g)
                    except StopIteration:
                        live.remove(g)

    NBLK = 23

    def moe_sparse(self, M, tiles):
        P, I, nc = self.P, self.I, self.nc
        NB = self.NBLK
        IO = bass.IndirectOffsetOnAxis
        Xs, Ys, t_Xs = self.moe_Xs, self.moe_Ys, self.t_moe_Xs
        t_Ys = tracks(NB * 4)
        ntile = len(tiles)
        with ExitStack() as es0:
            slots_i = self.sb(es0, "ms_slots", [128, ntile, 2], I32)
            ghl = self.sb(es0, "ms_ghl", [128, ntile, 2], F32)
            idx_i = self.sb(es0, "ms_idx", [128, NB, 7], I32)
            idxX_i = self.sb(es0, "ms_idxX", [128, NB, 4], I32)
            fing = self.sb(es0, "ms_fing", [128, D], F32)
            t_rt = Track()
            t_par = Track()
            t_Xw = tracks(2 * ntile)
            P.dma("sp", fing[:], I["final_g"].partition_broadcast(128), writes=[t_par])
            NWB = 3
            wall = [self.sb(es0, "mewall%d" % i, [128, 3 * 4096], BF16) for i in range(NWB)]
            w1b = [w[:, 0:4096].rearrange("p (k f) -> p k f", k=8) for w in wall]
            w3b = [w[:, 4096:8192].rearrange("p (k f) -> p k f", k=8) for w in wall]
            w2b = [w[:, 8192:12288].rearrange("p (k f) -> p k f", k=4) for w in wall]
            t_w = tracks(NWB)
            units = [(b, fb) for b in range(NB) for fb in range(7)]
            nw = [0]

            def load_unit(u):
                b, fb = u
                sel = nw[0] % NWB
                nw[0] += 1
                off = IO(ap=idx_i[:, b, fb:fb + 1], axis=0)
                P.idma(wall[sel][:], None, I["moe_wr"], off, reads=[t_rt], writes=[t_w[sel]])
                return sel
            with ExitStack() as es:
                B = {}
                B["xt"] = [self.sb(es, "msxt%d" % i, [128, D], F32) for i in range(2)]
                B["t_xt"] = tracks(2)
                B["junk"] = self.sb(es, "msjunk", [128, D], BF16)
                B["xn"] = [self.sb(es, "msxn%d" % i, [128, D], BF16) for i in range(2)]
                B["t_xn"] = tracks(2)
                B["st"] = self.sb(es, "msst", [128, 16], F32)
                B["t_st"] = Track()
                B["nx"] = 0
                h2tok = self.sb(es, "msh2tok", [128, ntile, D], BF16)
                t_h2tok = tracks(ntile)
                hTt = [self.sb(es, "mshTt%d" % i, [128, 8, 128], BF16) for i in range(2)]
                t_hTt = tracks(2)
                zer = self.sb(es, "mszer", [128, D], BF16)
                rw = self.sb(es, "msrw", [128, 8, 8], BF16)
                rbb = self.sb(es, "msrbb", [128, 8], F32)
                utri = self.sb(es, "msutri", [128, 128], BF16)
                onesb = self.sb(es, "msones", [128, 128], BF16)
                iota = self.sb(es, "msiota", [128, 1], F32)
                rs = self.sb(es, "msrs", [128, 8, 8], F32)
                t_rs = Track()
                maskb = self.sb(es, "msmaskb", [128, 8], BF16)
                mask_all = self.sb(es, "msmask", [128, ntile, 8], F32)
                gates_all = self.sb(es, "msgates", [128, ntile, 8], F32)
                rank_all = self.sb(es, "msrank", [128, ntile, 8], F32)
                carry = self.sb(es, "mscarry", [128, 8], F32)
                sc = self.sb(es, "mssc", [128, 16, 8], F32)
                sci = self.sb(es, "mssci", [128, 8], I32)
                be = self.sb(es, "msbe", [128, NB], F32)
                idx_f = self.sb(es, "msidxf", [128, NB, 7], F32)
                slots_f = self.sb(es, "msslotsf", [128, ntile, 2], F32)
                slotsR_i = self.sb(es, "msslotsRi", [128, ntile, 2], I32)
                eoff = self.sb(es, "mseoff", [128, NEXP], F32)
                bidx = self.sb(es, "msbidx", [128, NB], F32)
                sbk = self.sb(es, "mssbk", [128, NB], F32)
                idxX_f = self.sb(es, "msidxXf", [128, NB, 4], F32)
                sr = self.sb(es, "mssr", [128, 4, 8], F32)
                t_sr = Track()
                P.dma("pool", rw[:], I["router_wT"], writes=[t_par])
                P.dma("pool", utri[:], I["utri"], writes=[t_par])
                P.dma("sp", rbb[:], I["router_b"].partition_broadcast(128), writes=[t_par])
                P.dma("sp", iota[:], I["iota_p"], writes=[t_par])
                P.dma("sp", eoff[:], I["eoff"], writes=[t_par])
                P.dma("sp", bidx[:], I["bidx"], writes=[t_par])
                P.op("dve", lambda e: e.memset(onesb[:], 1.0), [], [t_par])
                P.op("dve", lambda e: e.memset(zer[:], 0.0), [], [t_par])
                P.op("dve", lambda e: e.memset(carry[:], 0.0), [], [t_rt])
                def stage_a(j, td):
                    hT, t_hT = hTt[j % 2], t_hTt[j % 2]
                    i = B["nx"] % 2
                    xt, t_xt = B["xt"][i], B["t_xt"][i]
                    P.dma("sp", xt[:], td["src"], reads=[td["t_src"]], writes=[t_xt])
                    self.norm_to_hT(B, xt, t_xt, M["gs2L"], M["modL"][:, 24:32], M["t"], hT[:, :, :], t_hT, 7)
                    yield

                def stage_b(j, td):
                    hT, t_hT = hTt[j % 2], t_hTt[j % 2]
                    pl, tpl = self.ps[6], self.t_ps[6]
                    for k in range(8):
                        self.mm(pl[:, 0:8], hT[:, k, :], rw[:, k, :], k == 0, k == 7, [t_hT, t_par], [tpl])
                    lg, top, ex, msk = rs[:, 0, :], rs[:, 1, :], rs[:, 2, :], mask_all[:, j, :]
                    self.tt("dve", lg, pl[:, 0:8], rbb[:], ALU.add, [tpl, t_par], [t_rs])
                    P.op("dve", lambda e_, top=top, lg=lg: e_.max(out=top, in_=lg), [t_rs], [t_rs])
                    self.ts("dve", rs[:, 3, 0:1], top[:, 0:1], -1.0, None, ALU.mult, None, [t_rs], [t_rs])
                    self.act(ex, lg, AF.Exp, [t_rs], [t_rs], bias=rs[:, 3, 0:1])
                    self.ts("dve", msk, lg, top[:, 1:2], None, ALU.is_ge, None, [t_rs], [t_rt])
                    self.tt("dve", ex, ex, msk, ALU.mult, [t_rs, t_rt], [t_rs])
                    P.op("dve", lambda e_, ex=ex: e_.reduce_sum(out=rs[:, 3, 1:2], in_=ex, axis=mybir.AxisListType.X), [t_rs], [t_rs])
                    P.op("dve", lambda e_: e_.reciprocal(out=rs[:, 3, 1:2], in_=rs[:, 3, 1:2]), [t_rs], [t_rs])
                    self.ts("dve", gates_all[:, j, :], ex, rs[:, 3, 1:2], None, ALU.mult, None, [t_rs], [t_rt])
                    yield
                    self.copy("dve", maskb[:], msk, [t_rt], [t_rs])
                    pr, tpr = self.ps[5], self.t_ps[5]
                    self.mm(pr[:, 0:8], utri[:], maskb[:], True, True, [t_rs, t_par], [tpr], signal=False)
                    self.mm(pr[:, 8:16], onesb[:], maskb[:], True, True, [t_rs, t_par], [tpr])
                    self.tt("dve", rank_all[:, j, :], pr[:, 0:8], carry[:], ALU.add, [tpr, t_rt], [t_rt])
                    self.tt("dve", carry[:], pr[:, 8:16], carry[:], ALU.add, [tpr, t_rt], [t_rt])
                    kr, m8r = sr[:, 0, :], sr[:, 1, :]
                    self.tt("dve", kr, rank_all[:, j, :], eoff[:], ALU.add, [t_rt, t_par], [t_sr])
                    self.stt(kr, kr, 1.0, msk, ALU.add, ALU.mult, [t_sr, t_rt], [t_sr])
                    P.op("dve", lambda e_, m8r=m8r, kr=kr: e_.max(out=m8r, in_=kr), [t_sr], [t_sr])
                    self.ts("dve", sr[:, 2, 0:2], m8r[:, 0:2], -1.0, None, ALU.add, None, [t_sr], [t_sr])
                    t_sx = Track()
                    self.copy("dve", slotsR_i[:, j, :], sr[:, 2, 0:2], [t_sr], [t_sx])
                    t_scat.append(t_sx)
                    yield
                    pT, tpT = self.ps[4], self.t_ps[4]
                    pv = pT[:, :].bitcast(BF16)
                    for k in range(8):
                        self.tr(pv[:, k * 128:(k + 1) * 128], hT[:, k, :], [t_hT], [tpT], signal=(k == 7))
                    self.copy("act", h2tok[:, j, :], pv[:, :], [tpT], [t_h2tok[j]])
                    for kk in range(2):
                        P.idma(Xs, IO(ap=slotsR_i[:, j, kk:kk + 1], axis=0), h2tok[:, j, :], None,
                               reads=[t_scat[j], t_h2tok[j]], writes=[t_Xw[j * 2 + kk]])
                    yield

                t_scat = []
                self.pipeline([(stage_a(j, td), stage_b(j, td)) for j, td in enumerate(tiles)])
                nbk, pend, pst = sc[:, 0, :], sc[:, 1, :], sc[:, 2, :]
                vq = sc[:, 7, :]
                self.ts("dve", vq, carry[:], 511.0, 1.0 / 512.0, ALU.add, ALU.mult, [t_rt], [t_rt])
                self.copy("dve", sci[:], vq, [t_rt], [t_rt])
                self.copy("dve", nbk, sci[:], [t_rt], [t_rt])
                self.tt("dve", sc[:, 8, :], nbk, vq, ALU.is_gt, [t_rt], [t_rt])
                self.tt("dve", nbk, nbk, sc[:, 8, :], ALU.subtract, [t_rt], [t_rt])
                self.copy("dve", pend[:, 0:1], nbk[:, 0:1], [t_rt], [t_rt])
                for e in range(1, NEXP):
                    self.tt("dve", pend[:, e:e + 1], pend[:, e - 1:e], nbk[:, e:e + 1], ALU.add, [t_rt], [t_rt])
                self.tt("dve", pst, pend, nbk, ALU.subtract, [t_rt], [t_rt])
                self.ts("dve", pst, pst, 512.0, None, ALU.mult, None, [t_rt], [t_rt])
                for b in range(NB):
                    self.ts("dve", sc[:, 3, :], pend, float(b), None, ALU.is_le, None, [t_rt], [t_rt])
                    P.op("dve", lambda e_, b=b: e_.reduce_sum(out=be[:, b:b + 1], in_=sc[:, 3, :], axis=mybir.AxisListType.X), [t_rt], [t_rt])
                    self.tt("dve", sc[:, 9, :], sc[:, 3, :], nbk, ALU.mult, [t_rt], [t_rt])
                    P.op("dve", lambda e_, b=b: e_.reduce_sum(out=sbk[:, b:b + 1], in_=sc[:, 9, :], axis=mybir.AxisListType.X), [t_rt], [t_rt])
                self.ts("dve", be[:], be[:], 7.0, None, ALU.min, None, [t_rt], [t_rt])
                self.tt("dve", sbk[:], bidx[:], sbk[:], ALU.subtract, [t_rt, t_par], [t_rt])
                self.ts("dve", sbk[:], sbk[:], 7.0, 0.0, ALU.min, ALU.max, [t_rt], [t_rt])
                self.ts("dve", sbk[:], sbk[:], 512.0, iota[:, 0:1], ALU.mult, ALU.add, [t_rt, t_par], [t_rt])
                self.stt(sbk[:], be[:], 4096.0, sbk[:], ALU.mult, ALU.add, [t_rt], [t_rt])
                for jj in range(4):
                    self.ts("dve", idxX_f[:, :, jj], sbk[:], float(jj * 128), None, ALU.add, None, [t_rt], [t_rt])
                self.copy("dve", idxX_i[:], idxX_f[:], [t_rt], [t_rt])
                self.ts("dve", be[:], be[:], 896.0, None, ALU.mult, None, [t_rt], [t_rt])
                self.ts("dve", be[:], be[:], iota[:, 0:1], None, ALU.add, None, [t_rt, t_par], [t_rt])
                for fb in range(7):
                    self.ts("dve", idx_f[:, :, fb], be[:], float(fb * 128), None, ALU.add, None, [t_rt], [t_rt])
                self.copy("dve", idx_i[:], idx_f[:], [t_rt], [t_rt])
                for u0 in range(NWB - 1):
                    load_unit(units[u0])
                t_slots = []
                for j in range(ntile):
                    key, m8, oh = sc[:, 4, :], sc[:, 5, :], sc[:, 6, :]
                    self.tt("dve", key, rank_all[:, j, :], pst, ALU.add, [t_rt], [t_rt])
                    self.stt(key, key, 1.0, mask_all[:, j, :], ALU.add, ALU.mult, [t_rt], [t_rt])
                    P.op("dve", lambda e_, m8=m8, key=key: e_.max(out=m8, in_=key), [t_rt], [t_rt])
                    self.ts("dve", slots_f[:, j, :], m8[:, 0:2], -1.0, None, ALU.add, None, [t_rt], [t_rt])
                    self.ts("dve", oh, key, m8[:, 0:1], None, ALU.is_equal, None, [t_rt], [t_rt])
                    self.tt("dve", oh, oh, gates_all[:, j, :], ALU.mult, [t_rt], [t_rt])
                    P.op("dve", lambda e_, j=j, oh=oh: e_.reduce_sum(out=ghl[:, j, 0:1], in_=oh, axis=mybir.AxisListType.X), [t_rt], [t_rt])
                    self.ts("dve", ghl[:, j, 1:2], ghl[:, j, 0:1], -1.0, 1.0, ALU.mult, ALU.add, [t_rt], [t_rt])
                    t_sl = Track()
                    self.copy("dve", slots_i[:, j, :], slots_f[:, j, :], [t_rt], [t_sl])
                    t_slots.append(t_sl)
                P.barrier()
            with ExitStack() as es:
                xtok = [self.sb(es, "mextok%d" % i, [128, D], BF16) for i in range(2)]
                t_xtok = tracks(2)
                XT = [self.sb(es, "meXT%d" % i, [128, 8, 512], BF16) for i in range(2)]
                t_XT = tracks(2)
                yblk = [self.sb(es, "meyb%d" % i, [128, 4, D], F32) for i in range(2)]
                t_yblk = [tracks(4) for _ in range(2)]
                gT = [self.sb(es, "megT%d" % i, [128, 4, 512], BF16) for i in range(2)]
                t_gT = tracks(2)
                sl = [self.sb(es, "mesl%d" % i, [128, 512], BF16) for i in range(2)]
                t_sl = tracks(2)
                nxt = [0]

                def load_x(b):
                    xs_, t_xs = XT[b % 2], t_XT[b % 2]
                    for jj in range(4):
                        i = nxt[0] % 2
                        nxt[0] += 1
                        r = (b * 4 + jj) * 128
                        P.idma(xtok[i][:], None, Xs, IO(ap=idxX_i[:, b, jj:jj + 1], axis=0), reads=[t_rt] + t_Xw, writes=[t_xtok[i]])
                        pT, tpT = self.ps[3], self.t_ps[3]
                        pv = pT[:, :].bitcast(BF16)
                        for k in range(8):
                            self.tr(pv[:, k * 128:(k + 1) * 128], xtok[i][:, k * 128:(k + 1) * 128], [t_xtok[i]], [tpT], signal=(k == 7))
                        self.copy("act", xs_[:, :, jj * 128:(jj + 1) * 128], pv[:, :].rearrange("p (k t) -> p k t", k=8), [tpT], [t_xs])

                load_x(0)
                ng = 0
                for ui, (b, fb) in enumerate(units):
                    cur = ui % NWB
                    if ui + NWB - 1 < len(units):
                        load_unit(units[ui + NWB - 1])
                    if fb == 3 and b + 1 < NB:
                        load_x(b + 1)
                    xs_, t_xs = XT[b % 2], t_XT[b % 2]
                    yb, t_yb = yblk[b % 2], t_yblk[b % 2]
                    g, t_g = gT[ng % 2], t_gT[ng % 2]
                    ng += 1
                    for fc in range(4):
                        p1, tp1 = self.ps[(fc % 2) * 2], self.t_ps[(fc % 2) * 2]
                        p3, tp3 = self.ps[(fc % 2) * 2 + 1], self.t_ps[(fc % 2) * 2 + 1]
                        for k in range(8):
                            self.mm(p1[:, :], w1b[cur][:, k, fc * 128:(fc + 1) * 128], xs_[:, k, :], k == 0, k == 7, [t_w[cur], t_xs], [tp1])
                        for k in range(8):
                            self.mm(p3[:, :], w3b[cur][:, k, fc * 128:(fc + 1) * 128], xs_[:, k, :], k == 0, k == 7, [t_w[cur], t_xs], [tp3])
                        s_, t_s = sl[fc % 2], t_sl[fc % 2]
                        self.act(s_[:, :], p1[:, :], AF.Silu, [tp1], [t_s])
                        self.tt("dve", g[:, fc, :], p3[:, :], s_[:, :], ALU.mult, [tp3, t_s], [t_g])
                    for jj in range(4):
                        for cb in range(2):
                            py, tpy = self.ps[4 + (jj % 2) * 2 + cb], self.t_ps[4 + (jj % 2) * 2 + cb]
                            for fc in range(4):
                                self.mm(py[:, :], g[:, fc, jj * 128:(jj + 1) * 128], w2b[cur][:, fc, cb * 512:(cb + 1) * 512], fc == 0, fc == 3,
                                        [t_g, t_w[cur]], [tpy])
                            ya = yb[:, jj, cb * 512:(cb + 1) * 512]
                            if fb == 0:
                                self.copy("dve", ya, py[:, :], [tpy], [t_yb[jj]])
                            else:
                                self.tt("dve", ya, py[:, :], ya, ALU.add, [tpy, t_yb[jj]], [t_yb[jj]])
                        if fb == 6:
                            r = (b * 4 + jj) * 128
                            P.dma("sp", Ys[r:r + 128, :], yb[:, jj, :], reads=[t_yb[jj]], writes=[t_Ys[b * 4 + jj]])
                P.barrier()
            with ExitStack() as es:
                yh = [self.sb(es, "mcyh%d" % i, [128, D], F32) for i in range(2)]
                yl = [self.sb(es, "mcyl%d" % i, [128, D], F32) for i in range(2)]
                xr = [self.sb(es, "mcxr%d" % i, [128, D], F32) for i in range(2)]
                t_yh, t_yl, t_xr = tracks(2), tracks(2), tracks(2)
                junk = self.sb(es, "mcjunk", [128, D], BF16)
                st = self.sb(es, "mcst", [128, 4], F32)
                t_st = Track()
                g2 = M["g2L"]
                for j, td in enumerate(tiles):
                    i = j % 2
                    P.idma(yh[i][:], None, Ys, IO(ap=slots_i[:, j, 0:1], axis=0), reads=[t_rt, t_slots[j]] + t_Ys, writes=[t_yh[i]])
                    P.idma(yl[i][:], None, Ys, IO(ap=slots_i[:, j, 1:2], axis=0), reads=[t_rt, t_slots[j]] + t_Ys, writes=[t_yl[i]])
                    P.dma("sp", xr[i][:], td["src"], reads=[td["t_src"]], writes=[t_xr[i]])
                    self.ts("dve", yh[i][:], yh[i][:], ghl[:, j, 0:1], None, ALU.mult, None, [t_yh[i], t_rt], [t_yh[i]])
                    self.stt(yh[i][:], yl[i][:], ghl[:, j, 1:2], yh[i][:], ALU.mult, ALU.add, [t_yl[i], t_yh[i], t_rt], [t_yh[i]])
                    self.tt("dve", yh[i][:], yh[i][:], g2[:], ALU.mult, [t_yh[i], M["t"]], [t_yh[i]])
                    self.tt("dve", yh[i][:], yh[i][:], xr[i][:], ALU.add, [t_yh[i], t_xr[i]], [t_yh[i]])
                    col = st[:, i:i + 1]
                    self.act(junk[:], yh[i][:], AF.Square, [t_yh[i]], [t_st], accum_out=col)
                    self.rstd_from(col, col, 1.0 / D, [t_st], t_st)
                    self.stt(yh[i][:], yh[i][:], col, fing[:], ALU.mult, ALU.mult, [t_yh[i], t_st, t_par], [t_yh[i]])
                    P.dma("sp", td["dst"], yh[i][:], reads=[t_yh[i]], writes=[td["t_dst"]])
                P.barrier()

    def build(self):
        nc = self.nc
        with ExitStack() as es:
            self.setup(es)
            P, I = self.P, self.I
            segL, segC = self.segL, self.segC
            groups = [[t for t in range(4 * s, 4 * s + 4)] for s in range(10)]
            for l in range(2):
                with ExitStack() as esl:
                    M = self.mod_phase(l, esl)
                    if STOP_AFTER == "mod":
                        break
                    last = (l == 1)
                    xsrc, t_xsrc = (I["x_ext"], None) if l == 0 else (self.xl1, self.t_xl1)
                    csrc, t_csrc = (I["ctxb"], None) if l == 0 else (self.xc1, self.t_xc1)
                    lo, hi = (0, NT0) if l == 0 else (EXT_LO, EXT_HI)
                    with ExitStack() as es1:
                        B = self.p1_alloc(es1, l)
                        self.phase1(B, segC, [0, 1], csrc, t_csrc, M["gs1C"], M["modC"][:, 0:8], M["t"], self.one1[:, 0:1], kv_only=last)
                        parts = []
                        for s, g in enumerate(groups):
                            tl = [t for t in g if lo <= t < hi]
                            if tl:
                                parts.append(self.phase1_parts(B, segL, tl, xsrc, t_xsrc, M["gs1L"], M["modL"][:, 0:8], M["t"], self.valid[:, s:s + 1]))
                        self.pipeline(parts)
                        P.barrier()
                    if STOP_AFTER == "l%dp1" % l:
                        break
                    lo2, hi2 = (EXT_LO, EXT_HI) if l == 0 else (OWN_LO, OWN_HI)
                    with ExitStack() as es2:
                        B = self.p2_alloc(es2, l)
                        self.table_prep(B, 0, B["TG"], B["t_TG"])
                        self.load_ctx_kv(B)
                        if not last:
                            self.phase2(B, segC, [0, 1], True, csrc, t_csrc, self.xcmid, self.t_xcmid, (M["g1C"], M["t"]))
                        parts = []
                        for g in groups:
                            tl = [t for t in g if lo2 <= t < hi2]
                            if tl:
                                parts.append(self.phase2_parts(B, segL, tl, False, xsrc, t_xsrc, self.xmid, self.t_xmid, (M["g1L"], M["t"])))
                        self.pipeline(parts)
                        P.barrier()
                    if STOP_AFTER == "l%dp2" % l:
                        break
                    tiles = []
                    for t in range(lo2, hi2):
                        if last:
                            dst, t_dst = self.out[(t - OWN_LO) * 128:(t - OWN_LO + 1) * 128, :], self.t_out[t - OWN_LO]
                        else:
                            dst, t_dst = self.xl1[t * 128:(t + 1) * 128, :], self.t_xl1[t]
                        tiles.append({"src": self.xmid[t * 128:(t + 1) * 128, :], "t_src": self.t_xmid[t], "dst": dst, "t_dst": t_dst,
                                      "mod": "L", "final": last})
                    if not last:
                        for t in range(2):
                            tiles.append({"src": self.xcmid[t * 128:(t + 1) * 128, :], "t_src": self.t_xcmid[t],
                                          "dst": self.xc1[t * 128:(t + 1) * 128, :], "t_dst": self.t_xc1[t], "mod": "C", "final": False})
                    blocks = [tiles[i:i + 8] for i in range(0, len(tiles), 8)]
                    if not last:
                        experts = [(I["ffn_w1"], I["ffn_w3"], I["ffn_w2"], DFF // 128)]
                        self.ffn_phase(l, M, blocks, experts, False)
                    else:
                        self.moe_sparse(M, tiles)
                    if STOP_AFTER == "l%d" % l:
                        break
            P.barrier()
        return nc


def build_program():
    nc = bass.Bass("TRN2", target_bir_lowering=False)
    kb = KB(nc)
    kb.t_out = tracks(32)
    kb.build()
    return nc, kb


def kernel(**inputs):
    maps = _prep_inputs(inputs)
    ident = np.eye(128, dtype=np.float32)
    for m in maps:
        m["ident"] = ident
        if STOP_AFTER is not None:
            for k in ("moe_wr",):
                m[k] = np.zeros((128, 128), np.float32)
    nc, kb = build_program()
    res = run_bass_kernel_spmd(nc, maps, core_ids=list(range(NCORES)))
    out = np.zeros((2, 16384, D), np.float32)
    for core in range(NCORES):
        b, R0 = core // 4, 64 * (core % 4)
        out[b, R0 * 64:(R0 + 64) * 64] = res.results[core]["out"]
    kernel.last_results = res.results
    return out
```

```python
import numpy as np
from contextlib import ExitStack
import concourse.bass as bass
import concourse.mybir as mybir
from concourse.bass_utils import run_bass_kernel_spmd

F32 = mybir.dt.float32
BF16 = mybir.dt.bfloat16
I32 = mybir.dt.int32
AF = mybir.ActivationFunctionType
ALU = mybir.AluOpType

NCORES = 8
D = 1024
NT0 = 40
PADF, PADB = 1, 3
TT = PADF + NT0 + PADB
EXT_LO, EXT_HI = 2, 38
OWN_LO, OWN_HI = 4, 36
EPS = 1e-6
DFF = 2816
DFE = 3584
NEXP = 8
NEG = -1.0e4
DEBUG_OUT = False
DBG_P2 = 9
STOP_AFTER = None


class Track:
    __slots__ = ("w", "r")

    def __init__(self):
        self.w = None
        self.r = []


def tracks(n):
    return [Track() for _ in range(n)]


class Prog:
    def __init__(self, nc, es):
        self.nc = nc
        self.E = {"pe": nc.tensor, "act": nc.scalar, "dve": nc.vector, "pool": nc.gpsimd, "sp": nc.sync}
        self.sem = {}
        self.cnt = {}
        for e in ("pe", "act", "dve", "pool"):
            self.sem[e] = es.enter_context(nc.semaphore("s_" + e))
            self.cnt[e] = 0
        self.dq = {}
        for q, k in (("sp", 16), ("pool", 12), ("act", 6)):
            sems = [es.enter_context(nc.semaphore("d_%s%d" % (q, i))) for i in range(k)]
            self.dq[q] = {"sems": sems, "n": 0}
            for i, s in enumerate(sems):
                self.sem[("d", q, i)] = s
        self.waited = {e: {} for e in self.E}
        self.pending = {e: [] for e in self.E}
        self.ninst = 0

    def _deps(self, reads, writes):
        evs = []
        for t in reads:
            if t.w is not None:
                evs.append(t.w)
        for t in writes:
            if t.w is not None:
                evs.append(t.w)
            evs.extend(t.r)
        return evs

    def _wait(self, eng, evs):
        need = {}
        for (k, v) in evs:
            if k == eng and eng == "pe":
                continue
            if v > need.get(k, 0):
                need[k] = v
        w = self.waited[eng]
        for k, v in need.items():
            if w.get(k, 0) >= v:
                continue
            w[k] = v
            self.E[eng].wait_ge(self.sem[k], v)

    def op(self, eng, fn, reads=(), writes=(), signal=True):
        self._wait(eng, self._deps(reads, writes))
        inst = fn(self.E[eng])
        self.ninst += 1
        if signal:
            self.cnt[eng] += 1
            inst.then_inc(self.sem[eng], 1)
            ev = (eng, self.cnt[eng])
            for (rs, ws) in self.pending[eng] + [(reads, writes)]:
                for t in rs:
                    t.r.append(ev)
                for t in ws:
                    t.w = ev
                    t.r = []
            self.pending[eng] = []
        else:
            self.pending[eng].append((tuple(reads), tuple(writes)))
        return inst

    def dma(self, q, out, in_, reads=(), writes=()):
        pool = self.dq[q]
        i = pool["n"]
        k = len(pool["sems"])
        idx, gen = i % k, i // k
        key = ("d", q, idx)
        evs = self._deps(reads, writes)
        if gen > 0:
            evs.append((key, 16 * gen))
        self._wait(q, evs)
        inst = self.E[q].dma_start(out=out, in_=in_)
        inst.then_inc(pool["sems"][idx], 16)
        pool["n"] += 1
        self.ninst += 1
        ev = (key, 16 * (gen + 1))
        for t in reads:
            t.r.append(ev)
        for t in writes:
            t.w = ev
            t.r = []

    def idma(self, out, out_offset, in_, in_offset, reads=(), writes=()):
        q = "pool"
        pool = self.dq[q]
        i = pool["n"]
        k = len(pool["sems"])
        idx, gen = i % k, i // k
        key = ("d", q, idx)
        evs = self._deps(reads, writes)
        if gen > 0:
            evs.append((key, 16 * gen))
        self._wait(q, evs)
        inst = self.E[q].indirect_dma_start(out=out, out_offset=out_offset, in_=in_, in_offset=in_offset)
        inst.then_inc(pool["sems"][idx], 16)
        pool["n"] += 1
        self.ninst += 1
        ev = (key, 16 * (gen + 1))
        for t in reads:
            t.r.append(ev)
        for t in writes:
            t.w = ev
            t.r = []

    def barrier(self):
        for e in self.pending:
            assert not self.pending[e], "pending unsignaled instructions at barrier"
        evs = [(e, self.cnt[e]) for e in ("pe", "act", "dve", "pool") if self.cnt[e] > 0]
        for q, pool in self.dq.items():
            k = len(pool["sems"])
            for idx in range(k):
                n = (pool["n"] - idx + k - 1) // k
                if n > 0:
                    evs.append((("d", q, idx), 16 * n))
        for eng in self.E:
            self._wait(eng, [ev for ev in evs if ev[0] != eng])


def _pcol(v, nch):
    return np.ascontiguousarray(np.asarray(v, np.float32).reshape(nch, 128).T)


def _bias_tables(rpb_l, R0):
    out = np.full((5, 6, 128, 4, 128), NEG, np.float32)
    specs = [(100, 96)] + [(R0 + 0, R0 - 4), (R0 + 2, R0 - 2), (R0 + 60, R0 + 56), (R0 + 62, R0 + 56)]
    kk = np.arange(768)
    jr, kc = kk // 64, kk % 64
    qq = np.arange(128)
    a, qc = qq // 64, qq % 64
    c_start = np.clip(qc - 8, 0, 48)
    col_ok = (kc[:, None] >= c_start[None, :]) & (kc[:, None] < c_start[None, :] + 16)
    dc = np.clip(kc[:, None] - qc[None, :], -15, 15) + 15
    for s, (r, ws) in enumerate(specs):
        rk = ws + jr
        rq = r + a
        lo = np.clip(rq - 4, 0, 248)
        ok = (rk[:, None] >= lo[None, :]) & (rk[:, None] < lo[None, :] + 8) & (rk[:, None] >= 0) & (rk[:, None] < 256)
        ok = ok & col_ok
        dr = np.clip(rk[:, None] - rq[None, :] + 7, 0, 14)
        for h in range(4):
            b = rpb_l[h][dr, dc]
            t = np.where(ok, b, np.float32(NEG)).astype(np.float32)
            out[s, :, :, h, :] = t.reshape(6, 128, 128)
    return out


def _prep_inputs(inp):
    f = lambda k: np.asarray(inp[k], np.float32)
    x, c, ctx, c_ctx = f("x"), f("c"), f("ctx"), f("c_ctx")
    shared = {}
    shared["w_mod"] = f("w_mod")
    shared["b_mod"] = f("b_mod")
    shared["b_modT"] = np.stack([_pcol(f("b_mod")[l], 48) for l in range(2)])
    shared["n1gT"] = np.stack([_pcol(f("norm1_g")[l], 8) for l in range(2)])
    shared["n2gT"] = np.stack([_pcol(f("norm2_g")[l], 8) for l in range(2)])
    shared["w_in"] = f("w_in")
    caw = f("conv_a_w")
    shared["cawT"] = np.ascontiguousarray(caw.reshape(2, 3, 2, 128).transpose(0, 3, 2, 1))
    cbw = f("conv_b_w")
    shared["cbwT"] = np.ascontiguousarray(cbw.reshape(2, 31, 2, 128).transpose(0, 3, 2, 1))
    for nm, key in (("cbbT", "conv_b_b"), ("clgT", "conv_ln_g"), ("clbT", "conv_ln_b")):
        shared[nm] = np.stack([_pcol(f(key)[l], 2) for l in range(2)])
    shared["sgu_ln_g"] = f("sgu_ln_g")
    shared["sgu_ln_b"] = f("sgu_ln_b")
    shared["sgu_wT"] = np.ascontiguousarray(f("sgu_w").transpose(0, 3, 1, 2))
    shared["sgu_bT"] = np.ascontiguousarray(f("sgu_b").transpose(0, 2, 1))
    shared["ggT"] = np.stack([_pcol(f("group_g")[l], 8) for l in range(2)])
    shared["w_out"] = f("w_out")
    shared["ffn_w1"] = f("ffn_w1")[0]
    shared["ffn_w3"] = f("ffn_w3")[0]
    shared["ffn_w2"] = f("ffn_w2")[0]
    shared["router_wT"] = np.ascontiguousarray(f("router_w")[0].reshape(8, 128, 8).transpose(1, 0, 2))
    shared["router_b"] = f("router_b")[0]
    wr = np.empty((NEXP * 7 * 128, 3 * 4096), np.float32)
    for i, nm in enumerate(("moe_w1", "moe_w3")):
        w = f(nm)[0].reshape(NEXP, 8, 128, 7, 512)
        wr[:, i * 4096:(i + 1) * 4096] = w.transpose(0, 3, 2, 1, 4).reshape(NEXP * 7 * 128, 4096)
    w = f("moe_w2")[0].reshape(NEXP, 7, 4, 128, D)
    wr[:, 8192:12288] = w.transpose(0, 1, 3, 2, 4).reshape(NEXP * 7 * 128, 4096)
    shared["moe_wr"] = wr
    shared["iota_p"] = np.arange(128, dtype=np.float32).reshape(128, 1)
    shared["utri"] = np.triu(np.ones((128, 128), np.float32), 1)
    shared["final_g"] = f("final_g")
    rpb = f("rpb")
    maps = []
    for core in range(NCORES):
        b, R0 = core // 4, 64 * (core % 4)
        m = dict(shared)
        xe = np.zeros((NT0 * 128, D), np.float32)
        lo, hi = R0 - 8, R0 + 72
        glo, ghi = max(lo, 0), min(hi, 256)
        xe[(glo - lo) * 64:(ghi - lo) * 64] = x[b, glo * 64:ghi * 64]
        m["x_ext"] = xe
        m["ctxb"] = np.ascontiguousarray(ctx[b])
        m["cT"] = np.ascontiguousarray(np.stack([_pcol(c[b], 8), _pcol(c_ctx, 8)], axis=-1))
        val = np.zeros((128, 10), np.float32)
        for s in range(10):
            r = lo + 8 * s
            val[:, s] = 1.0 if (0 <= r < 256) else 0.0
        m["valid"] = val
        m["btab"] = np.stack([_bias_tables(rpb[l], R0) for l in range(2)])
        maps.append(m)
    return maps


class Seg:
    def __init__(self, nc, name, ntiles, padf, padb):
        self.ntiles, self.padf, self.padb = ntiles, padf, padb
        self.tt = padf + ntiles + padb
        self.arr, self.trk = {}, {}
        for k in ("bg", "uA", "yB", "q", "k", "ynC"):
            self.arr[k] = nc.dram_tensor("%s_%s" % (name, k), [128, 2, self.tt * 128], BF16, kind="Internal").ap()
            self.trk[k] = tracks(self.tt)
        self.arr["v"] = nc.dram_tensor("%s_v" % name, [self.tt, 128, 264], BF16, kind="Internal").ap()
        self.trk["v"] = tracks(self.tt)

    def cols(self, t0, n):
        a = (t0 + self.padf) * 128
        return slice(a, a + n)

    def tr(self, k, t0, t1):
        return self.trk[k][t0 + self.padf:t1 + self.padf]


class KB:
    def __init__(self, nc):
        self.nc = nc

    def act(self, out, in_, func, reads, writes, bias=None, scale=None, accum_out=None):
        kw = {}
        if bias is not None:
            kw["bias"] = bias
        if scale is not None:
            kw["scale"] = scale
        if accum_out is not None:
            kw["accum_out"] = accum_out
        return self.P.op("act", lambda e: e.activation(out=out, in_=in_, func=func, **kw), reads, writes)

    def tt(self, eng, out, in0, in1, op, reads, writes):
        return self.P.op(eng, lambda e: e.tensor_tensor(out=out, in0=in0, in1=in1, op=op), reads, writes)

    def ts(self, eng, out, in0, s1, s2, op0, op1, reads, writes):
        if op1 is None:
            return self.P.op(eng, lambda e: e.tensor_scalar(out=out, in0=in0, scalar1=s1, scalar2=None, op0=op0), reads, writes)
        return self.P.op(eng, lambda e: e.tensor_scalar(out=out, in0=in0, scalar1=s1, scalar2=s2, op0=op0, op1=op1), reads, writes)

    def stt(self, out, in0, scalar, in1, op0, op1, reads, writes):
        return self.P.op("dve", lambda e: e.scalar_tensor_tensor(out=out, in0=in0, scalar=scalar, in1=in1, op0=op0, op1=op1), reads, writes)

    def copy(self, eng, out, in_, reads, writes):
        if eng == "act":
            return self.P.op("act", lambda e: e.activation(out=out, in_=in_, func=AF.Copy), reads, writes)
        return self.P.op(eng, lambda e: e.tensor_copy(out=out, in_=in_), reads, writes)

    def mm(self, out, lhsT, rhs, start, stop, reads, writes, signal=None, sgc=False):
        if signal is None:
            signal = stop
        return self.P.op("pe", lambda e: e.matmul(out, lhsT=lhsT, rhs=rhs, start=start, stop=stop, skip_group_check=sgc),
                         reads, writes, signal=signal)

    def tr(self, out, in_, reads, writes, signal=True):
        ident = self.ident[:]
        return self.P.op("pe", lambda e: e.transpose(out, in_, ident), list(reads) + [self.t_const], writes, signal=signal)

    def sb(self, es, name, shape, dt):
        self._uid = getattr(self, "_uid", 0) + 1
        return es.enter_context(self.nc.sbuf_tensor("%s_u%d" % (name, self._uid), shape, dt))

    def rstd_from(self, dst, src, scale, reads_t, t_dst):
        self.act(dst, src, AF.Ln, reads_t + [self.t_const], [t_dst], bias=self.eps[:, 0:1], scale=scale)
        self.act(dst, dst, AF.Exp, [t_dst], [t_dst], scale=-0.5)

    def setup(self, es):
        nc = self.nc
        self.P = Prog(nc, es)
        P = self.P
        di = lambda name, shape: nc.dram_tensor(name, list(shape), F32, kind="ExternalInput").ap()
        I = {}
        for name, shape in (
            ("x_ext", [NT0 * 128, D]), ("ctxb", [256, D]), ("cT", [128, 8, 2]), ("valid", [128, 10]),
            ("btab", [2, 5, 6, 128, 4, 128]), ("w_mod", [2, D, 6 * D]), ("b_mod", [2, 6 * D]), ("b_modT", [2, 128, 48]),
            ("n1gT", [2, 128, 8]), ("n2gT", [2, 128, 8]), ("w_in", [2, D, 2560]), ("cawT", [2, 128, 2, 3]),
            ("cbwT", [2, 128, 2, 31]), ("cbbT", [2, 128, 2]), ("clgT", [2, 128, 2]), ("clbT", [2, 128, 2]),
            ("sgu_ln_g", [2, 256]), ("sgu_ln_b", [2, 256]), ("sgu_wT", [2, 128, 4, 128]), ("sgu_bT", [2, 128, 4]),
            ("ggT", [2, 128, 8]), ("w_out", [2, D, D]), ("ffn_w1", [D, DFF]), ("ffn_w3", [D, DFF]), ("ffn_w2", [DFF, D]),
            ("router_wT", [128, 8, 8]), ("router_b", [8]), ("moe_wr", [NEXP * 7 * 128, 3 * 4096]), ("final_g", [D]), ("ident", [128, 128]), ("iota_p", [128, 1]), ("utri", [128, 128]),
        ):
            if STOP_AFTER is not None and name.startswith("moe_w"):
                shape = [128, 128]
            I[name] = di(name, shape)
        self.I = I
        self.out = nc.dram_tensor("out", [32 * 128, D], F32, kind="ExternalOutput").ap()
        kind = "ExternalOutput" if DEBUG_OUT else "Internal"
        self.xmid = nc.dram_tensor("xmid", [NT0 * 128, D], F32, kind=kind).ap()
        self.xl1 = nc.dram_tensor("xl1", [NT0 * 128, D], F32, kind=kind).ap()
        self.xcmid = nc.dram_tensor("xcmid", [256, D], F32, kind=kind).ap()
        self.xc1 = nc.dram_tensor("xc1", [256, D], F32, kind=kind).ap()
        self.t_xmid, self.t_xl1 = tracks(NT0), tracks(NT0)
        self.t_xcmid, self.t_xc1 = tracks(2), tracks(2)
        self.segL = Seg(nc, "L", NT0, PADF, PADB)
        self.segC = Seg(nc, "C", 2, 1, 1)
        self.psall = es.enter_context(nc.psum_tensor("psall", [128, 4096], F32))
        self.ps = [self.psall[:, i * 512:(i + 1) * 512] for i in range(8)]
        self.t_ps = tracks(8)
        self.t_const = Track()
        self.ident = self.sb(es, "ident_sb", [128, 128], BF16)
        self.onesM = self.sb(es, "onesM", [128, 128], BF16)
        self.eps = self.sb(es, "eps", [128, 1], F32)
        self.one1 = self.sb(es, "one1", [128, 1], F32)
        self.zeros = self.sb(es, "zeros", [128, 1024], BF16)
        self.valid = self.sb(es, "valid_sb", [128, 10], F32)
        self.scT = self.sb(es, "scT", [128, 8, 2], BF16)
        self.screp = self.sb(es, "screp", [128, 2, 8, 128], BF16)
        cTf = self.sb(es, "cTf", [128, 8, 2], F32)
        tc = self.t_const
        P.dma("pool", self.ident[:], I["ident"], writes=[tc])
        P.dma("sp", self.valid[:], I["valid"], writes=[tc])
        P.dma("sp", cTf[:], I["cT"], writes=[tc])
        P.op("dve", lambda e: e.memset(self.onesM[:], 1.0 / 256.0), [], [tc])
        P.op("dve", lambda e: e.memset(self.eps[:], EPS), [], [tc])
        P.op("dve", lambda e: e.memset(self.one1[:], 1.0), [], [tc])
        P.op("dve", lambda e: e.memset(self.zeros[:], 0.0), [], [tc])
        self.act(self.scT[:], cTf[:], AF.Silu, [tc], [tc])
        for j in range(2):
            for k in range(8):
                self.copy("dve", self.screp[:, j, k, :], self.scT[:, k, j:j + 1].to_broadcast([128, 128]), [tc], [tc])
        NB = self.NBLK
        self.moe_Xs = nc.dram_tensor("moe_xs", [NB * 512, D], BF16, kind="Internal").ap()
        self.moe_Ys = nc.dram_tensor("moe_ys", [NB * 512, D], F32, kind="Internal").ap()
        self.t_moe_Xs = Track()
        for seg in (self.segL, self.segC):
            for k in ("uA", "yB", "k"):
                a = seg.arr[k]
                for c in range(2):
                    P.dma("sp", a[:, c, 0:seg.padf * 128], self.zeros[:, 0:seg.padf * 128], reads=[tc], writes=seg.trk[k][0:seg.padf])
                    b0 = (seg.padf + seg.ntiles) * 128
                    P.dma("sp", a[:, c, b0:b0 + seg.padb * 128], self.zeros[:, 0:seg.padb * 128], reads=[tc], writes=seg.trk[k][seg.padf + seg.ntiles:])
            va = seg.arr["v"]
            for t in list(range(seg.padf)) + list(range(seg.padf + seg.ntiles, seg.tt)):
                P.dma("sp", va[t], self.zeros[:, 0:264], reads=[tc], writes=[seg.trk["v"][t]])

    def mod_phase(self, l, es_layer):
        P, I = self.P, self.I
        M = {}
        for nm in ("modL", "modC"):
            M[nm] = self.sb(es_layer, "%s_%d" % (nm, l), [128, 48], F32)
        for nm in ("gs1L", "gs2L", "gs1C", "gs2C"):
            M[nm] = self.sb(es_layer, "%s_%d" % (nm, l), [128, 8], F32)
        for nm in ("g1L", "g2L", "g1C", "g2C"):
            M[nm] = self.sb(es_layer, "%s_%d" % (nm, l), [128, D], F32)
        t_mod = Track()
        M["t"] = t_mod
        tc = self.t_const
        with ExitStack() as es:
            wm = [self.sb(es, "wm%d" % i, [128, 8, 512], BF16) for i in range(2)]
            t_wm = tracks(2)
            bmb = self.sb(es, "bmb", [128, 2, D], F32)
            bmT = self.sb(es, "bmT", [128, 48], F32)
            ngT = self.sb(es, "ngT", [128, 2, 8], F32)
            t_misc = Track()
            P.dma("sp", bmb[:, 0, :], I["b_mod"][l, 2048:3072].partition_broadcast(128), writes=[t_misc])
            P.dma("sp", bmb[:, 1, :], I["b_mod"][l, 5120:6144].partition_broadcast(128), writes=[t_misc])
            P.dma("sp", bmT[:], I["b_modT"][l], writes=[t_misc])
            P.dma("sp", ngT[:, 0, :], I["n1gT"][l], writes=[t_misc])
            P.dma("sp", ngT[:, 1, :], I["n2gT"][l], writes=[t_misc])
            wsrc = I["w_mod"][l].rearrange("(kc p) n -> p kc n", p=128)
            psm, t_psm = self.ps[0], self.t_ps[0]
            psm_v = psm[:, 0:96].rearrange("p (n j) -> p n j", j=2)
            bi = 0
            for nb in range(12):
                w, tw = wm[nb % 2], t_wm[nb % 2]
                P.dma("pool", w[:], wsrc[:, :, nb * 512:(nb + 1) * 512], writes=[tw])
                for j in range(4):
                    n = nb * 4 + j
                    for k in range(8):
                        self.mm(psm_v[:, n, :], w[:, k, j * 128:(j + 1) * 128], self.scT[:, k, :], k == 0, k == 7,
                                [tw, tc], [t_psm], signal=(k == 7 and j == 3))
                if nb in (4, 5, 10, 11):
                    gi, half = (0 if nb < 6 else 1), nb % 2
                    for ci, nm in enumerate(("L", "C")):
                        pb, tpb = self.ps[1 + bi % 2], self.t_ps[1 + bi % 2]
                        bi += 1
                        for k in range(8):
                            self.mm(pb[:, :], self.screp[:, ci, k, :], w[:, k, :], k == 0, k == 7, [tw, tc], [tpb])
                        dst = M["g%d%s" % (gi + 1, nm)]
                        self.tt("dve", dst[:, half * 512:(half + 1) * 512], pb[:, :], bmb[:, gi, half * 512:(half + 1) * 512],
                                ALU.add, [tpb, t_misc], [t_mod])
            self.tt("dve", M["modL"][:], psm_v[:, :, 0], bmT[:], ALU.add, [t_psm, t_misc], [t_mod])
            self.tt("dve", M["modC"][:], psm_v[:, :, 1], bmT[:], ALU.add, [t_psm, t_misc], [t_mod])
            for nm in ("L", "C"):
                for i, (gs, off) in enumerate((("gs1", 8), ("gs2", 32))):
                    dst = M[gs + nm]
                    self.ts("dve", dst[:], M["mod" + nm][:, off:off + 8], 1.0, None, ALU.add, None, [t_mod], [t_mod])
                    self.tt("dve", dst[:], dst[:], ngT[:, i, :], ALU.mult, [t_mod, t_misc], [t_mod])
            P.barrier()
        return M

    def p1_alloc(self, es, l):
        P, I = self.P, self.I
        B = {}
        B["w_in"] = self.sb(es, "w_in_sb", [128, 8, 2560], BF16)
        B["t_w"] = Track()
        wsrc = I["w_in"][l].rearrange("(kc p) n -> p kc n", p=128)
        for cb in range(5):
            P.dma("pool", B["w_in"][:, :, cb * 512:(cb + 1) * 512], wsrc[:, :, cb * 512:(cb + 1) * 512], writes=[B["t_w"]])
        B["xt"] = [self.sb(es, "p1xt%d" % i, [128, D], F32) for i in range(2)]
        B["t_xt"] = tracks(2)
        B["junk"] = self.sb(es, "p1junk", [128, D], BF16)
        B["xn"] = [self.sb(es, "p1xn%d" % i, [128, D], BF16) for i in range(2)]
        B["t_xn"] = tracks(2)
        B["st"] = self.sb(es, "p1st", [128, 16], F32)
        B["t_st"] = Track()
        B["hT"] = [self.sb(es, "p1hT%d" % i, [128, 8, 512], BF16) for i in range(3)]
        B["t_hT"] = tracks(3)
        for nm in ("bg", "uA", "yB", "q", "k", "ynC"):
            B[nm] = [self.sb(es, "p1%s%d" % (nm, i), [128, 2, 512], BF16) for i in range(2)]
            B["t_" + nm] = tracks(2)
        B["v"] = [self.sb(es, "p1v%d" % i, [128, 4, 4, 66], BF16) for i in range(2)]
        B["t_v"] = tracks(2)
        B["cg"] = [self.sb(es, "p1cg%d" % i, [128, 512], F32) for i in range(2)]
        B["t_cg"] = tracks(2)
        B["sig"] = [self.sb(es, "p1sig%d" % i, [128, 512], BF16) for i in range(2)]
        B["t_sig"] = tracks(2)
        B["zc"] = [self.sb(es, "p1zc%d" % i, [128, 512], F32) for i in range(4)]
        B["t_zc"] = tracks(4)
        B["tv"] = self.sb(es, "p1tv", [128, 256], F32)
        B["vnb"] = self.sb(es, "p1vnb", [128, 256], BF16)
        B["yC"] = self.sb(es, "p1yC", [128, 256], F32)
        B["ynCt"] = self.sb(es, "p1ynCt", [128, 256], BF16)
        B["t_sgu"] = Track()
        B["bst"] = self.sb(es, "p1bst", [128, 16], F32)
        B["swT"] = self.sb(es, "p1swT", [128, 4, 128], BF16)
        B["sbT"] = self.sb(es, "p1sbT", [128, 4], F32)
        B["slg"] = self.sb(es, "p1slg", [128, 256], F32)
        B["slb"] = self.sb(es, "p1slb", [128, 256], F32)
        B["ggT"] = self.sb(es, "p1ggT", [128, 8], F32)
        B["t_par"] = Track()
        tp = B["t_par"]
        P.dma("pool", B["swT"][:], I["sgu_wT"][l], writes=[tp])
        P.dma("sp", B["sbT"][:], I["sgu_bT"][l], writes=[tp])
        P.dma("sp", B["slg"][:], I["sgu_ln_g"][l].partition_broadcast(128), writes=[tp])
        P.dma("sp", B["slb"][:], I["sgu_ln_b"][l].partition_broadcast(128), writes=[tp])
        P.dma("sp", B["ggT"][:], I["ggT"][l], writes=[tp])
        for i in range(2):
            P.op("dve", lambda e, i=i: e.memset(B["v"][i][:], 1.0), [], [B["t_v"][i]])
        B["n"] = 0
        B["nx"] = 0
        return B

    def norm_to_hT(self, B, xt, t_xt, gsT, shT, t_mod, hT_dst, t_hT, ps_i):
        P = self.P
        i = B["nx"] % 2
        B["nx"] += 1
        xn, t_xn = B["xn"][i], B["t_xn"][i]
        st, t_st = B["st"], B["t_st"]
        col = st[:, i:i + 1]
        self.act(B["junk"][:], xt[:], AF.Square, [t_xt], [t_st], accum_out=col)
        self.rstd_from(col, col, 1.0 / D, [t_st], t_st)
        self.ts("dve", xn[:], xt[:], col, None, ALU.mult, None, [t_xt, t_st], [t_xn])
        psT, t_psT = self.ps[ps_i], self.t_ps[ps_i]
        pv = psT[:, :].bitcast(BF16).rearrange("p (k t) -> p k t", k=8)
        for k in range(8):
            self.tr(pv[:, k, :], xn[:, k * 128:(k + 1) * 128], [t_xn], [t_psT], signal=(k == 7))
        for k in range(8):
            self.ts("dve", hT_dst[:, k, :], pv[:, k, :], gsT[:, k:k + 1], shT[:, k:k + 1], ALU.mult, ALU.add, [t_psT, t_mod], [t_hT])

    def phase1(self, B, seg, tiles, xsrc, t_xsrc, gsT, shT, t_mod, valid_ap, kv_only=False):
        for g in self.phase1_parts(B, seg, tiles, xsrc, t_xsrc, gsT, shT, t_mod, valid_ap, kv_only):
            for _ in g:
                pass

    def phase1_parts(self, B, seg, tiles, xsrc, t_xsrc, gsT, shT, t_mod, valid_ap, kv_only=False):
        it = B["n"]
        B["n"] += 1
        sel = it % 2
        hsel = it % 3
        S = {nm: (B[nm][sel], B["t_" + nm][sel]) for nm in ("bg", "uA", "yB", "q", "k", "ynC", "v")}
        return (self.phase1_N(B, tiles, xsrc, t_xsrc, gsT, shT, t_mod, hsel),
                self.phase1_F(B, seg, tiles, valid_ap, kv_only, hsel, S),
                self.phase1_B(B, seg, tiles, kv_only, hsel, S))

    def phase1_N(self, B, tiles, xsrc, t_xsrc, gsT, shT, t_mod, hsel):
        P = self.P
        hT, t_hT = B["hT"][hsel], B["t_hT"][hsel]
        for j, t in enumerate(tiles):
            i = B["nx"] % 2
            xt, t_xt = B["xt"][i], B["t_xt"][i]
            P.dma("sp", xt[:], xsrc[t * 128:(t + 1) * 128, :], reads=([t_xsrc[t]] if t_xsrc is not None else []), writes=[t_xt])
            self.norm_to_hT(B, xt, t_xt, gsT, shT, t_mod, hT[:, :, j * 128:(j + 1) * 128], t_hT, 3)
            yield

    def phase1_F(self, B, seg, tiles, valid_ap, kv_only, sel, S):
        P = self.P
        nt = len(tiles)
        N = nt * 128
        hT, t_hT = B["hT"][sel], B["t_hT"][sel]
        w_in, t_w = B["w_in"], B["t_w"]
        tc = self.t_const
        order = [16, 17] if kv_only else [0, 1, 2, 4, 3, 5, 8, 6, 9, 7, 14, 15, 16, 17]
        for oi, cc in enumerate(order):
            pb, tpb = self.ps[oi % 3], self.t_ps[oi % 3]
            for k in range(8):
                self.mm(pb[:, 0:N], w_in[:, k, cc * 128:(cc + 1) * 128], hT[:, k, 0:N], k == 0, k == 7, [t_w, t_hT], [tpb])
            c = cc % 2
            if cc in (0, 1):
                self.copy("act", S["bg"][0][:, c, 0:N], pb[:, 0:N], [tpb], [S["bg"][1]])
            elif cc in (2, 3):
                self.copy("act", B["cg"][c][:, 0:N], pb[:, 0:N], [tpb], [B["t_cg"][c]])
            elif cc in (4, 5):
                self.stt(S["uA"][0][:, c, 0:N], pb[:, 0:N], valid_ap, B["cg"][c][:, 0:N], ALU.mult, ALU.mult,
                         [tpb, B["t_cg"][c], tc], [S["uA"][1]])
            elif cc in (8, 9):
                self.act(B["sig"][c][:, 0:N], pb[:, 0:N], AF.Sigmoid, [tpb], [B["t_sig"][c]])
            elif cc in (6, 7):
                self.stt(S["yB"][0][:, c, 0:N], pb[:, 0:N], valid_ap, B["sig"][c][:, 0:N], ALU.mult, ALU.mult,
                         [tpb, B["t_sig"][c], tc], [S["yB"][1]])
            elif cc in (14, 15):
                self.copy("act" if c else "dve", S["q"][0][:, c, 0:N], pb[:, 0:N], [tpb], [S["q"][1]])
            else:
                self.copy("act" if c else "dve", S["k"][0][:, c, 0:N], pb[:, 0:N], [tpb], [S["k"][1]])
            yield
        t0 = tiles[0]
        cs = seg.cols(t0, N)
        names = ("k",) if kv_only else ("bg", "uA", "yB", "q", "k")
        for nm in names:
            for c in range(2):
                P.dma("pool", seg.arr[nm][:, c, cs], S[nm][0][:, c, 0:N], reads=[S[nm][1]], writes=seg.tr(nm, t0, t0 + nt))
        yield

    def phase1_B(self, B, seg, tiles, kv_only, sel, S):
        P = self.P
        nt = len(tiles)
        N = nt * 128
        hT, t_hT = B["hT"][sel], B["t_hT"][sel]
        w_in, t_w = B["w_in"], B["t_w"]
        for j, t in enumerate(tiles):
            pv, tpv = self.ps[6], self.t_ps[6]
            for k in range(8):
                self.mm(pv[:, 0:256], hT[:, k, j * 128:(j + 1) * 128], w_in[:, k, 2304:2560], k == 0, k == 7, [t_w, t_hT], [tpv])
            self.copy("dve", S["v"][0][:, j, :, 0:64], pv[:, 0:256].rearrange("p (h d) -> p h d", h=4), [tpv], [S["v"][1]])
            yield
            if kv_only:
                continue
            pc, tpc = self.ps[4 + j % 2], self.t_ps[4 + j % 2]
            for k in range(8):
                self.mm(pc[:, :], hT[:, k, j * 128:(j + 1) * 128], w_in[:, k, 1280:1792], k == 0, k == 7, [t_w, t_hT], [tpc])
            self.act(B["zc"][j][:], pc[:, :], AF.Gelu_apprx_tanh, [tpc], [B["t_zc"][j]])
            yield
        if not kv_only:
            for j, t in enumerate(tiles):
                for _ in self.sgu_tile(B, S["ynC"][0][:, :, j * 128:(j + 1) * 128], S["ynC"][1], j):
                    yield
        t0 = tiles[0]
        cs = seg.cols(t0, N)
        if not kv_only:
            for c in range(2):
                P.dma("pool", seg.arr["ynC"][:, c, cs], S["ynC"][0][:, c, 0:N], reads=[S["ynC"][1]], writes=seg.tr("ynC", t0, t0 + nt))
        for j, t in enumerate(tiles):
            P.dma("pool", seg.arr["v"][t + seg.padf], S["v"][0][:, j, :, :].rearrange("p h d -> p (h d)"),
                  reads=[S["v"][1]], writes=seg.tr("v", t, t + 1))
        yield

    def sgu_tile(self, B, ynC_dst, t_ynC, j):
        P = self.P
        zc, t_zc = B["zc"][j], B["t_zc"][j]
        tp, ts_ = B["t_par"], B["t_sgu"]
        bst = B["bst"]
        stats = B["tv"]
        P.op("dve", lambda e: e.bn_stats(out=bst[:, 0:6], in_=zc[:, 256:512]), [t_zc], [ts_])
        mv = bst[:, 8:10]
        P.op("dve", lambda e: e.bn_aggr(out=mv[:, 0:2], in_=bst[:, 0:6]), [ts_], [ts_])
        self.rstd_from(mv[:, 1:2], mv[:, 1:2], 1.0, [ts_], ts_)
        self.ts("dve", stats[:], zc[:, 256:512], mv[:, 0:1], mv[:, 1:2], ALU.subtract, ALU.mult, [t_zc, ts_], [ts_])
        self.tt("dve", stats[:], stats[:], B["slg"][:], ALU.mult, [ts_, tp], [ts_])
        self.tt("dve", B["vnb"][:], stats[:], B["slb"][:], ALU.add, [ts_, tp], [ts_])
        yield
        pm, tpm = self.ps[6], self.t_ps[6]
        for h in range(4):
            self.mm(pm[:, 256 + h * 64:256 + (h + 1) * 64], B["swT"][:, h, :], B["vnb"][:, h * 64:(h + 1) * 64], True, True,
                    [ts_, tp], [tpm], signal=(h == 3))
        yC = B["yC"]
        for h in range(4):
            self.stt(yC[:, h * 64:(h + 1) * 64], pm[:, 256 + h * 64:256 + (h + 1) * 64], B["sbT"][:, h:h + 1],
                     zc[:, h * 64:(h + 1) * 64], ALU.add, ALU.mult, [tpm, tp, t_zc, ts_], [ts_])
        yield
        self.group_norm_T(B, yC, ts_, B["ynCt"], 4, ynC_dst, t_ynC)
        yield

    def group_norm_T(self, B, y, t_y, ynb, gidx, dstT, t_dst):
        P = self.P
        ts_ = t_y
        col = B["bst"][:, 10:11]
        self.act(B["junk"][:, 0:256], y[:], AF.Square, [t_y], [ts_], accum_out=col)
        self.rstd_from(col, col, 1.0 / 256.0, [ts_], ts_)
        self.ts("dve", ynb[:], y[:], col, None, ALU.mult, None, [ts_], [ts_])
        pT, tpT = self.ps[7], self.t_ps[7]
        pv = pT[:, :].bitcast(BF16)
        for c in range(2):
            self.tr(pv[:, c * 128:(c + 1) * 128], ynb[:, c * 128:(c + 1) * 128], [ts_], [tpT], signal=(c == 1))
        for c in range(2):
            self.act(dstT[:, c, :], pv[:, c * 128:(c + 1) * 128], AF.Copy, [tpT, B["t_par"]], [t_dst],
                     scale=B["ggT"][:, gidx + c:gidx + c + 1])

    def p2_alloc(self, es, l):
        P, I = self.P, self.I
        B = {}
        B["w_out"] = self.sb(es, "w_out_sb", [128, 8, D], BF16)
        B["t_w"] = Track()
        wsrc = I["w_out"][l].rearrange("(kc p) n -> p kc n", p=128)
        for cb in range(2):
            P.dma("pool", B["w_out"][:, :, cb * 512:(cb + 1) * 512], wsrc[:, :, cb * 512:(cb + 1) * 512], writes=[B["t_w"]])
        B["t_par"] = Track()
        tp = B["t_par"]
        for nm, key, shape in (("caw", "cawT", [128, 2, 3]), ("cbw", "cbwT", [128, 2, 31]), ("cbb", "cbbT", [128, 2]),
                               ("clg", "clgT", [128, 2]), ("clb", "clbT", [128, 2]), ("ggT", "ggT", [128, 8])):
            B[nm] = self.sb(es, "p2" + nm, shape, F32)
            P.dma("sp", B[nm][:], I[key][l], writes=[tp])
        for nm, shape in (("uAw", [128, 2, 514]), ("bgw", [128, 2, 512]), ("yBw", [128, 2, 542]), ("qw", [128, 2, 512]),
                          ("kw", [128, 2, 1280]), ("vw", [128, 10, 4, 66])):
            B[nm] = [self.sb(es, "p2%s%d" % (nm, i), shape, BF16) for i in range(2)]
            B["t_" + nm] = tracks(2)
        B["ynT"] = [self.sb(es, "p2ynT%d" % i, [128, 8, 512], BF16) for i in range(2)]
        B["t_ynT"] = tracks(2)
        B["dg"] = self.sb(es, "p2dg", [128, 2, 31, 128], BF16)
        for c in range(2):
            for jt in range(31):
                self.ts("dve", B["dg"][:, c, jt, :], self.ident[:], B["cbw"][:, c, jt:jt + 1], None, ALU.mult, None,
                        [self.t_const, tp], [tp])
        B["accA"] = self.sb(es, "p2accA", [128, 2, 512], F32)
        B["zf"] = self.sb(es, "p2zf", [128, 2, 512], F32)
        B["zb"] = self.sb(es, "p2zb", [128, 2, 512], BF16)
        B["zsq"] = self.sb(es, "p2zsq", [128, 2, 512], BF16)
        B["mean"] = self.sb(es, "p2mean", [128, 512], F32)
        B["var"] = self.sb(es, "p2var", [128, 512], F32)
        B["tmpn"] = self.sb(es, "p2tmpn", [128, 512], F32)
        B["t_cv"] = Track()
        B["PT"] = [self.sb(es, "p2PT%d" % i, [128, 512], BF16) for i in range(2)]
        B["t_PT"] = tracks(2)
        B["rec"] = self.sb(es, "p2rec", [128, 4], F32)
        B["yD"] = self.sb(es, "p2yD", [128, 256], F32)
        B["ynD"] = self.sb(es, "p2ynD", [128, 256], BF16)
        B["bst"] = self.sb(es, "p2bst", [128, 16], F32)
        B["junk"] = self.sb(es, "p2junk", [128, 256], BF16)
        B["t_at"] = Track()
        B["TG"] = self.sb(es, "p2TG", [128, 6, 512], BF16)
        B["TS"] = [self.sb(es, "p2TS%d" % i, [128, 6, 512], BF16) for i in range(2)]
        B["t_TG"], B["t_TS"] = Track(), tracks(2)
        B["tst"] = [self.sb(es, "p2tst%d" % i, [128, 512], F32) for i in range(2)]
        B["t_tst"] = tracks(2)
        B["cK"] = self.sb(es, "p2cK", [128, 2, 256], BF16)
        B["cV"] = self.sb(es, "p2cV", [128, 2, 4, 66], BF16)
        B["t_ckv"] = Track()
        B["xres"] = [self.sb(es, "p2xres%d" % i, [128, D], F32) for i in range(2)]
        B["t_xres"] = tracks(2)
        B["tmpo"] = [self.sb(es, "p2tmpo%d" % i, [128, D], F32) for i in range(2)]
        B["t_tmpo"] = tracks(2)
        B["n"] = 0
        B["nt"] = 0
        B["ntab"] = 0
        B["l"] = l
        return B

    def table_prep(self, B, slot, dst, t_dst):
        P, I = self.P, self.I
        for ch in range(6):
            i = B["ntab"] % 2
            B["ntab"] += 1
            st, t_st = B["tst"][i], B["t_tst"][i]
            P.dma("sp", st[:], I["btab"][B["l"], slot, ch].rearrange("p h q -> p (h q)"), writes=[t_st])
            self.act(dst[:, ch, :], st[:], AF.Exp, [t_st], [t_dst])

    def load_ctx_kv(self, B):
        P, seg = self.P, self.segC
        cs = seg.cols(0, 256)
        for c in range(2):
            P.dma("sp", B["cK"][:, c, :], seg.arr["k"][:, c, cs], reads=seg.tr("k", 0, 2), writes=[B["t_ckv"]])
        for t in range(2):
            P.dma("sp", B["cV"][:, t, :, :].rearrange("p h d -> p (h d)"), seg.arr["v"][t + seg.padf],
                  reads=seg.tr("v", t, t + 1), writes=[B["t_ckv"]])

    def attn_super(self, B, qw, t_qw, specs, t_ynT):
        O, tO = self.ps[6], self.t_ps[6]
        Ov = O[:, 0:260].rearrange("p (h d) -> p h d", h=4)
        flat = [(j, ci, len(chunks), ch, dst) for (j, chunks, dst) in specs for ci, ch in enumerate(chunks)]

        def s_mm(idx):
            j, ci, nch, (kap, vap, tab, rtr), dst = flat[idx]
            ba = 4 if idx % 2 == 0 else 2
            for h in range(4):
                c, hp = h // 2, (h % 2) * 64
                bk = ba + (h % 2)
                self.mm(self.ps[bk][:, c * 128:(c + 1) * 128], kap[hp:hp + 64, c, :], qw[hp:hp + 64, c, j * 128:(j + 1) * 128], True, True,
                        list(rtr) + [t_qw], [self.t_ps[bk]], signal=(h >= 2))

        for idx in range(min(2, len(flat))):
            s_mm(idx)
        for idx, (j, ci, nch, (kap, vap, tab, rtr), dst) in enumerate(flat):
            ba = 4 if idx % 2 == 0 else 2
            PT, tPT = B["PT"][idx % 2], B["t_PT"][idx % 2]
            src = self.psall[:, ba * 512:(ba + 2) * 512].rearrange("p (two r) -> p two r", two=2)[:, :, 0:256].rearrange("p two (c q) -> p two c q", c=2)
            dstv = PT[:].rearrange("p (c two q) -> p two c q", c=2, two=2)
            self.act(dstv, src, AF.Exp, [self.t_ps[ba], self.t_ps[ba + 1]], [tPT], scale=0.125)
            if idx + 2 < len(flat):
                s_mm(idx + 2)
            if tab is not None:
                self.tt("dve", PT[:], PT[:], tab[0], ALU.mult, [tPT, tab[1]], [tPT])
            for h in range(4):
                self.mm(Ov[:, h, :], PT[:, h * 128:(h + 1) * 128], vap[:, h, 0:65], (ci == 0 and h == 0), (ci == nch - 1),
                        list(rtr) + [tPT], [tO], signal=(h == 3), sgc=True)
            yield
            if ci == nch - 1:
                ta = B["t_at"]
                self.P.op("dve", lambda e: e.reciprocal(out=B["rec"][:], in_=Ov[:, :, 64]), [tO], [ta])
                for h in range(4):
                    self.ts("dve", B["yD"][:, h * 64:(h + 1) * 64], Ov[:, h, 0:64], B["rec"][:, h:h + 1], None, ALU.mult, None, [tO, ta], [ta])
                self.group_norm_T(B, B["yD"], ta, B["ynD"], 6, dst, t_ynT)
                yield

    def xpart_rms(self, B, y, gidx, ynT, t_ynT, N):
        tcv, tp = B["t_cv"], B["t_par"]
        sq = B["zsq"]
        for c in range(2):
            self.act(sq[:, c, 0:N], y[:, c, 0:N], AF.Square, [tcv], [tcv])
        pm, tpm = self.ps[1], self.t_ps[1]
        for c in range(2):
            self.mm(pm[:, 0:N], self.onesM[:], sq[:, c, 0:N], c == 0, c == 1, [tcv, self.t_const], [tpm])
        r = B["var"]
        self.rstd_from(r[:, 0:N], pm[:, 0:N], 1.0, [tpm, tcv], tcv)
        for c in range(2):
            self.stt(ynT[:, gidx + c, 0:N], y[:, c, 0:N], B["ggT"][:, gidx + c:gidx + c + 1], r[:, 0:N], ALU.mult, ALU.mult,
                     [tcv, tp], [t_ynT])

    def phase2(self, B, seg, tiles, is_ctx, xsrc, t_xsrc, xdst, t_xdst, gate):
        ga, gb = self.phase2_parts(B, seg, tiles, is_ctx, xsrc, t_xsrc, xdst, t_xdst, gate)
        for _ in ga:
            pass
        for _ in gb:
            pass

    def phase2_parts(self, B, seg, tiles, is_ctx, xsrc, t_xsrc, xdst, t_xdst, gate):
        sel = B["n"] % 2
        B["n"] += 1
        W = {nm: (B[nm][sel], B["t_" + nm][sel]) for nm in ("uAw", "bgw", "yBw", "qw", "kw", "vw", "ynT")}
        return (self.phase2_A(B, seg, tiles, is_ctx, W), self.phase2_B(B, seg, tiles, is_ctx, xsrc, t_xsrc, xdst, t_xdst, gate, W))

    def phase2_A(self, B, seg, tiles, is_ctx, W):
        P = self.P
        nt = len(tiles)
        N = nt * 128
        t0 = tiles[0]
        ynT, t_ynT = W["ynT"]
        a0 = (t0 + seg.padf) * 128
        wl = 1 if is_ctx else 3
        nwin = nt + 2 if is_ctx else nt + 6
        tcv, tp = B["t_cv"], B["t_par"]
        for c in range(2):
            P.dma("sp", W["uAw"][0][:, c, 0:N + 2], seg.arr["uA"][:, c, a0 - 1:a0 + N + 1], reads=seg.tr("uA", t0 - 1, t0 + nt + 1), writes=[W["uAw"][1]])
            P.dma("sp", W["yBw"][0][:, c, 0:N + 30], seg.arr["yB"][:, c, a0 - 15:a0 + N + 15], reads=seg.tr("yB", t0 - 1, t0 + nt + 1), writes=[W["yBw"][1]])
            P.dma("sp", W["bgw"][0][:, c, 0:N], seg.arr["bg"][:, c, a0:a0 + N], reads=seg.tr("bg", t0, t0 + nt), writes=[W["bgw"][1]])
            P.dma("sp", W["qw"][0][:, c, 0:N], seg.arr["q"][:, c, a0:a0 + N], reads=seg.tr("q", t0, t0 + nt), writes=[W["qw"][1]])
            P.dma("sp", ynT[:, 4 + c, 0:N], seg.arr["ynC"][:, c, a0:a0 + N], reads=seg.tr("ynC", t0, t0 + nt), writes=[t_ynT])
            if not is_ctx:
                P.dma("sp", W["kw"][0][:, c, 0:nwin * 128], seg.arr["k"][:, c, a0 - wl * 128:a0 + (nwin - wl) * 128],
                      reads=seg.tr("k", t0 - wl, t0 - wl + nwin), writes=[W["kw"][1]])
        if not is_ctx:
            for wi in range(nwin):
                tt_ = t0 - wl + wi
                P.dma("sp", W["vw"][0][:, wi, :, :].rearrange("p h d -> p (h d)"), seg.arr["v"][tt_ + seg.padf],
                      reads=seg.tr("v", tt_, tt_ + 1), writes=[W["vw"][1]])
        yield
        uAw, t_uAw = W["uAw"]
        acc = B["accA"]
        for c in range(2):
            self.ts("dve", acc[:, c, 0:N], uAw[:, c, 0:N], B["caw"][:, c, 0:1], None, ALU.mult, None, [t_uAw, tp], [tcv])
            for jt in (1, 2):
                self.stt(acc[:, c, 0:N], uAw[:, c, jt:jt + N], B["caw"][:, c, jt:jt + 1], acc[:, c, 0:N], ALU.mult, ALU.add, [t_uAw, tp, tcv], [tcv])
            yield
            self.tt("dve", acc[:, c, 0:N], acc[:, c, 0:N], W["bgw"][0][:, c, 0:N], ALU.mult, [tcv, W["bgw"][1]], [tcv])
            yield
        self.xpart_rms(B, acc, 0, ynT, t_ynT, N)
        yield
        yBw, t_yBw = W["yBw"]
        zf = B["zf"]
        for c in range(2):
            pcv, tpcv = self.ps[c], self.t_ps[c]
            for jt in range(31):
                self.mm(pcv[:, 0:N], B["dg"][:, c, jt, :], yBw[:, c, jt:jt + N], jt == 0, jt == 30, [t_yBw, tp], [tpcv])
                if jt % 4 == 3:
                    yield
            self.act(zf[:, c, 0:N], pcv[:, 0:N], AF.Identity, [tpcv, tp], [tcv], bias=B["cbb"][:, c:c + 1])
            yield
            self.copy("act", B["zb"][:, c, 0:N], zf[:, c, 0:N], [tcv], [tcv])
            self.act(B["zsq"][:, c, 0:N], zf[:, c, 0:N], AF.Square, [tcv], [tcv])
            yield
        pmean, tpmean = self.ps[0], self.t_ps[0]
        pmsq, tpmsq = self.ps[1], self.t_ps[1]
        for c in range(2):
            self.mm(pmean[:, 0:N], self.onesM[:], B["zb"][:, c, 0:N], c == 0, c == 1, [tcv, self.t_const], [tpmean])
        for c in range(2):
            self.mm(pmsq[:, 0:N], self.onesM[:], B["zsq"][:, c, 0:N], c == 0, c == 1, [tcv, self.t_const], [tpmsq])
        mean, var, tmpn = B["mean"], B["var"], B["tmpn"]
        yield
        self.copy("act", mean[:, 0:N], pmean[:, 0:N], [tpmean], [tcv])
        self.act(tmpn[:, 0:N], pmean[:, 0:N], AF.Square, [tpmean], [tcv])
        yield
        self.tt("dve", var[:, 0:N], pmsq[:, 0:N], tmpn[:, 0:N], ALU.subtract, [tpmsq, tcv], [tcv])
        self.ts("dve", var[:, 0:N], var[:, 0:N], 0.0, None, ALU.max, None, [tcv], [tcv])
        self.rstd_from(var[:, 0:N], var[:, 0:N], 1.0, [tcv], tcv)
        yield
        for c in range(2):
            self.tt("dve", tmpn[:, 0:N], zf[:, c, 0:N], mean[:, 0:N], ALU.subtract, [tcv], [tcv])
            self.tt("dve", tmpn[:, 0:N], tmpn[:, 0:N], var[:, 0:N], ALU.mult, [tcv], [tcv])
            self.act(zf[:, c, 0:N], tmpn[:, 0:N], AF.Silu, [tcv, tp], [tcv], bias=B["clb"][:, c:c + 1], scale=B["clg"][:, c:c + 1])
            yield
        self.xpart_rms(B, zf, 2, ynT, t_ynT, N)
        yield

    def phase2_B(self, B, seg, tiles, is_ctx, xsrc, t_xsrc, xdst, t_xdst, gate, W):
        P = self.P
        nt = len(tiles)
        N = nt * 128
        t0 = tiles[0]
        ynT, t_ynT = W["ynT"]
        wl = 1 if is_ctx else 3
        qw, t_qw = W["qw"]
        kw, t_kw = W["kw"]
        vw, t_vw = W["vw"]
        ctx_chunks = [(B["cK"][:, :, ci * 128:(ci + 1) * 128], B["cV"][:, ci, :, :], None, [B["t_ckv"]]) for ci in range(2)]
        specs = []
        tabs = {}
        for j, t in enumerate(tiles):
            chunks = []
            if not is_ctx:
                own = t - OWN_LO
                ws, slot = t - 2, 0
                if own in (0, 1, 30, 31):
                    slot = {0: 1, 1: 2, 30: 3, 31: 4}[own]
                    if own == 31:
                        ws = t - 3
                    tab, t_tab = B["TS"][j % 2], B["t_TS"][j % 2]
                    self.table_prep(B, slot, tab, t_tab)
                else:
                    tab, t_tab = B["TG"], B["t_TG"]
                for ch in range(6):
                    wi = ws + ch - (t0 - wl)
                    chunks.append((kw[:, :, wi * 128:(wi + 1) * 128], vw[:, wi, :, :], (tab[:, ch, :], t_tab), [t_kw, t_vw]))
            specs.append((j, chunks + ctx_chunks, ynT[:, 6:8, j * 128:(j + 1) * 128]))
        for _ in self.attn_super(B, qw, t_qw, specs, t_ynT):
            yield
        for j, t in enumerate(tiles):
            i = B["nt"] % 2
            B["nt"] += 1
            xr, t_xr = B["xres"][i], B["t_xres"][i]
            tm, t_tm = B["tmpo"][i], B["t_tmpo"][i]
            P.dma("sp", xr[:], xsrc[t * 128:(t + 1) * 128, :], reads=([t_xsrc[t]] if t_xsrc is not None else []), writes=[t_xr])
            for cb in range(2):
                po, tpo = self.ps[2 + cb], self.t_ps[2 + cb]
                for k in range(8):
                    self.mm(po[:, :], ynT[:, k, j * 128:(j + 1) * 128], B["w_out"][:, k, cb * 512:(cb + 1) * 512], k == 0, k == 7,
                            [t_ynT, B["t_w"]], [tpo])
                self.tt("dve", tm[:, cb * 512:(cb + 1) * 512], po[:, :], gate[0][:, cb * 512:(cb + 1) * 512], ALU.mult, [tpo, gate[1]], [t_tm])
            self.tt("pool", tm[:], tm[:], xr[:], ALU.add, [t_tm, t_xr], [t_tm])
            P.dma("pool", xdst[t * 128:(t + 1) * 128, :], tm[:], reads=[t_tm], writes=[t_xdst[t]])
            yield

    def ffn_phase(self, l, M, blocks, experts, moe):
        P, I = self.P, self.I
        assert not moe
        w1, w3, w2, nf = experts[0]
        with ExitStack() as es:
            B = {}
            B["xt"] = [self.sb(es, "p3xt%d" % i, [128, D], F32) for i in range(2)]
            B["t_xt"] = tracks(2)
            B["junk"] = self.sb(es, "p3junk", [128, D], BF16)
            B["xn"] = [self.sb(es, "p3xn%d" % i, [128, D], BF16) for i in range(2)]
            B["t_xn"] = tracks(2)
            B["st"] = self.sb(es, "p3st", [128, 16], F32)
            B["t_st"] = Track()
            B["nx"] = 0
            h2T = [self.sb(es, "p3h2T%d" % i, [128, 8, 1024], BF16) for i in range(2)]
            t_h2T = tracks(2)
            yacc = self.sb(es, "p3yacc", [128, 8, D], F32)
            t_yacc = tracks(8)
            NWB = 3
            w1b = [self.sb(es, "p3w1b%d" % i, [128, 8, 512], BF16) for i in range(NWB)]
            w3b = [self.sb(es, "p3w3b%d" % i, [128, 8, 512], BF16) for i in range(NWB)]
            w2b = [self.sb(es, "p3w2b%d" % i, [128, 4, D], BF16) for i in range(NWB)]
            t_w = tracks(NWB)
            gT = [self.sb(es, "p3gT%d" % i, [128, 4, 512], BF16) for i in range(2)]
            t_gT = tracks(2)
            sl = [self.sb(es, "p3sl%d" % i, [128, 512], BF16) for i in range(2)]
            t_sl = tracks(2)
            ro = [self.sb(es, "p3ro%d" % i, [128, D], F32) for i in range(2)]
            t_ro = tracks(2)
            units = []
            f0 = 0
            while f0 < nf:
                nfc = min(4, nf - f0)
                units.append((f0, nfc))
                f0 += nfc
            allu = [(bi, u) for bi in range(len(blocks)) for u in units]
            nw = [0]

            def load_unit(u):
                f0, nfc = u
                sel = nw[0] % NWB
                nw[0] += 1
                s1 = w1.rearrange("(kc p) f -> p kc f", p=128)[:, :, f0 * 128:(f0 + nfc) * 128]
                s3 = w3.rearrange("(kc p) f -> p kc f", p=128)[:, :, f0 * 128:(f0 + nfc) * 128]
                s2 = w2.rearrange("(fc p) d -> p fc d", p=128)[:, f0:f0 + nfc, :]
                P.dma("pool", w1b[sel][:, :, 0:nfc * 128], s1, writes=[t_w[sel]])
                P.dma("pool", w3b[sel][:, :, 0:nfc * 128], s3, writes=[t_w[sel]])
                P.dma("pool", w2b[sel][:, 0:nfc, :], s2, writes=[t_w[sel]])

            def gen_norm(bi):
                hb, t_hb = h2T[bi % 2], t_h2T[bi % 2]
                for j, td in enumerate(blocks[bi]):
                    i = B["nx"] % 2
                    xt, t_xt = B["xt"][i], B["t_xt"][i]
                    P.dma("sp", xt[:], td["src"], reads=[td["t_src"]], writes=[t_xt])
                    self.norm_to_hT(B, xt, t_xt, M["gs2" + td["mod"]], M["mod" + td["mod"]][:, 24:32], M["t"],
                                    hb[:, :, j * 128:(j + 1) * 128], t_hb, 7)
                    yield

            for u0 in range(NWB - 1):
                load_unit(allu[u0][1])
            for _ in gen_norm(0):
                pass
            ng = 0
            gu = 0
            for bi, blk in enumerate(blocks):
                nt = len(blk)
                hb, t_hb = h2T[bi % 2], t_h2T[bi % 2]
                nxt = gen_norm(bi + 1) if bi + 1 < len(blocks) else None
                for ui, u in enumerate(units):
                    cur = gu % NWB
                    if gu + NWB - 1 < len(allu):
                        load_unit(allu[gu + NWB - 1][1])
                    gu += 1
                    f0, nfc = u
                    first = (ui == 0)
                    for sb0 in range(0, nt, 4):
                        sbt = min(4, nt - sb0)
                        N = sbt * 128
                        g, t_g = gT[ng % 2], t_gT[ng % 2]
                        ng += 1
                        for fc in range(nfc):
                            p1, tp1 = self.ps[(fc % 2) * 2], self.t_ps[(fc % 2) * 2]
                            p3, tp3 = self.ps[(fc % 2) * 2 + 1], self.t_ps[(fc % 2) * 2 + 1]
                            for k in range(8):
                                self.mm(p1[:, 0:N], w1b[cur][:, k, fc * 128:(fc + 1) * 128], hb[:, k, sb0 * 128:sb0 * 128 + N], k == 0, k == 7,
                                        [t_w[cur], t_hb], [tp1])
                            for k in range(8):
                                self.mm(p3[:, 0:N], w3b[cur][:, k, fc * 128:(fc + 1) * 128], hb[:, k, sb0 * 128:sb0 * 128 + N], k == 0, k == 7,
                                        [t_w[cur], t_hb], [tp3])
                            s_, t_s = sl[fc % 2], t_sl[fc % 2]
                            self.act(s_[:, 0:N], p1[:, 0:N], AF.Silu, [tp1], [t_s])
                            self.tt("dve", g[:, fc, 0:N], p3[:, 0:N], s_[:, 0:N], ALU.mult, [tp3, t_s], [t_g])
                        for jj in range(sbt):
                            j = sb0 + jj
                            for cb in range(2):
                                py, tpy = self.ps[4 + (jj % 2) * 2 + cb], self.t_ps[4 + (jj % 2) * 2 + cb]
                                for fc in range(nfc):
                                    self.mm(py[:, :], g[:, fc, jj * 128:(jj + 1) * 128], w2b[cur][:, fc, cb * 512:(cb + 1) * 512], fc == 0, fc == nfc - 1,
                                            [t_g, t_w[cur]], [tpy])
                                ya = yacc[:, j, cb * 512:(cb + 1) * 512]
                                if first:
                                    self.copy("dve", ya, py[:, :], [tpy], [t_yacc[j]])
                                else:
                                    self.tt("dve", ya, py[:, :], ya, ALU.add, [tpy, t_yacc[j]], [t_yacc[j]])
                        if nxt is not None and ui >= 1:
                            for _ in range(1):
                                try:
                                    next(nxt)
                                except StopIteration:
                                    nxt = None
                                    break
                if nxt is not None:
                    for _ in nxt:
                        pass
                for j, td in enumerate(blk):
                    i = B["nx"] % 2
                    B["nx"] += 1
                    xt, t_xt = B["xt"][i], B["t_xt"][i]
                    P.dma("sp", xt[:], td["src"], reads=[td["t_src"]], writes=[t_xt])
                    g2 = M["g2" + td["mod"]]
                    r_, t_r = ro[j % 2], t_ro[j % 2]
                    self.tt("dve", r_[:], yacc[:, j, :], g2[:], ALU.mult, [t_yacc[j], M["t"]], [t_r])
                    self.tt("dve", r_[:], r_[:], xt[:], ALU.add, [t_r, t_xt], [t_r])
                    P.dma("pool", td["dst"], r_[:], reads=[t_r], writes=[td["t_dst"]])
            P.barrier()

    def pipeline(self, parts):
        if not parts:
            return
        ns = len(parts[0])
        n = len(parts)
        for step in range(n + ns - 1):
            live = []
            for k in range(ns):
                s_ = step - k
                if 0 <= s_ < n:
                    live.append(parts[s_][k])
            live.reverse()
            while live:
                for g in list(live):
                    try:
                        next(g)
                    except StopIteration:
                        live.remove(g)

    NBLK = 23

    def moe_sparse(self, M, tiles):
        P, I, nc = self.P, self.I, self.nc
        NB = self.NBLK
        IO = bass.IndirectOffsetOnAxis
        Xs, Ys, t_Xs = self.moe_Xs, self.moe_Ys, self.t_moe_Xs
        t_Ys = tracks(NB * 4)
        ntile = len(tiles)
        with ExitStack() as es0:
            slots_i = self.sb(es0, "ms_slots", [128, ntile, 2], I32)
            ghl = self.sb(es0, "ms_ghl", [128, ntile, 2], F32)
            idx_i = self.sb(es0, "ms_idx", [128, NB, 7], I32)
            fing = self.sb(es0, "ms_fing", [128, D], F32)
            t_rt = Track()
            t_par = Track()
            P.dma("sp", fing[:], I["final_g"].partition_broadcast(128), writes=[t_par])
            NWB = 3
            wall = [self.sb(es0, "mewall%d" % i, [128, 3 * 4096], BF16) for i in range(NWB)]
            w1b = [w[:, 0:4096].rearrange("p (k f) -> p k f", k=8) for w in wall]
            w3b = [w[:, 4096:8192].rearrange("p (k f) -> p k f", k=8) for w in wall]
            w2b = [w[:, 8192:12288].rearrange("p (k f) -> p k f", k=4) for w in wall]
            t_w = tracks(NWB)
            units = [(b, fb) for b in range(NB) for fb in range(7)]
            nw = [0]

            def load_unit(u):
                b, fb = u
                sel = nw[0] % NWB
                nw[0] += 1
                off = IO(ap=idx_i[:, b, fb:fb + 1], axis=0)
                P.idma(wall[sel][:], None, I["moe_wr"], off, reads=[t_rt], writes=[t_w[sel]])
                return sel
            with ExitStack() as es:
                B = {}
                B["xt"] = [self.sb(es, "msxt%d" % i, [128, D], F32) for i in range(2)]
                B["t_xt"] = tracks(2)
                B["junk"] = self.sb(es, "msjunk", [128, D], BF16)
                B["xn"] = [self.sb(es, "msxn%d" % i, [128, D], BF16) for i in range(2)]
                B["t_xn"] = tracks(2)
                B["st"] = self.sb(es, "msst", [128, 16], F32)
                B["t_st"] = Track()
                B["nx"] = 0
                h2tok = self.sb(es, "msh2tok", [128, ntile, D], BF16)
                t_h2tok = tracks(ntile)
                hTt = [self.sb(es, "mshTt%d" % i, [128, 8, 128], BF16) for i in range(2)]
                t_hTt = tracks(2)
                zer = self.sb(es, "mszer", [128, D], BF16)
                rw = self.sb(es, "msrw", [128, 8, 8], BF16)
                rbb = self.sb(es, "msrbb", [128, 8], F32)
                utri = self.sb(es, "msutri", [128, 128], BF16)
                onesb = self.sb(es, "msones", [128, 128], BF16)
                iota = self.sb(es, "msiota", [128, 1], F32)
                rs = self.sb(es, "msrs", [128, 8, 8], F32)
                t_rs = Track()
                maskb = self.sb(es, "msmaskb", [128, 8], BF16)
                mask_all = self.sb(es, "msmask", [128, ntile, 8], F32)
                gates_all = self.sb(es, "msgates", [128, ntile, 8], F32)
                rank_all = self.sb(es, "msrank", [128, ntile, 8], F32)
                carry = self.sb(es, "mscarry", [128, 8], F32)
                sc = self.sb(es, "mssc", [128, 16, 8], F32)
                sci = self.sb(es, "mssci", [128, 8], I32)
                be = self.sb(es, "msbe", [128, NB], F32)
                idx_f = self.sb(es, "msidxf", [128, NB, 7], F32)
                slots_f = self.sb(es, "msslotsf", [128, ntile, 2], F32)
                P.dma("pool", rw[:], I["router_wT"], writes=[t_par])
                P.dma("pool", utri[:], I["utri"], writes=[t_par])
                P.dma("sp", rbb[:], I["router_b"].partition_broadcast(128), writes=[t_par])
                P.dma("sp", iota[:], I["iota_p"], writes=[t_par])
                P.op("dve", lambda e: e.memset(onesb[:], 1.0), [], [t_par])
                P.op("dve", lambda e: e.memset(zer[:], 0.0), [], [t_par])
                P.op("dve", lambda e: e.memset(carry[:], 0.0), [], [t_rt])
                def stage_a(j, td):
                    hT, t_hT = hTt[j % 2], t_hTt[j % 2]
                    i = B["nx"] % 2
                    xt, t_xt = B["xt"][i], B["t_xt"][i]
                    P.dma("sp", xt[:], td["src"], reads=[td["t_src"]], writes=[t_xt])
                    self.norm_to_hT(B, xt, t_xt, M["gs2L"], M["modL"][:, 24:32], M["t"], hT[:, :, :], t_hT, 7)
                    yield

                def stage_b(j, td):
                    hT, t_hT = hTt[j % 2], t_hTt[j % 2]
                    pl, tpl = self.ps[6], self.t_ps[6]
                    for k in range(8):
                        self.mm(pl[:, 0:8], hT[:, k, :], rw[:, k, :], k == 0, k == 7, [t_hT, t_par], [tpl])
                    lg, top, ex, msk = rs[:, 0, :], rs[:, 1, :], rs[:, 2, :], mask_all[:, j, :]
                    self.tt("dve", lg, pl[:, 0:8], rbb[:], ALU.add, [tpl, t_par], [t_rs])
                    P.op("dve", lambda e_, top=top, lg=lg: e_.max(out=top, in_=lg), [t_rs], [t_rs])
                    self.ts("dve", rs[:, 3, 0:1], top[:, 0:1], -1.0, None, ALU.mult, None, [t_rs], [t_rs])
                    self.act(ex, lg, AF.Exp, [t_rs], [t_rs], bias=rs[:, 3, 0:1])
                    self.ts("dve", msk, lg, top[:, 1:2], None, ALU.is_ge, None, [t_rs], [t_rt])
                    self.tt("dve", ex, ex, msk, ALU.mult, [t_rs, t_rt], [t_rs])
                    P.op("dve", lambda e_, ex=ex: e_.reduce_sum(out=rs[:, 3, 1:2], in_=ex, axis=mybir.AxisListType.X), [t_rs], [t_rs])
                    P.op("dve", lambda e_: e_.reciprocal(out=rs[:, 3, 1:2], in_=rs[:, 3, 1:2]), [t_rs], [t_rs])
                    self.ts("dve", gates_all[:, j, :], ex, rs[:, 3, 1:2], None, ALU.mult, None, [t_rs], [t_rt])
                    yield
                    self.copy("dve", maskb[:], msk, [t_rt], [t_rs])
                    pr, tpr = self.ps[5], self.t_ps[5]
                    self.mm(pr[:, 0:8], utri[:], maskb[:], True, True, [t_rs, t_par], [tpr], signal=False)
                    self.mm(pr[:, 8:16], onesb[:], maskb[:], True, True, [t_rs, t_par], [tpr])
                    self.tt("dve", rank_all[:, j, :], pr[:, 0:8], carry[:], ALU.add, [tpr, t_rt], [t_rt])
                    self.tt("dve", carry[:], pr[:, 8:16], carry[:], ALU.add, [tpr, t_rt], [t_rt])
                    yield
                    pT, tpT = self.ps[4], self.t_ps[4]
                    pv = pT[:, :].bitcast(BF16)
                    for k in range(8):
                        self.tr(pv[:, k * 128:(k + 1) * 128], hT[:, k, :], [t_hT], [tpT], signal=(k == 7))
                    self.copy("act", h2tok[:, j, :], pv[:, :], [tpT], [t_h2tok[j]])
                    yield

                self.pipeline([(stage_a(j, td), stage_b(j, td)) for j, td in enumerate(tiles)])
                nbk, pend, pst = sc[:, 0, :], sc[:, 1, :], sc[:, 2, :]
                vq = sc[:, 7, :]
                self.ts("dve", vq, carry[:], 511.0, 1.0 / 512.0, ALU.add, ALU.mult, [t_rt], [t_rt])
                self.copy("dve", sci[:], vq, [t_rt], [t_rt])
                self.copy("dve", nbk, sci[:], [t_rt], [t_rt])
                self.tt("dve", sc[:, 8, :], nbk, vq, ALU.is_gt, [t_rt], [t_rt])
                self.tt("dve", nbk, nbk, sc[:, 8, :], ALU.subtract, [t_rt], [t_rt])
                self.copy("dve", pend[:, 0:1], nbk[:, 0:1], [t_rt], [t_rt])
                for e in range(1, NEXP):
                    self.tt("dve", pend[:, e:e + 1], pend[:, e - 1:e], nbk[:, e:e + 1], ALU.add, [t_rt], [t_rt])
                self.tt("dve", pst, pend, nbk, ALU.subtract, [t_rt], [t_rt])
                self.ts("dve", pst, pst, 512.0, None, ALU.mult, None, [t_rt], [t_rt])
                for b in range(NB):
                    self.ts("dve", sc[:, 3, :], pend, float(b), None, ALU.is_le, None, [t_rt], [t_rt])
                    P.op("dve", lambda e_, b=b: e_.reduce_sum(out=be[:, b:b + 1], in_=sc[:, 3, :], axis=mybir.AxisListType.X), [t_rt], [t_rt])
                self.ts("dve", be[:], be[:], 7.0, 896.0, ALU.min, ALU.mult, [t_rt], [t_rt])
                self.ts("dve", be[:], be[:], iota[:, 0:1], None, ALU.add, None, [t_rt, t_par], [t_rt])
                for fb in range(7):
                    self.ts("dve", idx_f[:, :, fb], be[:], float(fb * 128), None, ALU.add, None, [t_rt], [t_rt])
                self.copy("dve", idx_i[:], idx_f[:], [t_rt], [t_rt])
                for u0 in range(NWB - 1):
                    load_unit(units[u0])
                t_slots = []
                for j in range(ntile):
                    key, m8, oh = sc[:, 4, :], sc[:, 5, :], sc[:, 6, :]
                    self.tt("dve", key, rank_all[:, j, :], pst, ALU.add, [t_rt], [t_rt])
                    self.stt(key, key, 1.0, mask_all[:, j, :], ALU.add, ALU.mult, [t_rt], [t_rt])
                    P.op("dve", lambda e_, m8=m8, key=key: e_.max(out=m8, in_=key), [t_rt], [t_rt])
                    self.ts("dve", slots_f[:, j, :], m8[:, 0:2], -1.0, None, ALU.add, None, [t_rt], [t_rt])
                    self.ts("dve", oh, key, m8[:, 0:1], None, ALU.is_equal, None, [t_rt], [t_rt])
                    self.tt("dve", oh, oh, gates_all[:, j, :], ALU.mult, [t_rt], [t_rt])
                    P.op("dve", lambda e_, j=j, oh=oh: e_.reduce_sum(out=ghl[:, j, 0:1], in_=oh, axis=mybir.AxisListType.X), [t_rt], [t_rt])
                    self.ts("dve", ghl[:, j, 1:2], ghl[:, j, 0:1], -1.0, 1.0, ALU.mult, ALU.add, [t_rt], [t_rt])
                    t_sl = Track()
                    self.copy("dve", slots_i[:, j, :], slots_f[:, j, :], [t_rt], [t_sl])
                    for kk in range(2):
                        P.idma(Xs, IO(ap=slots_i[:, j, kk:kk + 1], axis=0), h2tok[:, j, :], None,
                               reads=[t_sl, t_h2tok[j]], writes=[t_Xs])
                    t_slots.append(t_sl)
                P.barrier()
            with ExitStack() as es:
                xtok = [self.sb(es, "mextok%d" % i, [128, D], BF16) for i in range(2)]
                t_xtok = tracks(2)
                XT = [self.sb(es, "meXT%d" % i, [128, 8, 512], BF16) for i in range(2)]
                t_XT = tracks(2)
                yblk = [self.sb(es, "meyb%d" % i, [128, 4, D], F32) for i in range(2)]
                t_yblk = [tracks(4) for _ in range(2)]
                gT = [self.sb(es, "megT%d" % i, [128, 4, 512], BF16) for i in range(2)]
                t_gT = tracks(2)
                sl = [self.sb(es, "mesl%d" % i, [128, 512], BF16) for i in range(2)]
                t_sl = tracks(2)
                nxt = [0]

                def load_x(b):
                    xs_, t_xs = XT[b % 2], t_XT[b % 2]
                    for jj in range(4):
                        i = nxt[0] % 2
                        nxt[0] += 1
                        r = (b * 4 + jj) * 128
                        P.dma("sp", xtok[i][:], Xs[r:r + 128, :], reads=[t_Xs], writes=[t_xtok[i]])
                        pT, tpT = self.ps[3], self.t_ps[3]
                        pv = pT[:, :].bitcast(BF16)
                        for k in range(8):
                            self.tr(pv[:, k * 128:(k + 1) * 128], xtok[i][:, k * 128:(k + 1) * 128], [t_xtok[i]], [tpT], signal=(k == 7))
                        self.copy("act", xs_[:, :, jj * 128:(jj + 1) * 128], pv[:, :].rearrange("p (k t) -> p k t", k=8), [tpT], [t_xs])

                load_x(0)
                ng = 0
                for ui, (b, fb) in enumerate(units):
                    cur = ui % NWB
                    if ui + NWB - 1 < len(units):
                        load_unit(units[ui + NWB - 1])
                    if fb == 3 and b + 1 < NB:
                        load_x(b + 1)
                    xs_, t_xs = XT[b % 2], t_XT[b % 2]
                    yb, t_yb = yblk[b % 2], t_yblk[b % 2]
                    g, t_g = gT[ng % 2], t_gT[ng % 2]
                    ng += 1
                    for fc in range(4):
                        p1, tp1 = self.ps[(fc % 2) * 2], self.t_ps[(fc % 2) * 2]
                        p3, tp3 = self.ps[(fc % 2) * 2 + 1], self.t_ps[(fc % 2) * 2 + 1]
                        for k in range(8):
                            self.mm(p1[:, :], w1b[cur][:, k, fc * 128:(fc + 1) * 128], xs_[:, k, :], k == 0, k == 7, [t_w[cur], t_xs], [tp1])
                        for k in range(8):
                            self.mm(p3[:, :], w3b[cur][:, k, fc * 128:(fc + 1) * 128], xs_[:, k, :], k == 0, k == 7, [t_w[cur], t_xs], [tp3])
                        s_, t_s = sl[fc % 2], t_sl[fc % 2]
                        self.act(s_[:, :], p1[:, :], AF.Silu, [tp1], [t_s])
                        self.tt("dve", g[:, fc, :], p3[:, :], s_[:, :], ALU.mult, [tp3, t_s], [t_g])
                    for jj in range(4):
                        for cb in range(2):
                            py, tpy = self.ps[4 + (jj % 2) * 2 + cb], self.t_ps[4 + (jj % 2) * 2 + cb]
                            for fc in range(4):
                                self.mm(py[:, :], g[:, fc, jj * 128:(jj + 1) * 128], w2b[cur][:, fc, cb * 512:(cb + 1) * 512], fc == 0, fc == 3,
                                        [t_g, t_w[cur]], [tpy])
                            ya = yb[:, jj, cb * 512:(cb + 1) * 512]
                            if fb == 0:
                                self.copy("dve", ya, py[:, :], [tpy], [t_yb[jj]])
                            else:
                                self.tt("dve", ya, py[:, :], ya, ALU.add, [tpy, t_yb[jj]], [t_yb[jj]])
                        if fb == 6:
                            r = (b * 4 + jj) * 128
                            P.dma("sp", Ys[r:r + 128, :], yb[:, jj, :], reads=[t_yb[jj]], writes=[t_Ys[b * 4 + jj]])
                P.barrier()
            with ExitStack() as es:
                yh = [self.sb(es, "mcyh%d" % i, [128, D], F32) for i in range(2)]
                yl = [self.sb(es, "mcyl%d" % i, [128, D], F32) for i in range(2)]
                xr = [self.sb(es, "mcxr%d" % i, [128, D], F32) for i in range(2)]
                t_yh, t_yl, t_xr = tracks(2), tracks(2), tracks(2)
                junk = self.sb(es, "mcjunk", [128, D], BF16)
                st = self.sb(es, "mcst", [128, 4], F32)
                t_st = Track()
                g2 = M["g2L"]
                for j, td in enumerate(tiles):
                    i = j % 2
                    P.idma(yh[i][:], None, Ys, IO(ap=slots_i[:, j, 0:1], axis=0), reads=[t_rt, t_slots[j]] + t_Ys, writes=[t_yh[i]])
                    P.idma(yl[i][:], None, Ys, IO(ap=slots_i[:, j, 1:2], axis=0), reads=[t_rt, t_slots[j]] + t_Ys, writes=[t_yl[i]])
                    P.dma("sp", xr[i][:], td["src"], reads=[td["t_src"]], writes=[t_xr[i]])
                    self.ts("dve", yh[i][:], yh[i][:], ghl[:, j, 0:1], None, ALU.mult, None, [t_yh[i], t_rt], [t_yh[i]])
                    self.stt(yh[i][:], yl[i][:], ghl[:, j, 1:2], yh[i][:], ALU.mult, ALU.add, [t_yl[i], t_yh[i], t_rt], [t_yh[i]])
                    self.tt("dve", yh[i][:], yh[i][:], g2[:], ALU.mult, [t_yh[i], M["t"]], [t_yh[i]])
                    self.tt("dve", yh[i][:], yh[i][:], xr[i][:], ALU.add, [t_yh[i], t_xr[i]], [t_yh[i]])
                    col = st[:, i:i + 1]
                    self.act(junk[:], yh[i][:], AF.Square, [t_yh[i]], [t_st], accum_out=col)
                    self.rstd_from(col, col, 1.0 / D, [t_st], t_st)
                    self.stt(yh[i][:], yh[i][:], col, fing[:], ALU.mult, ALU.mult, [t_yh[i], t_st, t_par], [t_yh[i]])
                    P.dma("sp", td["dst"], yh[i][:], reads=[t_yh[i]], writes=[td["t_dst"]])
                P.barrier()

    def build(self):
        nc = self.nc
        with ExitStack() as es:
            self.setup(es)
            P, I = self.P, self.I
            segL, segC = self.segL, self.segC
            groups = [[t for t in range(4 * s, 4 * s + 4)] for s in range(10)]
            for l in range(2):
                with ExitStack() as esl:
                    M = self.mod_phase(l, esl)
                    if STOP_AFTER == "mod":
                        break
                    last = (l == 1)
                    xsrc, t_xsrc = (I["x_ext"], None) if l == 0 else (self.xl1, self.t_xl1)
                    csrc, t_csrc = (I["ctxb"], None) if l == 0 else (self.xc1, self.t_xc1)
                    lo, hi = (0, NT0) if l == 0 else (EXT_LO, EXT_HI)
                    with ExitStack() as es1:
                        B = self.p1_alloc(es1, l)
                        self.phase1(B, segC, [0, 1], csrc, t_csrc, M["gs1C"], M["modC"][:, 0:8], M["t"], self.one1[:, 0:1], kv_only=last)
                        parts = []
                        for s, g in enumerate(groups):
                            tl = [t for t in g if lo <= t < hi]
                            if tl:
                                parts.append(self.phase1_parts(B, segL, tl, xsrc, t_xsrc, M["gs1L"], M["modL"][:, 0:8], M["t"], self.valid[:, s:s + 1]))
                        self.pipeline(parts)
                        P.barrier()
                    if STOP_AFTER == "l%dp1" % l:
                        break
                    lo2, hi2 = (EXT_LO, EXT_HI) if l == 0 else (OWN_LO, OWN_HI)
                    with ExitStack() as es2:
                        B = self.p2_alloc(es2, l)
                        self.table_prep(B, 0, B["TG"], B["t_TG"])
                        self.load_ctx_kv(B)
                        if not last:
                            self.phase2(B, segC, [0, 1], True, csrc, t_csrc, self.xcmid, self.t_xcmid, (M["g1C"], M["t"]))
                        parts = []
                        for g in groups:
                            tl = [t for t in g if lo2 <= t < hi2]
                            if tl:
                                parts.append(self.phase2_parts(B, segL, tl, False, xsrc, t_xsrc, self.xmid, self.t_xmid, (M["g1L"], M["t"])))
                        self.pipeline(parts)
                        P.barrier()
                    if STOP_AFTER == "l%dp2" % l:
                        break
                    tiles = []
                    for t in range(lo2, hi2):
                        if last:
                            dst, t_dst = self.out[(t - OWN_LO) * 128:(t - OWN_LO + 1) * 128, :], self.t_out[t - OWN_LO]
                        else:
                            dst, t_dst = self.xl1[t * 128:(t + 1) * 128, :], self.t_xl1[t]
                        tiles.append({"src": self.xmid[t * 128:(t + 1) * 128, :], "t_src": self.t_xmid[t], "dst": dst, "t_dst": t_dst,
                                      "mod": "L", "final": last})
                    if not last:
                        for t in range(2):
                            tiles.append({"src": self.xcmid[t * 128:(t + 1) * 128, :], "t_src": self.t_xcmid[t],
                                          "dst": self.xc1[t * 128:(t + 1) * 128, :], "t_dst": self.t_xc1[t], "mod": "C", "final": False})
                    blocks = [tiles[i:i + 8] for i in range(0, len(tiles), 8)]
                    if not last:
                        experts = [(I["ffn_w1"], I["ffn_w3"], I["ffn_w2"], DFF // 128)]
                        self.ffn_phase(l, M, blocks, experts, False)
                    else:
                        self.moe_sparse(M, tiles)
                    if STOP_AFTER == "l%d" % l:
                        break
            P.barrier()
        return nc


def build_program():
    nc = bass.Bass("TRN2", target_bir_lowering=False)
    kb = KB(nc)
    kb.t_out = tracks(32)
    kb.build()
    return nc, kb


def kernel(**inputs):
    maps = _prep_inputs(inputs)
    ident = np.eye(128, dtype=np.float32)
    for m in maps:
        m["ident"] = ident
        if STOP_AFTER is not None:
            for k in ("moe_wr",):
                m[k] = np.zeros((128, 128), np.float32)
    nc, kb = build_program()
    res = run_bass_kernel_spmd(nc, maps, core_ids=list(range(NCORES)))
    out = np.zeros((2, 16384, D), np.float32)
    for core in range(NCORES):
        b, R0 = core // 4, 64 * (core % 4)
        out[b, R0 * 64:(R0 + 64) * 64] = res.results[core]["out"]
    kernel.last_results = res.results
    return out
```
